# Optimizing a Trainium2 kernel written in Bass

```python
import math
import jax, jax.numpy as jnp
from jax import lax
import numpy as np

D_MODEL = 1024
BATCH = 8
SEQ = 4096
DEPTH = 1

HEAD_DIM = 64
RWKV_HEADS = 8
ATTN_HEADS = 8
RWKV_WIDTH = RWKV_HEADS * HEAD_DIM
ATTN_WIDTH = ATTN_HEADS * HEAD_DIM
D_MIX = RWKV_WIDTH + ATTN_WIDTH
DECAY_RANK = 64
ICLR_RANK = 64
MOBA_BLOCK = 256
MOBA_TOPK = 3
Q_CHUNK = 32
RMS_EPS = 1e-6
GN_EPS = 64e-5
NEG = -1e30

N_RWKV_COLS = 3 * RWKV_WIDTH + DECAY_RANK + ICLR_RANK
N_ATTN_COLS = 3 * ATTN_WIDTH
D_IN_PROJ = N_RWKV_COLS + N_ATTN_COLS + D_MIX
RWKV_SPLITS = (RWKV_WIDTH, 2 * RWKV_WIDTH, 3 * RWKV_WIDTH, 3 * RWKV_WIDTH + DECAY_RANK)

kernel_name = "hymba_rwkv7_moba_alibi_sandwich"


def rms_norm(x, g):
    xf = x.astype(jnp.float32)
    y = xf * lax.rsqrt(jnp.mean(xf * xf, axis=-1, keepdims=True) + RMS_EPS)
    return (y * g.astype(jnp.float32)).astype(x.dtype)


def _rwkv7_step(S, inp):
    r_t, w_t, k_t, v_t, a_t, b_t = inp
    Sa = jnp.einsum('bhij,bhj->bhi', S, a_t)
    S = S * w_t[:, :, None, :] + Sa[..., None] * b_t[:, :, None, :] + v_t[..., None] * k_t[:, :, None, :]
    y = jnp.einsum('bhij,bhj->bhi', S, r_t)
    return S, y


def rwkv7_time_mix(p, mu, w0, w2, a0, a2, k_k, k_a, r_k, lnx_w, lnx_b):
    B, T, _ = p.shape
    H, N = RWKV_HEADS, HEAD_DIM
    f32 = jnp.float32
    p_prev = jnp.pad(p, ((0, 0), (1, 0), (0, 0)))[:, :T]
    p = p + (p_prev - p) * mu
    r, k, v, wd, ad = jnp.split(p, RWKV_SPLITS, axis=-1)
    w = -jax.nn.softplus(-(w0 + jnp.tanh(wd) @ w2)) - 0.5
    decay = jnp.exp(-jnp.exp(w.astype(f32)))
    a = jax.nn.sigmoid(a0 + ad @ a2)
    kk = (k * k_k).astype(f32).reshape(B, T, H, N)
    kk = kk / jnp.maximum(jnp.linalg.norm(kk, axis=-1, keepdims=True), 1e-12)
    k = k * (1 + (a - 1) * k_a)
    heads = lambda z: z.astype(f32).reshape(B, T, H, N)
    r_h, k_h, v_h, w_h, a_h = heads(r), heads(k), heads(v), heads(decay), heads(a)
    vec_a = -kk
    vec_b = kk * a_h
    tm = lambda z: jnp.moveaxis(z, 1, 0)
    S0 = jnp.zeros((B, H, N, N), f32)
    _, y = lax.scan(_rwkv7_step, S0, (tm(r_h), tm(w_h), tm(k_h), tm(v_h), tm(vec_a), tm(vec_b)))
    y = jnp.moveaxis(y, 0, 1)
    mean = jnp.mean(y, axis=-1, keepdims=True)
    var = jnp.mean(jnp.square(y - mean), axis=-1, keepdims=True)
    y = (y - mean) * lax.rsqrt(var + GN_EPS)
    y = y.reshape(B, T, H * N) * lnx_w.astype(f32) + lnx_b.astype(f32)
    bonus = jnp.sum(r_h * k_h * r_k.astype(f32), axis=-1, keepdims=True) * v_h
    y = y + bonus.reshape(B, T, H * N)
    return y.astype(p.dtype)


def moba_attention(q, k, v):
    B, T, _ = q.shape
    H, Dh, BS = ATTN_HEADS, HEAD_DIM, MOBA_BLOCK
    f32 = jnp.float32
    nb = -(-T // BS)
    tp = nb * BS
    topk = min(MOBA_TOPK, nb)
    C = Q_CHUNK
    nc = T // C
    q = q.reshape(B, T, H, Dh).transpose(0, 2, 1, 3) * (Dh ** -0.5)
    to_heads = lambda z: jnp.pad(z.reshape(B, T, H, Dh).transpose(0, 2, 1, 3),
                                 ((0, 0), (0, 0), (0, tp - T), (0, 0)))
    kp, vp = to_heads(k), to_heads(v)
    kb = kp.reshape(B, H, nb, BS, Dh)
    vb = vp.reshape(B, H, nb, BS, Dh)
    k_mean = jnp.mean(kb.astype(f32), axis=3)
    t_pos = jnp.arange(T)
    q_blk = t_pos // BS
    gate = jnp.einsum('bhtd,bhnd->bhtn', q.astype(f32), k_mean)
    past = jnp.arange(nb)[None, :] < q_blk[:, None]
    gate = jnp.where(past, gate, NEG)
    _, sel = lax.top_k(gate, topk)
    slopes = 2.0 ** (-8.0 * jnp.arange(1, H + 1, dtype=f32) / H)
    q_c = q.reshape(B, H, nc, C, Dh).transpose(2, 0, 1, 3, 4)
    sel_c = sel.reshape(B, H, nc, C, topk).transpose(2, 0, 1, 3, 4)
    b_ix = jnp.arange(B)[:, None, None, None]
    h_ix = jnp.arange(H)[None, :, None, None]

    def chunk(args):
        qc, sc, ci = args
        t = ci * C + jnp.arange(C)
        ob = (ci * C) // BS
        k_sel = kb[b_ix, h_ix, sc]
        v_sel = vb[b_ix, h_ix, sc]
        s_sel = sc[..., None] * BS + jnp.arange(BS)
        lg_sel = jnp.einsum('bhcd,bhcksd->bhcks', qc, k_sel).astype(f32)
        lg_sel = lg_sel - slopes[None, :, None, None, None] * (t[:, None, None] - s_sel).astype(f32)
        valid = jnp.arange(topk)[None, :] < (t // BS)[:, None]
        lg_sel = jnp.where(valid[:, :, None], lg_sel, NEG)
        k_own = lax.dynamic_slice_in_dim(kp, ob * BS, BS, axis=2)
        v_own = lax.dynamic_slice_in_dim(vp, ob * BS, BS, axis=2)
        s_own = ob * BS + jnp.arange(BS)
        lg_own = jnp.einsum('bhcd,bhsd->bhcs', qc, k_own).astype(f32)
        lg_own = lg_own - slopes[:, None, None] * (t[:, None] - s_own[None, :]).astype(f32)
        lg_own = jnp.where(s_own[None, :] <= t[:, None], lg_own, NEG)
        logits = jnp.concatenate([lg_sel.reshape(B, H, C, topk * BS), lg_own], axis=-1)
        prob = jax.nn.softmax(logits, axis=-1)
        p_sel = prob[..., :topk * BS].reshape(B, H, C, topk, BS).astype(vb.dtype)
        p_own = prob[..., topk * BS:].astype(vb.dtype)
        return (jnp.einsum('bhcks,bhcksd->bhcd', p_sel, v_sel)
                + jnp.einsum('bhcs,bhsd->bhcd', p_own, v_own))

    o = lax.map(chunk, (q_c, sel_c, jnp.arange(nc)))
    return o.transpose(1, 0, 3, 2, 4).reshape(B, T, H * Dh)


def setup_inputs(seed: int = 0) -> dict:
    key = jax.random.key(seed)
    ks = jax.random.split(key, 16)
    L, D = DEPTH, D_MODEL
    nrm = jax.random.normal
    return {
        "x": nrm(ks[0], (BATCH, SEQ, D), jnp.float32),
        "g_pre": 1.0 + 0.05 * nrm(ks[1], (L, D), jnp.float32),
        "w_in": nrm(ks[2], (L, D, D_IN_PROJ), jnp.float32) * D ** -0.5,
        "tshift_mu": jax.random.uniform(ks[3], (L, N_RWKV_COLS), jnp.float32),
        "w0": jax.random.uniform(ks[4], (L, RWKV_WIDTH), jnp.float32, -6.0, 1.0),
        "w2": nrm(ks[5], (L, DECAY_RANK, RWKV_WIDTH), jnp.float32) * 0.5 * DECAY_RANK ** -0.5,
        "a0": 0.1 * nrm(ks[6], (L, RWKV_WIDTH), jnp.float32),
        "a2": nrm(ks[7], (L, ICLR_RANK, RWKV_WIDTH), jnp.float32) * 0.5 * ICLR_RANK ** -0.5,
        "k_k": 0.85 + 0.05 * nrm(ks[8], (L, RWKV_WIDTH), jnp.float32),
        "k_a": 1.0 + 0.05 * nrm(ks[9], (L, RWKV_WIDTH), jnp.float32),
        "r_k": 0.1 * nrm(ks[10], (L, RWKV_HEADS, HEAD_DIM), jnp.float32),
        "lnx_w": 1.0 + 0.05 * nrm(ks[11], (L, RWKV_WIDTH), jnp.float32),
        "lnx_b": 0.02 * nrm(ks[12], (L, RWKV_WIDTH), jnp.float32),
        "w_out": nrm(ks[13], (L, D_MIX, D), jnp.float32) * D_MIX ** -0.5,
        "g_post": 1.0 + 0.05 * nrm(ks[14], (L, D), jnp.float32),
    }


def reference(x, g_pre, w_in, tshift_mu, w0, w2, a0, a2, k_k, k_a, r_k, lnx_w, lnx_b, w_out, g_post):
    for l in range(DEPTH):
        h = rms_norm(x, g_pre[l])
        proj = h @ w_in[l]
        p_rwkv = proj[..., :N_RWKV_COLS]
        p_attn = proj[..., N_RWKV_COLS:N_RWKV_COLS + N_ATTN_COLS]
        gates = proj[..., N_RWKV_COLS + N_ATTN_COLS:]
        y_r = rwkv7_time_mix(p_rwkv, tshift_mu[l], w0[l], w2[l], a0[l], a2[l],
                             k_k[l], k_a[l], r_k[l], lnx_w[l], lnx_b[l])
        q, k, v = jnp.split(p_attn, 3, axis=-1)
        y_a = moba_attention(q, k, v)
        y = jnp.concatenate([y_r, y_a], axis=-1) * jax.nn.silu(gates)
        o = y @ w_out[l]
        x = x + rms_norm(o, g_post[l])
    return x
```

```python
import math
from contextlib import ExitStack

import numpy as np
import ml_dtypes

import concourse.bass as bass
import concourse.mybir as mybir
from concourse.bass_utils import run_bass_kernel_spmd

F32 = mybir.dt.float32
BF16 = mybir.dt.bfloat16
AF = mybir.ActivationFunctionType
ALU = mybir.AluOpType
AX = mybir.AxisListType

T = 4096
D = 1024
NT = T // 128
C0 = math.exp(-0.5)
BIG = 30000.0
RMS_EPS = 1e-6
GN_EPS = 64e-5


class Buf:
    __slots__ = ("name", "last_write", "reads", "excl")

    def __init__(self, name="", excl=False):
        self.name = name
        self.last_write = None
        self.reads = {}
        self.excl = excl


class Sched:
    ENG = ("pe", "act", "dve", "pool", "sp")

    def __init__(self, nc, n_dma_sems=48):
        self.nc = nc
        self.lists = {k: [] for k in self.ENG}
        self.cnt = {k: 0 for k in self.ENG}
        self.known = {k: {} for k in self.ENG}
        self.sems = {}
        self.n_dma = n_dma_sems
        self.dma_tot = [0] * n_dma_sems
        self.dma_next = 0
        self._stack = []
        self.total = 0
        self.log = []
        self.max_ops = None
        self.marks = []

    def mark(self, name):
        self.marks.append((name, self.total))

    def _skip(self):
        self.total += 1
        return self.max_ops is not None and self.total > self.max_ops

    def open(self):
        nc = self.nc
        for k in self.ENG:
            cm = nc.semaphore("s_" + k)
            self.sems[k] = cm.__enter__()
            self._stack.append(cm)
        for i in range(self.n_dma):
            cm = nc.semaphore("s_dma%d" % i)
            self.sems[("dma", i)] = cm.__enter__()
            self._stack.append(cm)

    def close(self):
        for cm in reversed(self._stack):
            cm.__exit__(None, None, None)

    def _deps(self, reads, writes):
        deps = {}

        def add(k, v):
            if deps.get(k, 0) < v:
                deps[k] = v

        for b in reads:
            if b.last_write is not None:
                add(*b.last_write)
            if b.excl:
                for k, v in b.reads.items():
                    add(k, v)
        for b in writes:
            if b.last_write is not None:
                add(*b.last_write)
            for k, v in b.reads.items():
                add(k, v)
        return deps

    def _emit_waits(self, eng, deps):
        kn = self.known[eng]
        for k, v in deps.items():
            if k == eng and eng == "pe":
                continue
            if kn.get(k, 0) >= v:
                continue
            kn[k] = v
            sem = self.sems[k]
            self.log.append((eng, "wait", k, v))
            self.lists[eng].append(lambda e, sem=sem, v=v: e.wait_ge(sem, v))

    def op(self, eng, fn, reads=(), writes=()):
        if self._skip():
            return 0
        deps = self._deps(reads, writes)
        self._emit_waits(eng, deps)
        self.cnt[eng] += 1
        v = self.cnt[eng]
        sem = self.sems[eng]
        self.log.append((eng, "op", self.total, v))
        self.lists[eng].append(lambda e, fn=fn, sem=sem: fn(e).then_inc(sem, 1))
        for b in reads:
            if b.reads.get(eng, 0) < v:
                b.reads[eng] = v
        for b in writes:
            b.last_write = (eng, v)
            b.reads = {}
        return v

    def dma(self, eng, out, in_, reads=(), writes=(), force=False):
        if self._skip() and not force:
            return None
        deps = self._deps(reads, writes)
        i = self.dma_next
        self.dma_next = (self.dma_next + 1) % self.n_dma
        k = ("dma", i)
        if self.dma_tot[i] > 0:
            deps[k] = max(deps.get(k, 0), self.dma_tot[i])
        self._emit_waits(eng, deps)
        self.dma_tot[i] += 16
        v = self.dma_tot[i]
        sem = self.sems[k]
        self.lists[eng].append(
            lambda e, out=out, in_=in_, sem=sem: e.dma_start(out=out, in_=in_).then_inc(sem, 16))
        for b in reads:
            if b.reads.get(k, 0) < v:
                b.reads[k] = v
        for b in writes:
            b.last_write = (k, v)
            b.reads = {}
        return (k, v)

    def _all_deps(self):
        deps = {}
        for i in range(self.n_dma):
            if self.dma_tot[i] > 0:
                deps[("dma", i)] = self.dma_tot[i]
        for k in self.ENG:
            if self.cnt[k] > 0:
                deps[k] = self.cnt[k]
        return deps

    def barrier(self):
        deps = self._all_deps()
        for e in self.ENG:
            self._emit_waits(e, dict(deps))

    def emit(self):
        nc = self.nc
        self._emit_waits("sp", self._all_deps())
        with nc.Block() as block:
            @block.sync
            def _(e):
                for f in self.lists["sp"]:
                    f(e)

            @block.tensor
            def _(e):
                for f in self.lists["pe"]:
                    f(e)

            @block.scalar
            def _(e):
                for f in self.lists["act"]:
                    f(e)

            @block.vector
            def _(e):
                for f in self.lists["dve"]:
                    f(e)

            @block.gpsimd
            def _(e):
                for f in self.lists["pool"]:
                    f(e)


def host_consts():
    c = {}
    c["c_ident"] = np.eye(128, dtype=np.float32)
    tri = np.zeros((3, 128, 128), np.float32)
    for s in range(128):
        for t in range(128):
            if s // 64 == t // 64:
                tri[2, s, t] = -C0
                if s <= t:
                    tri[0, s, t] = -C0
                else:
                    tri[1, s, t] = -C0
    c["c_tri"] = tri
    mu = np.triu(np.ones((64, 64), np.float32), 1)
    mle = np.triu(np.ones((64, 64), np.float32), 0)
    c["c_maskAM"] = np.concatenate([-mu, mle], axis=1)
    c["c_maskX"] = np.ascontiguousarray(-mu.T)
    past = np.zeros((16, 16), np.float32)
    own = np.full((16, 16), -BIG, np.float32)
    for blk in range(16):
        for n in range(16):
            if n >= blk:
                past[blk, n] = -1e30
            if n == blk:
                own[blk, n] = 0.0
    c["c_past"] = past.reshape(1, 256)
    c["c_own"] = own.reshape(1, 256)
    bf = ml_dtypes.bfloat16
    onehot = np.zeros((16, T), np.float32)
    for n in range(16):
        onehot[n, n * 256:(n + 1) * 256] = 1.0
    c["c_onehot"] = onehot.astype(bf)
    tw = (np.arange(T) % 256).astype(np.float32)
    qrows = np.zeros((8, 2, T), np.float32)
    krows = np.zeros((8, 2, T), np.float32)
    for h in range(8):
        slope = 2.0 ** (-(h + 1))
        qrows[h, 0] = -slope * tw
        qrows[h, 1] = 1.0
        krows[h, 0] = 1.0
        krows[h, 1] = slope * tw
    c["c_qrows"] = qrows.astype(bf)
    c["c_krows"] = krows.astype(bf)
    causal = np.zeros((128, 2, 256), np.float32)
    for sc in range(2):
        for p in range(128):
            s = sc * 128 + p
            causal[p, sc, :s] = -BIG
    c["c_causal"] = causal.astype(bf)
    return c


CONST_DT = {"c_ident": F32, "c_tri": F32, "c_maskAM": F32, "c_maskX": F32, "c_past": F32, "c_own": F32,
            "c_onehot": BF16, "c_qrows": BF16, "c_krows": BF16, "c_causal": BF16}


def build(dbg=None):
    dbg = dbg or {}
    do_R = dbg.get("R", (0, 1))
    do_M = dbg.get("M", (0, 1, 2, 3))
    do_O = dbg.get("O", True)
    dump_yg = dbg.get("dump_yg", False)
    nt_lim = dbg.get("nt", NT)

    nc = bass.Bass("TRN2", target_bir_lowering=False)
    dr = {}

    def din(name, shape, dt=F32):
        dr[name] = nc.dram_tensor(name, list(shape), dt, kind="ExternalInput").ap()
        return dr[name]

    x = din("x", [T, D])
    w_in = din("w_in", [D, 4224])
    w_out = din("w_out", [D, D])
    gpre_pc = din("gpre_pc", [128, 8])
    mu_row = din("mu_row", [1, 1664])
    w2w0 = din("w2w0", [65, 512])
    a2a0 = din("a2a0", [65, 512])
    prow = din("prow", [5, 512])
    gpost_row = din("gpost_row", [1, D])
    hc = host_consts()
    for k, v in hc.items():
        din(k, v.shape, CONST_DT[k])
    out = nc.dram_tensor("out", [T, D], F32, kind="ExternalOutput").ap()
    if dump_yg:
        ygdump = nc.dram_tensor("ygdump", [128, 8, T], BF16, kind="ExternalOutput").ap()

    S = Sched(nc)
    S.max_ops = dbg.get("max_ops")
    build.last_sched = S
    w_in_v = w_in.rearrange("(c p) n -> p c n", p=128)
    w_out_v = w_out.rearrange("(c p) n -> p c n", p=128)

    with ExitStack() as es:
        S.open()

        def sb(name, shape, dt=F32, stack=es):
            return stack.enter_context(nc.sbuf_tensor(name, list(shape), dt))

        PB = [es.enter_context(nc.psum_tensor("pb%d" % i, [128, 512], F32)) for i in range(8)]
        bPB = [Buf("pb%d" % i, excl=True) for i in range(8)]

        def pb_bf(i):
            return PB[i][:].bitcast(BF16)

        YgT = sb("YgT", [128, 8, T], BF16)
        bYg = [Buf("YgT%d" % i) for i in range(8)]
        identf = sb("identf", [128, 128])
        identb = sb("identb", [128, 128], BF16)
        neghalf = sb("neghalf", [128, 4])
        gpre = sb("gpre", [128, 8])
        bconst = Buf("const")
        S.dma("sp", identf[:], dr["c_ident"][:], writes=[bconst])
        S.dma("sp", gpre[:], gpre_pc[:], writes=[bconst])
        S.op("dve", lambda e: e.tensor_copy(out=identb[:], in_=identf[:]), reads=[bconst], writes=[bconst])
        S.op("pool", lambda e: e.memset(neghalf[:], -0.5), writes=[bconst])

        xt = [sb("xt%d" % i, [128, D]) for i in range(2)]
        bxt = [Buf("xt%d" % i) for i in range(2)]
        xnb = [sb("xnb%d" % i, [128, D], BF16) for i in range(2)]
        bxnb = [Buf("xnb%d" % i) for i in range(2)]
        junk = sb("junk", [128, D], BF16)
        bjunk = Buf("junk")
        fes = [sb("fes%d" % i, [128, 4]) for i in range(2)]
        bfes = [Buf("fes%d" % i) for i in range(2)]

        fe_nbuf = [2]

        def fe1(tau):
            k = tau % fe_nbuf[0]
            xtk, xnbk, fesk, bxtk, bxnbk, bfesk = xt[k], xnb[k], fes[k], bxt[k], bxnb[k], bfes[k]
            S.dma("sp", xtk[:], x[tau * 128:(tau + 1) * 128, :], writes=[bxtk])
            S.op("act", lambda e: e.activation(out=junk[:], in_=xtk[:], func=AF.Square, accum_out=fesk[:, 0:1]),
                 reads=[bxtk], writes=[bjunk, bfesk])
            S.op("pool", lambda e: e.tensor_scalar(out=fesk[:, 1:2], in0=fesk[:, 0:1], scalar1=1.0 / D, scalar2=RMS_EPS,
                                                   op0=ALU.mult, op1=ALU.add), reads=[bfesk], writes=[bfesk])
            S.op("pool", lambda e: e.tensor_tensor(out=fesk[:, 2:3], in0=fesk[:, 1:2], in1=neghalf[:, 0:1], op=ALU.pow),
                 reads=[bfesk, bconst], writes=[bfesk])

        def fe2(tau, dst_ap, bdst, evac_eng="act"):
            k = tau % fe_nbuf[0]
            xtk, xnbk, fesk, bxtk, bxnbk, bfesk = xt[k], xnb[k], fes[k], bxt[k], bxnb[k], bfes[k]
            S.op("act", lambda e: e.activation(out=xnbk[:], in_=xtk[:], func=AF.Copy, scale=fesk[:, 2:3]),
                 reads=[bxtk, bfesk], writes=[bxnbk])
            psT = pb_bf(0).rearrange("p (c t) -> p c t", c=8)
            for c in range(8):
                S.op("pe", lambda e, c=c: e.transpose(out=psT[:, c, :], in_=xnbk[:, c * 128:(c + 1) * 128], identity=identb[:]),
                     reads=[bxnbk, bconst], writes=[bPB[0]])
            if evac_eng == "act":
                S.op("act", lambda e: e.copy(out=dst_ap, in_=psT), reads=[bPB[0]], writes=[bdst])
            else:
                S.op("dve", lambda e: e.tensor_copy(out=dst_ap, in_=psT), reads=[bPB[0]], writes=[bdst])

        def frontend(tau, dst_ap, bdst, evac_eng="act"):
            fe1(tau)
            fe2(tau, dst_ap, bdst, evac_eng)

        wst = [None, None]
        bwst = [None, None]
        wst_ctr = [0]
        wst_gen = [0]

        def alloc_staging(stack):
            g = wst_gen[0]
            wst_gen[0] += 1
            for i in range(2):
                wst[i] = sb("wst%d_%d" % (g, i), [128, 8, 256], stack=stack)
                bwst[i] = Buf("wst%d" % i)

        def load_w(src_ap, ncols):
            k = wst_ctr[0] % 2
            wst_ctr[0] += 1
            S.dma("sp", wst[k][:, :, 0:ncols], src_ap, writes=[bwst[k]])
            return wst[k], bwst[k]

        def phase_R(half):
            with ExitStack() as rs:
                def rsb(name, shape, dt=F32):
                    return sb("R%d_%s" % (half, name), shape, dt, stack=rs)

                ch0 = 256 * half
                tri = rsb("tri", [128, 3, 128])
                maskAM = rsb("maskAM", [128, 128])
                maskX = rsb("maskX", [128, 64])
                pbc = rsb("pbc", [128, 5, 256])
                w2w0h = rsb("w2w0h", [65, 256])
                a2a0h = rsb("a2a0h", [65, 256])
                brc = Buf("rconst")
                S.dma("sp", tri[:], dr["c_tri"].rearrange("k s t -> s k t"), writes=[brc])
                for j in range(2):
                    S.dma("sp", maskAM[64 * j:64 * j + 64, :], dr["c_maskAM"][:], writes=[brc])
                    S.dma("sp", maskX[64 * j:64 * j + 64, :], dr["c_maskX"][:], writes=[brc])
                for i in range(5):
                    S.dma("sp", pbc[:, i, :], prow[i:i + 1, ch0:ch0 + 256].partition_broadcast(128), writes=[brc])
                S.dma("sp", w2w0h[:], w2w0[:, ch0:ch0 + 256], writes=[brc])
                S.dma("sp", a2a0h[:], a2a0[:, ch0:ch0 + 256], writes=[brc])
                W1 = rsb("W1", [128, 8, 896], BF16)
                W2 = rsb("W2", [128, 8, 896], BF16)
                Wg = rsb("Wg", [128, 8, 256], BF16)
                bW = Buf("RW")
                with ExitStack() as ws:
                    alloc_staging(ws)
                    mub = sb("R%d_mub" % half, [128, 896], stack=ws)
                    omub = sb("R%d_omub" % half, [128, 896], stack=ws)
                    bmu = Buf("mu")
                    srcs = [(0 + ch0, 256), (512 + ch0, 256), (1024 + ch0, 256), (1536, 128)]
                    off = 0
                    for (c0, n) in srcs:
                        S.dma("sp", mub[:, off:off + n], mu_row[0:1, c0:c0 + n].partition_broadcast(128), writes=[bmu])
                        off += n
                    S.op("pool", lambda e: e.tensor_scalar(out=omub[:], in0=mub[:], scalar1=-1.0, scalar2=1.0, op0=ALU.mult, op1=ALU.add),
                         reads=[bmu], writes=[bmu])
                    off = 0
                    for (c0, n) in srcs:
                        st, bst = load_w(w_in_v[:, :, c0:c0 + n], n)
                        S.op("pool", lambda e, st=st, n=n: e.tensor_tensor(out=st[:, :, 0:n], in0=st[:, :, 0:n],
                                                                          in1=gpre[:, :, None].to_broadcast([128, 8, n]), op=ALU.mult),
                             reads=[bst, bconst], writes=[bst])
                        S.op("pool", lambda e, st=st, n=n, off=off: e.tensor_tensor(out=W2[:, :, off:off + n], in0=st[:, :, 0:n],
                                                                                   in1=mub[:, None, off:off + n].to_broadcast([128, 8, n]), op=ALU.mult),
                             reads=[bst, bmu], writes=[bW])
                        S.op("pool", lambda e, st=st, n=n, off=off: e.tensor_tensor(out=W1[:, :, off:off + n], in0=st[:, :, 0:n],
                                                                                   in1=omub[:, None, off:off + n].to_broadcast([128, 8, n]), op=ALU.mult),
                             reads=[bst, bmu], writes=[bW])
                        off += n
                    st, bst = load_w(w_in_v[:, :, 3200 + ch0:3200 + ch0 + 256], 256)
                    S.op("pool", lambda e, st=st: e.tensor_tensor(out=Wg[:], in0=st[:, :, 0:256],
                                                                 in1=gpre[:, :, None].to_broadcast([128, 8, 256]), op=ALU.mult),
                         reads=[bst, bconst], writes=[bW])
                    S.barrier()
                NXT = 4
                xT = [rsb("xT%d" % i, [128, 8, 129], BF16) for i in range(NXT)]
                bxT = [Buf("xT%d" % i) for i in range(NXT)]
                TW = rsb("TW", [65, 128]); AD = rsb("AD", [65, 128])
                E3 = rsb("E3", [128, 256]); sq = rsb("sq", [128, 256]); kkn = rsb("kkn", [128, 256])
                kmod = rsb("kmod", [128, 256]); bvec = rsb("bvec", [128, 256]); rkt = rsb("rkt", [128, 256])
                dbl = {}
                for nm_, shp_ in [("rk32", [128, 512]), ("v32", [128, 256]), ("thz", [128, 512]), ("sgi", [128, 512]), ("d3", [128, 256]),
                                  ("E1", [128, 256]), ("E2", [128, 256]), ("E4", [128, 256]), ("kk", [128, 256]), ("km1", [128, 256]),
                                  ("thg", [128, 256]), ("g32", [128, 256]), ("sm", [128, 16])]:
                    dbl[nm_] = [rsb("%s_%d" % (nm_, i_), shp_) for i_ in range(2)]
                bws = [{n: Buf(n) for n in "rk32 v32 thz sgi d3 E1 E2 E4 kk km1 thg g32 sm".split()} for _ in range(2)]
                bw = {n: Buf(n) for n in "TW AD E3 sq kkn kmod bvec rkt".split()}
                S.op("pool", lambda e: e.memset(TW[64:65, :], 1.0), writes=[bw["TW"]])
                S.op("pool", lambda e: e.memset(AD[64:65, :], 1.0), writes=[bw["AD"]])
                TM = [rsb("TM%d" % i, [128, 6, 256], BF16) for i in range(3)]
                Vb = [rsb("Vb%d" % i, [128, 256], BF16) for i in range(3)]
                EC = [rsb("EC%d" % i, [128, 256]) for i in range(3)]
                bonus = [rsb("bonus%d" % i, [128, 256]) for i in range(3)]
                FM = [rsb("FM%d" % i, [128, 4, 4, 64], BF16) for i in range(3)]
                bTM = [Buf() for _ in range(3)]; bVb = [Buf() for _ in range(3)]; bEC = [Buf() for _ in range(3)]
                bbonus = [Buf() for _ in range(3)]; bFM = [Buf() for _ in range(3)]
                SAM = rsb("SAM", [128, 4, 128], BF16); SKM = rsb("SKM", [128, 4, 128], BF16)
                XY = [rsb("XY%d" % i, [128, 2, 4, 64], BF16) for i in range(2)]
                Wt = rsb("Wt", [128, 4, 128], BF16)
                DG = rsb("DG", [128, 4, 64]); MTs = rsb("MTs", [128, 4, 64]); Gs = rsb("Gs", [128, 4, 64])
                RpT = rsb("RpT", [128, 4, 64], BF16)
                Hf = rsb("Hf", [128, 4, 64]); Hb = rsb("Hb", [128, 4, 64], BF16)
                bSAM = [Buf() for _ in range(2)]; bSKM = [Buf() for _ in range(2)]
                bXY = [[Buf() for _ in range(2)] for _ in range(2)]
                bWt = [Buf() for _ in range(2)]; bDG = [Buf() for _ in range(2)]; bMTs = [Buf() for _ in range(2)]
                bGs = [Buf() for _ in range(2)]; bRpT = [Buf() for _ in range(2)]
                bHf = [Buf() for _ in range(2)]; bHb = [Buf() for _ in range(2)]
                S.op("pool", lambda e: e.memset(Hf[:], 0.0), writes=[bHf[0], bHf[1]])
                S.op("pool", lambda e: e.memset(Hb[:], 0.0), writes=[bHb[0], bHb[1]])
                Yt = [rsb("Yt%d" % i, [128, 256]) for i in range(2)]
                bYt = [Buf() for _ in range(2)]
                t1g = [rsb("t1g%d" % i, [128, 256]) for i in range(3)]
                bt1g = [Buf() for _ in range(3)]
                yc = rsb("yc", [128, 256]); ysq = rsb("ysq", [128, 256])
                ygb = rsb("ygb", [128, 256], BF16); pm = rsb("pm", [128, 16])
                bp = {n: Buf(n) for n in "yc ysq ygb pm".split()}

                def tile_RF(tau):
                    kx = tau % NXT
                    if tau == 0:
                        S.op("act", lambda e: e.memset(xT[kx][:, :, 0:1], 0.0), writes=[bxT[kx]]) if False else \
                            S.op("dve", lambda e: e.memset(xT[kx][:, :, 0:1], 0.0), writes=[bxT[kx]])
                    else:
                        kp = (tau - 1) % NXT
                        S.op("act", lambda e: e.copy(out=xT[kx][:, :, 0:1], in_=xT[kp][:, :, 128:129]),
                             reads=[bxT[kp]], writes=[bxT[kx]])
                    frontend(tau, xT[kx][:, :, 1:129], bxT[kx])
                    yield

                def tile_RA(tau):
                    k = tau % NXT
                    k3 = tau % 3
                    ka = tau % 2
                    rk32, v32, thz, sgi, d3, E1, E2, E4, kk, km1, thg, g32, sm = [dbl[n_][ka] for n_ in
                        "rk32 v32 thz sgi d3 E1 E2 E4 kk km1 thg g32 sm".split()]
                    bwd = bws[ka]
                    cur = lambda c: xT[k][:, c, 1:129]
                    prv = lambda c: xT[k][:, c, 0:128]
                    for i, (wt_, xv) in enumerate([(W1, cur), (W2, prv)]):
                        for c in range(8):
                            S.op("pe", lambda e, c=c, wt_=wt_, xv=xv, i=i: e.matmul(PB[1][:, :], lhsT=xv(c), rhs=wt_[:, c, 0:512],
                                                                                 start=(i == 0 and c == 0), stop=(i == 1 and c == 7)),
                                 reads=[bxT[k], bW], writes=[bPB[1]])
                    for i, (wt_, xv) in enumerate([(W1, cur), (W2, prv)]):
                        for c in range(8):
                            S.op("pe", lambda e, c=c, wt_=wt_, xv=xv, i=i: e.matmul(PB[2][:, 0:256], lhsT=xv(c), rhs=wt_[:, c, 512:768],
                                                                                 start=(i == 0 and c == 0), stop=(i == 1 and c == 7)),
                                 reads=[bxT[k], bW], writes=[bPB[2]])
                    for i, (wt_, xv) in enumerate([(W1, cur), (W2, prv)]):
                        for c in range(8):
                            S.op("pe", lambda e, c=c, wt_=wt_, xv=xv, i=i: e.matmul(PB[2][:, 256:384], lhsT=wt_[:, c, 768:896], rhs=xv(c),
                                                                                 start=(i == 0 and c == 0), stop=(i == 1 and c == 7)),
                                 reads=[bxT[k], bW], writes=[bPB[2]])
                    yield
                    S.op("act", lambda e: e.copy(out=rk32[:], in_=PB[1][:, :]), reads=[bPB[1]], writes=[bwd["rk32"]])
                    S.op("act", lambda e: e.copy(out=v32[:], in_=PB[2][:, 0:256]), reads=[bPB[2]], writes=[bwd["v32"]])
                    S.op("act", lambda e: e.copy(out=Vb[k3][:], in_=PB[2][:, 0:256]), reads=[bPB[2]], writes=[bVb[k3]])
                    S.op("act", lambda e: e.activation(out=TW[0:64, :], in_=PB[2][0:64, 256:384], func=AF.Tanh),
                         reads=[bPB[2]], writes=[bw["TW"]])
                    S.op("act", lambda e: e.copy(out=AD[0:64, :], in_=PB[2][64:128, 256:384]), reads=[bPB[2]], writes=[bw["AD"]])
                    yield
                    yield
                    S.op("pe", lambda e: e.matmul(PB[3][:, 0:256], lhsT=TW[:, :], rhs=w2w0h[:, :], start=True, stop=True),
                         reads=[bw["TW"], brc], writes=[bPB[3]])
                    S.op("pe", lambda e: e.matmul(PB[3][:, 256:512], lhsT=AD[:, :], rhs=a2a0h[:, :], start=True, stop=True),
                         reads=[bw["AD"], brc], writes=[bPB[3]])
                    for c in range(8):
                        S.op("pe", lambda e, c=c: e.matmul(PB[1][:, 256:512], lhsT=cur(c), rhs=Wg[:, c, :], start=(c == 0), stop=(c == 7)),
                             reads=[bxT[k], bW], writes=[bPB[1]])
                    S.op("act", lambda e: e.activation(out=thz[:], in_=PB[3][:, :], func=AF.Tanh, scale=0.5), reads=[bPB[3]], writes=[bwd["thz"]])
                    S.op("act", lambda e: e.activation(out=thg[:], in_=PB[1][:, 256:512], func=AF.Tanh, scale=0.5), reads=[bPB[1]], writes=[bwd["thg"]])
                    S.op("act", lambda e: e.copy(out=g32[:], in_=PB[1][:, 256:512]), reads=[bPB[1]], writes=[bwd["g32"]])
                    S.op("pool", lambda e: e.tensor_scalar(out=sgi[:], in0=thz[:], scalar1=0.5, scalar2=0.5, op0=ALU.mult, op1=ALU.add),
                         reads=[bwd["thz"]], writes=[bwd["sgi"]])
                    sg = sgi[:, 0:256]
                    icl = sgi[:, 256:512]
                    r32 = rk32[:, 0:256]
                    k32 = rk32[:, 256:512]
                    v3 = lambda ap: ap.rearrange("p (h c) -> p h c", h=4)
                    S.op("pool", lambda e: e.tensor_scalar(out=km1[:], in0=thz[:, 256:512], scalar1=0.5, scalar2=-0.5, op0=ALU.mult, op1=ALU.add),
                         reads=[bwd["thz"]], writes=[bwd["km1"]])
                    S.op("pool", lambda e: e.tensor_tensor(out=kk[:], in0=k32, in1=pbc[:, 0, :], op=ALU.mult), reads=[bwd["rk32"], brc], writes=[bwd["kk"]])
                    yield
                    yield
                    S.op("pe", lambda e: e.matmul(PB[3][:, 0:256], lhsT=tri[:, 0, :], rhs=sg, start=True, stop=True),
                         reads=[bwd["sgi"], brc], writes=[bPB[3]])
                    S.op("pe", lambda e: e.matmul(PB[3][:, 256:512], lhsT=tri[:, 1, :], rhs=sg, start=True, stop=True),
                         reads=[bwd["sgi"], brc], writes=[bPB[3]])
                    S.op("pe", lambda e: e.matmul(PB[1][:, 0:256], lhsT=tri[:, 2, :], rhs=sg, start=True, stop=True),
                         reads=[bwd["sgi"], brc], writes=[bPB[1]])
                    S.op("act", lambda e: e.activation(out=E1[:], in_=PB[3][:, 0:256], func=AF.Exp), reads=[bPB[3]], writes=[bwd["E1"]])
                    S.op("act", lambda e: e.activation(out=E2[:], in_=PB[3][:, 0:256], func=AF.Exp, scale=-1.0), reads=[bPB[3]], writes=[bwd["E2"]])
                    S.op("act", lambda e: e.activation(out=E4[:], in_=PB[3][:, 256:512], func=AF.Exp), reads=[bPB[3]], writes=[bwd["E4"]])
                    S.op("act", lambda e: e.activation(out=EC[k3][:], in_=PB[1][:, 0:256], func=AF.Exp), reads=[bPB[1]], writes=[bEC[k3]])
                    S.op("act", lambda e: e.activation(out=d3[:], in_=sg, func=AF.Exp, scale=C0), reads=[bwd["sgi"]], writes=[bwd["d3"]])
                    for hl in range(4):
                        S.op("act", lambda e, hl=hl: e.activation(out=sq[:, hl * 64:(hl + 1) * 64], in_=kk[:, hl * 64:(hl + 1) * 64], func=AF.Square,
                                                                 accum_out=sm[:, hl:hl + 1]), reads=[bwd["kk"]], writes=[bw["sq"], bwd["sm"]])
                    yield
                def tile_RB(tau):
                    k = tau % 2
                    k3 = tau % 3
                    ka = tau % 2
                    rk32, v32, thz, sgi, d3, E1, E2, E4, kk, km1, thg, g32, sm = [dbl[n_][ka] for n_ in
                        "rk32 v32 thz sgi d3 E1 E2 E4 kk km1 thg g32 sm".split()]
                    bwd = bws[ka]
                    sg = sgi[:, 0:256]
                    icl = sgi[:, 256:512]
                    r32 = rk32[:, 0:256]
                    k32 = rk32[:, 256:512]
                    v3 = lambda ap: ap.rearrange("p (h c) -> p h c", h=4)
                    S.op("pool", lambda e: e.tensor_scalar(out=t1g[k3][:], in0=thg[:], scalar1=0.5, scalar2=0.5, op0=ALU.mult, op1=ALU.add),
                         reads=[bwd["thg"]], writes=[bt1g[k3]])
                    S.op("pool", lambda e: e.tensor_tensor(out=t1g[k3][:], in0=t1g[k3][:], in1=g32[:], op=ALU.mult), reads=[bt1g[k3], bwd["g32"]], writes=[bt1g[k3]])
                    S.op("pool", lambda e: e.tensor_tensor(out=E3[:], in0=E1[:], in1=d3[:], op=ALU.mult), reads=[bwd["E1"], bwd["d3"]], writes=[bw["E3"]])
                    S.op("pool", lambda e: e.tensor_scalar(out=sm[:, 4:8], in0=sm[:, 0:4], scalar1=1e-24, scalar2=None, op0=ALU.max),
                         reads=[bwd["sm"]], writes=[bwd["sm"]])
                    S.op("pool", lambda e: e.tensor_tensor(out=sm[:, 8:12], in0=sm[:, 4:8], in1=neghalf[:, 0:4], op=ALU.pow),
                         reads=[bwd["sm"], bconst], writes=[bwd["sm"]])
                    S.op("pool", lambda e: e.tensor_tensor(out=v3(kkn[:]), in0=v3(kk[:]), in1=sm[:, 8:12, None].to_broadcast([128, 4, 64]), op=ALU.mult),
                         reads=[bwd["kk"], bwd["sm"]], writes=[bw["kkn"]])
                    yield
                    S.op("pool", lambda e: e.tensor_tensor(out=km1[:], in0=km1[:], in1=pbc[:, 1, :], op=ALU.mult), reads=[bwd["km1"], brc], writes=[bwd["km1"]])
                    S.op("pool", lambda e: e.tensor_tensor(out=kmod[:], in0=km1[:], in1=k32, op=ALU.mult), reads=[bwd["km1"], bwd["rk32"]], writes=[bw["kmod"]])
                    S.op("pool", lambda e: e.tensor_tensor(out=kmod[:], in0=kmod[:], in1=k32, op=ALU.add), reads=[bw["kmod"], bwd["rk32"]], writes=[bw["kmod"]])
                    S.op("pool", lambda e: e.tensor_tensor(out=bvec[:], in0=kkn[:], in1=icl, op=ALU.mult), reads=[bw["kkn"], bwd["sgi"]], writes=[bw["bvec"]])
                    yield
                    S.op("pool", lambda e: e.tensor_tensor(out=TM[k3][:, 0, :], in0=kkn[:], in1=E3[:], op=ALU.mult), reads=[bw["kkn"], bw["E3"]], writes=[bTM[k3]])
                    S.op("pool", lambda e: e.tensor_tensor(out=TM[k3][:, 1, :], in0=r32, in1=E1[:], op=ALU.mult), reads=[bwd["rk32"], bwd["E1"]], writes=[bTM[k3]])
                    S.op("pool", lambda e: e.tensor_tensor(out=TM[k3][:, 2, :], in0=bvec[:], in1=E2[:], op=ALU.mult), reads=[bw["bvec"], bwd["E2"]], writes=[bTM[k3]])
                    S.op("pool", lambda e: e.tensor_tensor(out=TM[k3][:, 3, :], in0=kmod[:], in1=E2[:], op=ALU.mult), reads=[bw["kmod"], bwd["E2"]], writes=[bTM[k3]])
                    yield
                    S.op("pool", lambda e: e.tensor_tensor(out=TM[k3][:, 4, :], in0=bvec[:], in1=E4[:], op=ALU.mult), reads=[bw["bvec"], bwd["E4"]], writes=[bTM[k3]])
                    S.op("pool", lambda e: e.tensor_tensor(out=TM[k3][:, 5, :], in0=kmod[:], in1=E4[:], op=ALU.mult), reads=[bw["kmod"], bwd["E4"]], writes=[bTM[k3]])
                    S.op("pool", lambda e: e.tensor_tensor(out=rkt[:], in0=r32, in1=kmod[:], op=ALU.mult), reads=[bwd["rk32"], bw["kmod"]], writes=[bw["rkt"]])
                    S.op("pool", lambda e: e.tensor_tensor(out=rkt[:], in0=rkt[:], in1=pbc[:, 2, :], op=ALU.mult), reads=[bw["rkt"], brc], writes=[bw["rkt"]])
                    for hl in range(4):
                        S.op("act", lambda e, hl=hl: e.activation(out=sq[:, hl * 64:(hl + 1) * 64], in_=rkt[:, hl * 64:(hl + 1) * 64], func=AF.Copy,
                                                                 accum_out=sm[:, 12 + hl:13 + hl]), reads=[bw["rkt"]], writes=[bw["sq"], bwd["sm"]])
                    yield
                    yield
                    for hp in range(2):
                        fbank = (0, 3)[hp]
                        pf = pb_bf(fbank).rearrange("p (h q t) -> p h q t", h=2, q=4)
                        for hh in range(2):
                            hl = 2 * hp + hh
                            for q in range(4):
                                S.op("pe", lambda e, hh=hh, hl=hl, q=q, pf=pf: e.transpose(out=pf[0:64, hh, q, :], in_=TM[k3][:, q, hl * 64:(hl + 1) * 64],
                                                                                         identity=identb[:]),
                                     reads=[bTM[k3], bconst], writes=[bPB[fbank]])
                        S.op("act", lambda e, hp=hp, pf=pf: e.copy(out=FM[k3][0:64, 2 * hp:2 * hp + 2, :, :], in_=pf[0:64, :, :, 0:64]),
                             reads=[bPB[fbank]], writes=[bFM[k3]])
                        S.op("act", lambda e, hp=hp, pf=pf: e.copy(out=FM[k3][64:128, 2 * hp:2 * hp + 2, :, :], in_=pf[0:64, :, :, 64:128]),
                             reads=[bPB[fbank]], writes=[bFM[k3]])
                        yield
                    S.op("pool", lambda e: e.tensor_tensor(out=v3(bonus[k3][:]), in0=v3(v32[:]), in1=sm[:, 12:16, None].to_broadcast([128, 4, 64]), op=ALU.mult),
                         reads=[bwd["v32"], bwd["sm"]], writes=[bbonus[k3]])

                def chunk_R(tau, j):
                    k = tau % 2
                    k3 = tau % 3
                    lo = 64 * j
                    L = slice(lo, lo + 64)
                    Ba, Bb = 4 + 2 * j, 5 + 2 * j
                    bBa, bBb = bPB[Ba], bPB[Bb]
                    pa3 = PB[Ba][0:64, :].rearrange("p (h c) -> p h c", h=4)
                    pb3 = PB[Bb][0:64, :].rearrange("p (h c) -> p h c", h=4)
                    pa4 = PB[Ba][0:64, :].rearrange("p (a h c) -> p a h c", a=2, h=4)
                    pb4 = PB[Bb][0:64, :].rearrange("p (a h c) -> p a h c", a=2, h=4)
                    fm = FM[k3]
                    for hl in range(4):
                        S.op("pe", lambda e, hl=hl: e.matmul(pa3[:, hl, :].rearrange("p (a b) -> p a b", a=2), lhsT=fm[L, hl, 2, :], rhs=fm[L, hl, 0:2, :],
                                                             start=True, stop=True), reads=[bFM[k3]], writes=[bBa])
                    for hl in range(4):
                        S.op("pe", lambda e, hl=hl: e.matmul(pb3[:, hl, :].rearrange("p (a b) -> p a b", a=2), lhsT=fm[L, hl, 3, :], rhs=fm[L, hl, 0:2, :],
                                                             start=True, stop=True), reads=[bFM[k3]], writes=[bBb])
                    S.op("dve", lambda e: e.tensor_tensor(out=SAM[L, :, :], in0=pa3, in1=maskAM[L, None, :].to_broadcast([64, 4, 128]), op=ALU.mult),
                         reads=[bBa, brc], writes=[bSAM[j]])
                    S.op("dve", lambda e: e.tensor_tensor(out=SKM[L, :, :], in0=pb3, in1=maskAM[L, None, :].to_broadcast([64, 4, 128]), op=ALU.mult),
                         reads=[bBb, brc], writes=[bSKM[j]])
                    yield
                    for hl in range(4):
                        S.op("pe", lambda e, hl=hl: e.matmul(pa4[:, 0, hl, :], lhsT=fm[L, hl, 0, :], rhs=fm[L, hl, 2, :], start=True, stop=True),
                             reads=[bFM[k3]], writes=[bBa])
                    for hl in range(4):
                        S.op("pe", lambda e, hl=hl: e.matmul(pa4[:, 1, hl, :], lhsT=SKM[L, hl, 0:64], rhs=Vb[k3][L, hl * 64:(hl + 1) * 64], start=True, stop=True),
                             reads=[bSKM[j], bVb[k3]], writes=[bBa])
                    xy0 = XY[0]
                    S.op("dve", lambda e: e.tensor_tensor(out=xy0[L, 0, :, :], in0=pa4[:, 0, :, :], in1=maskX[L, None, :].to_broadcast([64, 4, 64]), op=ALU.mult),
                         reads=[bBa, brc], writes=[bXY[0][j]])
                    S.op("dve", lambda e: e.tensor_copy(out=xy0[L, 1, :, :], in_=SAM[L, :, 0:64]), reads=[bSAM[j]], writes=[bXY[0][j]])
                    S.op("dve", lambda e: e.tensor_copy(out=Wt[L, :, 64:128], in_=pa4[:, 1, :, :]), reads=[bBa], writes=[bWt[j]])
                    S.op("dve", lambda e: e.tensor_scalar(out=Wt[L, :, 0:64], in0=TM[k3][L, 0, :].rearrange("p (h c) -> p h c", h=4), scalar1=-1.0, scalar2=None, op0=ALU.mult),
                         reads=[bTM[k3]], writes=[bWt[j]])
                    yield
                    for kx in range(6):
                        cur_, nxt_ = XY[kx % 2], XY[(kx + 1) % 2]
                        bcur, bnxt = bXY[kx % 2][j], bXY[(kx + 1) % 2][j]
                        if kx < 5:
                            for hl in range(4):
                                S.op("pe", lambda e, hl=hl, cur_=cur_: e.matmul(pb4[:, 0, hl, :], lhsT=cur_[L, 1, hl, :], rhs=cur_[L, 0, hl, :], start=True, stop=True),
                                     reads=[bcur], writes=[bBb])
                                S.op("pe", lambda e, hl=hl, cur_=cur_: e.matmul(pb4[:, 1, hl, :], lhsT=cur_[L, 0, hl, :], rhs=cur_[L, 1, hl, :], start=True, stop=True),
                                     reads=[bcur], writes=[bBb])
                        for hl in range(4):
                            S.op("pe", lambda e, hl=hl, cur_=cur_: e.matmul(pa3[:, hl, :], lhsT=cur_[L, 1, hl, :], rhs=Wt[L, hl, :], start=True, stop=True),
                                 reads=[bcur, bWt[j]], writes=[bBa])
                        if kx < 5:
                            S.op("dve", lambda e, nxt_=nxt_: e.tensor_copy(out=nxt_[L, :, :, :], in_=pb4), reads=[bBb], writes=[bnxt])
                        S.op("dve", lambda e: e.tensor_tensor(out=Wt[L, :, :], in0=pa3, in1=Wt[L, :, :], op=ALU.add), reads=[bBa, bWt[j]], writes=[bWt[j]])
                        yield
                    tm = TM[k3]
                    hc = lambda hl: slice(hl * 64, (hl + 1) * 64)
                    for hl in range(4):
                        S.op("pe", lambda e, hl=hl: e.matmul(pb4[:, 0, hl, :], lhsT=Wt[L, hl, 0:64], rhs=tm[L, 4, hc(hl)], start=True, stop=True),
                             reads=[bWt[j], bTM[k3]], writes=[bBb])
                    for hl in range(4):
                        S.op("pe", lambda e, hl=hl: e.matmul(pb4[:, 1, hl, :], lhsT=tm[L, 4, hc(hl)], rhs=Wt[L, hl, 64:128], start=True, stop=False),
                             reads=[bWt[j], bTM[k3]], writes=[bBb])
                        S.op("pe", lambda e, hl=hl: e.matmul(pb4[:, 1, hl, :], lhsT=tm[L, 5, hc(hl)], rhs=Vb[k3][L, hc(hl)], start=False, stop=True),
                             reads=[bVb[k3], bTM[k3]], writes=[bBb])
                    for hl in range(4):
                        S.op("pe", lambda e, hl=hl: e.matmul(pa4[:, 0, hl, :], lhsT=Wt[L, hl, 0:64], rhs=SAM[L, hl, 64:128], start=True, stop=False),
                             reads=[bWt[j], bSAM[j]], writes=[bBa])
                        S.op("pe", lambda e, hl=hl: e.matmul(pa4[:, 0, hl, :], lhsT=tm[L, 1, hc(hl)], rhs=identb[L, L], start=False, stop=True),
                             reads=[bTM[k3], bconst], writes=[bBa])
                    S.op("dve", lambda e: e.tensor_tensor(out=DG[L, :, :], in0=EC[k3][L, :].rearrange("p (h c) -> p h c", h=4),
                                                          in1=identf[L, None, lo:lo + 64].to_broadcast([64, 4, 64]), op=ALU.mult),
                         reads=[bEC[k3], bconst], writes=[bDG[j]])
                    S.op("dve", lambda e: e.tensor_tensor(out=MTs[L, :, :], in0=pb4[:, 0, :, :], in1=DG[L, :, :], op=ALU.add),
                         reads=[bBb, bDG[j]], writes=[bMTs[j]])
                    S.op("dve", lambda e: e.tensor_copy(out=Gs[L, :, :], in_=pb4[:, 1, :, :]), reads=[bBb], writes=[bGs[j]])
                    S.op("dve", lambda e: e.tensor_copy(out=RpT[L, :, :], in_=pa4[:, 0, :, :]), reads=[bBa], writes=[bRpT[j]])
                    yield
                    for hl in range(4):
                        S.op("pe", lambda e, hl=hl: e.matmul(pa4[:, 1, hl, :], lhsT=SAM[L, hl, 64:128], rhs=Wt[L, hl, 64:128], start=True, stop=False),
                             reads=[bWt[j], bSAM[j]], writes=[bBa])
                        S.op("pe", lambda e, hl=hl: e.matmul(pa4[:, 1, hl, :], lhsT=SKM[L, hl, 64:128], rhs=Vb[k3][L, hc(hl)], start=False, stop=False),
                             reads=[bSKM[j], bVb[k3]], writes=[bBa])
                        S.op("pe", lambda e, hl=hl: e.matmul(pa4[:, 1, hl, :], lhsT=RpT[L, hl, :], rhs=Hb[L, hl, :], start=False, stop=True),
                             reads=[bRpT[j], bHb[j]], writes=[bBa])
                    for hl in range(4):
                        S.op("pe", lambda e, hl=hl: e.matmul(pb4[:, 0, hl, :], lhsT=MTs[L, hl, :], rhs=Hf[L, hl, :], start=True, stop=True),
                             reads=[bMTs[j], bHf[j]], writes=[bBb])
                    S.op("dve", lambda e: e.tensor_copy(out=Yt[k][L, :].rearrange("p (h c) -> p h c", h=4), in_=pa4[:, 1, :, :]), reads=[bBa], writes=[bYt[k]])
                    Lo = slice(64 * (1 - j), 64 * (1 - j) + 64)
                    S.op("dve", lambda e: e.tensor_tensor(out=Hf[Lo, :, :], in0=pb4[:, 0, :, :], in1=Gs[L, :, :], op=ALU.add),
                         reads=[bBb, bGs[j]], writes=[bHf[1 - j]])
                    S.op("dve", lambda e: e.tensor_copy(out=Hb[Lo, :, :], in_=Hf[Lo, :, :]), reads=[bHf[1 - j]], writes=[bHb[1 - j]])
                    yield

                def post_R(tau):
                    k = tau % 2
                    k3 = tau % 3
                    v3 = lambda ap: ap.rearrange("p (h c) -> p h c", h=4)
                    yt = Yt[k]
                    for hl in range(4):
                        S.op("act", lambda e, hl=hl: e.activation(out=ysq[:, hl * 64:(hl + 1) * 64], in_=yt[:, hl * 64:(hl + 1) * 64], func=AF.Copy,
                                                                 accum_out=pm[:, hl:hl + 1]), reads=[bYt[k]], writes=[bp["ysq"], bp["pm"]])
                    S.op("pool", lambda e: e.tensor_scalar(out=pm[:, 4:8], in0=pm[:, 0:4], scalar1=1.0 / 64, scalar2=None, op0=ALU.mult),
                         reads=[bp["pm"]], writes=[bp["pm"]])
                    yield
                    S.op("pool", lambda e: e.tensor_tensor(out=v3(yc[:]), in0=v3(yt[:]), in1=pm[:, 4:8, None].to_broadcast([128, 4, 64]), op=ALU.subtract),
                         reads=[bYt[k], bp["pm"]], writes=[bp["yc"]])
                    yield
                    for hl in range(4):
                        S.op("act", lambda e, hl=hl: e.activation(out=ysq[:, hl * 64:(hl + 1) * 64], in_=yc[:, hl * 64:(hl + 1) * 64], func=AF.Square,
                                                                 accum_out=pm[:, 8 + hl:9 + hl]), reads=[bp["yc"]], writes=[bp["ysq"], bp["pm"]])
                    S.op("pool", lambda e: e.tensor_scalar(out=pm[:, 8:12], in0=pm[:, 8:12], scalar1=1.0 / 64, scalar2=GN_EPS, op0=ALU.mult, op1=ALU.add),
                         reads=[bp["pm"]], writes=[bp["pm"]])
                    yield
                    S.op("pool", lambda e: e.tensor_tensor(out=pm[:, 12:16], in0=pm[:, 8:12], in1=neghalf[:, 0:4], op=ALU.pow),
                         reads=[bp["pm"], bconst], writes=[bp["pm"]])
                    S.op("pool", lambda e: e.tensor_tensor(out=v3(yc[:]), in0=v3(yc[:]), in1=pm[:, 12:16, None].to_broadcast([128, 4, 64]), op=ALU.mult),
                         reads=[bp["yc"], bp["pm"]], writes=[bp["yc"]])
                    S.op("pool", lambda e: e.tensor_tensor(out=yc[:], in0=yc[:], in1=pbc[:, 3, :], op=ALU.mult), reads=[bp["yc"], brc], writes=[bp["yc"]])
                    S.op("pool", lambda e: e.tensor_tensor(out=yc[:], in0=yc[:], in1=pbc[:, 4, :], op=ALU.add), reads=[bp["yc"], brc], writes=[bp["yc"]])
                    S.op("pool", lambda e: e.tensor_tensor(out=yc[:], in0=yc[:], in1=bonus[k3][:], op=ALU.add), reads=[bp["yc"], bbonus[k3]], writes=[bp["yc"]])
                    S.op("pool", lambda e: e.tensor_tensor(out=ygb[:], in0=yc[:], in1=t1g[k3][:], op=ALU.mult), reads=[bp["yc"], bt1g[k3]], writes=[bp["ygb"]])
                    yield
                    yield
                    yield
                    yield
                    pyt = pb_bf(3)[:, 0:256].rearrange("p (a t) -> p a t", a=2)
                    for a_ in range(2):
                        S.op("pe", lambda e, a_=a_: e.transpose(out=pyt[:, a_, :], in_=ygb[:, a_ * 128:(a_ + 1) * 128], identity=identb[:]),
                             reads=[bp["ygb"], bconst], writes=[bPB[3]])
                    S.op("act", lambda e: e.copy(out=YgT[:, 2 * half:2 * half + 2, tau * 128:(tau + 1) * 128], in_=pyt),
                         reads=[bPB[3]], writes=[bYg[2 * half], bYg[2 * half + 1]])
                    yield

                def backend_R(tau):
                    g0 = chunk_R(tau, 0)
                    g1 = chunk_R(tau, 1)
                    for st_ in range(9):
                        next(g0)
                        next(g1)
                        yield
                    next(g0)
                    yield
                    next(g1)
                    yield

                def run_streams(streams):
                    live = list(streams)
                    while live:
                        for g in list(live):
                            try:
                                next(g)
                            except StopIteration:
                                live.remove(g)

                ntr = dbg.get('nt_R', {}).get(half, nt_lim)
                a_gen = None; a_next = 0; a_done = 0
                p_gen = None; p_next = 0; p_done = 0
                b_gen = None; b_tile = 0
                f_gen = None; f_next = 0; f_done = 0
                q_gen = None; q_next = 0; q_done = 0
                while q_done < ntr:
                    if f_gen is None and f_next < ntr and f_next <= a_next + dbg.get('aheadF', 2):
                        f_gen = tile_RF(f_next)
                    if f_gen is not None:
                        try:
                            next(f_gen)
                        except StopIteration:
                            f_gen = None
                            f_next += 1
                            f_done = f_next
                    if a_gen is None and a_next < ntr and f_done > a_next and a_next <= p_done + dbg.get('aheadA', 1) and a_next <= b_tile + 2:
                        a_gen = tile_RA(a_next)
                    if a_gen is not None:
                        try:
                            next(a_gen)
                        except StopIteration:
                            a_gen = None
                            a_next += 1
                            a_done = a_next
                    if p_gen is None and p_next < ntr and a_done > p_next and p_next <= q_next + dbg.get('aheadB', 2):
                        p_gen = tile_RB(p_next)
                    if p_gen is not None:
                        try:
                            next(p_gen)
                        except StopIteration:
                            p_gen = None
                            p_next += 1
                            p_done = p_next
                    if b_gen is None and b_tile < ntr and p_done > b_tile and q_done > b_tile - 2:
                        b_gen = backend_R(b_tile)
                    if b_gen is not None:
                        try:
                            next(b_gen)
                        except StopIteration:
                            b_gen = None
                            b_tile += 1
                    if q_gen is None and q_next < ntr and b_tile > q_next:
                        q_gen = post_R(q_next)
                    if q_gen is not None:
                        try:
                            next(q_gen)
                        except StopIteration:
                            q_gen = None
                            q_next += 1
                            q_done = q_next
                S.barrier()

        for half_ in do_R:
            phase_R(half_)

        def phase_M(pair):
            with ExitStack() as ms:
                def msb(name, shape, dt=F32):
                    return sb("M%d_%s" % (pair, name), shape, dt, stack=ms)

                KA = [msb("KA%d" % i, [82, T], BF16) for i in range(2)]
                QA = [msb("QA%d" % i, [82, T], BF16) for i in range(2)]
                Vaug = msb("Vaug", [128, NT, 2, 65], BF16)
                SG = msb("SG", [128, T], BF16)
                GT = msb("GT", [128, T], BF16)
                Yall = msb("Yall", [128, NT, 128], BF16)
                Wp = msb("Wp", [128, 8, 512], BF16)
                xTb = [msb("xTb%d" % i, [128, 8, 512], BF16) for i in range(2)]
                qT32 = msb("qT32", [128, 512])
                kmBD = msb("kmBD", [128, 32])
                pastb = msb("pastb", [128, 16, 16])
                ownb = msb("ownb", [128, 16, 16])
                causal = msb("causal", [128, 2, 256], BF16)
                gm = msb("gm", [128, 4, 16]); m8 = msb("m8", [128, 4, 8]); lt = msb("lt", [128, 4, 16])
                mbts = [msb("mbt%d" % i, [128, 4, 2, 32], BF16) for i in range(2)]
                bmbts = [Buf() for _ in range(2)]
                PT = [msb("PT%d" % i, [128, 2, 256], BF16) for i in range(3)]
                rec = msb("rec", [128, 4])
                for i_ in (2, 3):
                    xt.append(msb("xt%d" % i_, [128, D])); bxt.append(Buf())
                    xnb.append(msb("xnb%d" % i_, [128, D], BF16)); bxnb.append(Buf())
                    fes.append(msb("fes%d" % i_, [128, 4])); bfes.append(Buf())
                fe_nbuf[0] = 4
                bKA = [Buf() for _ in range(2)]; bQA = [Buf() for _ in range(2)]
                bVaug = Buf(); bSG = Buf(); bYall = Buf(); bWp = Buf()
                bxTb = [Buf() for _ in range(2)]; bq32 = Buf(); bkm = Buf(); bmc = Buf()
                bgm = Buf(); bm8 = Buf(); blt = Buf()
                bPT = [Buf() for _ in range(3)]; brec = Buf()
                S.dma("sp", pastb[:].rearrange("p a b -> p (a b)"), dr["c_past"][0:1, :].partition_broadcast(128), writes=[bmc])
                S.dma("sp", ownb[:].rearrange("p a b -> p (a b)"), dr["c_own"][0:1, :].partition_broadcast(128), writes=[bmc])
                S.dma("sp", causal[:], dr["c_causal"][:], writes=[bmc])
                S.op("pool", lambda e: e.memset(kmBD[:], 0.0), writes=[bkm])
                for i_ in range(2):
                    S.op("pool", lambda e, i_=i_: e.memset(mbts[i_][:], 0.0), writes=[bmbts[i_]])
                S.op("pool", lambda e: e.memset(Vaug[:, :, :, 64:65], 1.0), writes=[bVaug])
                for hq in range(2):
                    h = 2 * pair + hq
                    S.dma("sp", KA[hq][64:80, :], dr["c_onehot"][:], writes=[bKA[hq]])
                    S.dma("sp", KA[hq][80:82, :], dr["c_krows"][h], writes=[bKA[hq]])
                    S.dma("sp", QA[hq][80:82, :], dr["c_qrows"][h], writes=[bQA[hq]])
                cols = [1664 + 128 * pair, 2176 + 128 * pair, 2688 + 128 * pair, 3712 + 128 * pair]
                with ExitStack() as wsm:
                    alloc_staging(wsm)
                    for i, c0 in enumerate(cols):
                        st, bst = load_w(w_in_v[:, :, c0:c0 + 128], 128)
                        S.op("pool", lambda e, st=st, i=i: e.tensor_tensor(out=Wp[:, :, 128 * i:128 * i + 128], in0=st[:, :, 0:128],
                                                                          in1=gpre[:, :, None].to_broadcast([128, 8, 128]), op=ALU.mult),
                             reads=[bst, bconst], writes=[bWp])
                    S.barrier()

                nblk = nt_lim // 4 if nt_lim >= 4 else 1
                def m_block(tb):
                    kb = tb % 2
                    xb = xTb[kb]
                    mbt = mbts[tb % 2]
                    bmbt = bmbts[tb % 2]
                    for jj in range(4):
                        tile_ = 4 * tb + jj
                        if tile_ == 0:
                            fe1(0)
                            if 4 * nblk > 1:
                                fe1(1)
                        if tile_ + 2 < 4 * nblk:
                            fe1(tile_ + 2)
                        fe2(tile_, xb[:, :, 128 * jj:128 * jj + 128], bxTb[kb], evac_eng="dve")
                    tsl = slice(512 * tb, 512 * tb + 512)
                    for (bank, wi) in ((1, 1), (2, 0), (4, 3)):
                        for c in range(8):
                            S.op("pe", lambda e, c=c, bank=bank, wi=wi: e.matmul(PB[bank][:, :], lhsT=Wp[:, c, 128 * wi:128 * wi + 128], rhs=xb[:, c, :],
                                                                               start=(c == 0), stop=(c == 7)),
                                 reads=[bWp, bxTb[kb]], writes=[bPB[bank]])
                    pv = PB[3][:, :].rearrange("p (a c) -> p a c", a=4)
                    for jj in range(4):
                        for c in range(8):
                            S.op("pe", lambda e, c=c, jj=jj: e.matmul(pv[:, jj, :], lhsT=xb[:, c, 128 * jj:128 * jj + 128], rhs=Wp[:, c, 256:384],
                                                                     start=(c == 0), stop=(c == 7)),
                                 reads=[bWp, bxTb[kb]], writes=[bPB[3]])
                    S.op("act", lambda e: e.copy(out=KA[0][0:64, tsl], in_=PB[1][0:64, :]), reads=[bPB[1]], writes=[bKA[0]])
                    S.op("act", lambda e: e.copy(out=KA[1][0:64, tsl], in_=PB[1][64:128, :]), reads=[bPB[1]], writes=[bKA[1]])
                    for hq in range(2):
                        for bb in range(2):
                            blk = 2 * tb + bb
                            S.op("dve", lambda e, hq=hq, bb=bb, blk=blk: e.tensor_reduce(out=kmBD[64 * hq:64 * hq + 64, 16 * hq + blk:16 * hq + blk + 1],
                                                                                        in_=PB[1][64 * hq:64 * hq + 64, 256 * bb:256 * bb + 256], axis=AX.X, op=ALU.add),
                                 reads=[bPB[1]], writes=[bkm])
                    S.op("act", lambda e: e.mul(out=QA[0][0:64, tsl], in_=PB[2][0:64, :], mul=0.125), reads=[bPB[2]], writes=[bQA[0]])
                    S.op("act", lambda e: e.mul(out=QA[1][0:64, tsl], in_=PB[2][64:128, :], mul=0.125), reads=[bPB[2]], writes=[bQA[1]])
                    S.op("dve", lambda e: e.tensor_copy(out=qT32[:], in_=PB[2][:, :]), reads=[bPB[2]], writes=[bq32])
                    S.op("act", lambda e: e.activation(out=SG[:, tsl], in_=PB[4][:, :], func=AF.Tanh, scale=0.5), reads=[bPB[4]], writes=[bSG])
                    S.op("dve", lambda e: e.tensor_copy(out=GT[:, tsl], in_=PB[4][:, :]), reads=[bPB[4]], writes=[bSG])
                    S.op("act", lambda e: e.copy(out=Vaug[:, 4 * tb:4 * tb + 4, :, 0:64], in_=PB[3][:, :].rearrange("p (a h c) -> p a h c", a=4, h=2)),
                         reads=[bPB[3]], writes=[bVaug])
                    pg = PB[5][:, 0:128].rearrange("p (a c) -> p a c", a=4)
                    for jj in range(4):
                        S.op("pe", lambda e, jj=jj: e.matmul(pg[:, jj, :], lhsT=qT32[:, 128 * jj:128 * jj + 128], rhs=kmBD[:, :], start=True, stop=True),
                             reads=[bq32, bkm], writes=[bPB[5]])
                    for bb in range(2):
                        blk = 2 * tb + bb
                        pg2 = PB[5][:, 64 * bb:64 * bb + 64].rearrange("p (a c) -> p a c", a=4)
                        S.op("dve", lambda e, pg2=pg2, blk=blk: e.tensor_tensor(out=gm[:], in0=pg2, in1=pastb[:, blk:blk + 1, :].to_broadcast([128, 4, 16]), op=ALU.add),
                             reads=[bPB[5], bmc], writes=[bgm])
                        for g in range(4):
                            S.op("dve", lambda e, g=g: e.max(out=m8[:, g, :], in_=gm[:, g, :]), reads=[bgm], writes=[bm8])
                        S.op("dve", lambda e: e.tensor_tensor(out=lt[:], in0=gm[:], in1=m8[:, :, 2:3].to_broadcast([128, 4, 16]), op=ALU.is_lt),
                             reads=[bgm, bm8], writes=[blt])
                        S.op("dve", lambda e, bb=bb, blk=blk: e.scalar_tensor_tensor(out=mbt[:, 2 * bb:2 * bb + 2, :, 0:16].rearrange("p a h c -> p (a h) c"),
                                                                                   in0=lt[:], scalar=-BIG,
                                                                                   in1=ownb[:, blk:blk + 1, :].to_broadcast([128, 4, 16]),
                                                                                   op0=ALU.mult, op1=ALU.max),
                             reads=[blt, bmc], writes=[bmbt])

                def m_block_b(tb):
                    tsl = slice(512 * tb, 512 * tb + 512)
                    mbt = mbts[tb % 2]
                    bmbt = bmbts[tb % 2]
                    pmt = pb_bf(6)[0:64, 0:512].rearrange("p (a t) -> p a t", a=4)
                    for jj in range(4):
                        S.op("pe", lambda e, jj=jj: e.transpose(out=pmt[:, jj, :], in_=mbt[:, jj, :, :].rearrange("p h c -> p (h c)"), identity=identb[:]),
                             reads=[bmbt, bconst], writes=[bPB[6]])
                    for hq in range(2):
                        S.op("act", lambda e, hq=hq: e.copy(out=QA[hq][64:80, tsl], in_=pb_bf(6)[32 * hq:32 * hq + 16, 0:512]),
                             reads=[bPB[6]], writes=[bQA[hq]])
                for tb in range(nblk):
                    m_block(tb)
                    if tb >= 1:
                        m_block_b(tb - 1)
                m_block_b(nblk - 1)
                S.barrier()
                sbanks = [1, 2, 3, 4]
                obanks = [5, 6]
                items = [(hq, i) for hq in range(2) for i in range(nblk * 2)]
                sctr = [0]

                def qk_stage(hq, i):
                    h = 2 * pair + hq
                    slope = 2.0 ** (-(h + 1))
                    res = []
                    for n in range(i + 1):
                        bank = sbanks[sctr[0] % 4]
                        pi = sctr[0] % 3
                        sctr[0] += 1
                        ps = PB[bank][:, :].rearrange("p (a t) -> p a t", a=2)
                        for sc in range(2):
                            s0 = 256 * n + 128 * sc
                            S.op("pe", lambda e, sc=sc, s0=s0, ps=ps, n=n: e.matmul(ps[:, sc, :], lhsT=KA[hq][0:82, s0:s0 + 128], rhs=QA[hq][0:82, 256 * i:256 * i + 256],
                                                                             start=True, stop=(n != i)),
                                 reads=[bKA[hq], bQA[hq]], writes=[bPB[bank]])
                            if n == i:
                                S.op("pe", lambda e, sc=sc, ps=ps: e.matmul(ps[:, sc, :], lhsT=identb[:, :], rhs=causal[:, sc, :], start=False, stop=True),
                                     reads=[bconst, bmc], writes=[bPB[bank]])
                        S.op("act", lambda e, ps=ps, pi=pi, n=n: e.activation(out=PT[pi][:], in_=ps, func=AF.Exp, bias=float(-slope * 256.0 * (i - n)), scale=1.0),
                             reads=[bPB[bank]], writes=[bPT[pi]])
                        res.append((pi, n))
                        yield (pi, n)

                LOOK = dbg.get("look", 2)

                def pv_stage(hq, i, it, pi, n):
                    ob_idx = ((5, 6), (7, 0))[it % 2]
                    ob = PB[ob_idx[0]], PB[ob_idx[1]]
                    for tc in range(2):
                        for sc in range(2):
                            S.op("pe", lambda e, tc=tc, sc=sc: e.matmul(ob[tc][:, 0:65], lhsT=PT[pi][:, sc, 128 * tc:128 * tc + 128],
                                                                       rhs=Vaug[:, 2 * n + sc, hq, :],
                                                                       start=(n == 0 and sc == 0), stop=(n == i and sc == 1)),
                                 reads=[bPT[pi], bVaug], writes=[bPB[ob_idx[tc]]])
                    if n == i:
                        for tc in range(2):
                            rc = rec[:, 2 * (it % 2) + tc:2 * (it % 2) + tc + 1]
                            S.op("dve", lambda e, tc=tc, rc=rc: e.reciprocal(out=rc, in_=ob[tc][:, 64:65]), reads=[bPB[ob_idx[tc]]], writes=[brec])
                            S.op("dve", lambda e, tc=tc, rc=rc: e.tensor_scalar(out=Yall[:, 2 * i + tc, 64 * hq:64 * hq + 64], in0=ob[tc][:, 0:64], scalar1=rc,
                                                                               scalar2=None, op0=ALU.mult),
                                 reads=[bPB[ob_idx[tc]], brec], writes=[bYall])

                pending = []
                for it, (hq, i) in enumerate(items):
                    for (pi, n) in qk_stage(hq, i):
                        pending.append((hq, i, it, pi, n))
                        if len(pending) > LOOK:
                            pv_stage(*pending.pop(0))
                while pending:
                    pv_stage(*pending.pop(0))
                ygm = msb("ygm", [128, 512], BF16)
                t1m = msb("t1m", [128, 512], BF16)
                bygm = Buf(); bt1m = Buf()
                def m_gate(tb):
                    tsl = slice(512 * tb, 512 * tb + 512)
                    pyt = pb_bf(7)[:, 0:512].rearrange("p (a t) -> p a t", a=4)
                    for jj in range(4):
                        S.op("pe", lambda e, jj=jj, tb=tb: e.transpose(out=pyt[:, jj, :], in_=Yall[:, 4 * tb + jj, :], identity=identb[:]),
                             reads=[bYall, bconst], writes=[bPB[7]])
                    S.op("dve", lambda e, tsl=tsl: e.scalar_tensor_tensor(out=t1m[:], in0=SG[:, tsl], scalar=1.0, in1=GT[:, tsl], op0=ALU.add, op1=ALU.mult),
                         reads=[bSG], writes=[bt1m])
                    S.op("dve", lambda e, tsl=tsl, pyt=pyt: e.scalar_tensor_tensor(out=YgT[:, 4 + pair, tsl], in0=t1m[:], scalar=0.5,
                                                                                  in1=pyt.rearrange("p a t -> p (a t)"), op0=ALU.mult, op1=ALU.mult),
                         reads=[bt1m, bPB[7]], writes=[bYg[4 + pair]])
                for tb in range(nblk):
                    m_gate(tb)
                S.barrier()
                for lst_ in (xt, bxt, xnb, bxnb, fes, bfes):
                    del lst_[2:]
                fe_nbuf[0] = 2

        for pair_ in do_M:
            phase_M(pair_)

        if dump_yg:
            for c in dbg.get("dump_chunks", range(8)):
                S.dma("sp", ygdump[:, c, 0:nt_lim * 128], YgT[:, c, 0:nt_lim * 128], reads=[bYg[c]], force=True)
        if do_O:
            with ExitStack() as os_:
                def osb(name, shape, dt=F32):
                    return sb("O_" + name, shape, dt, stack=os_)
                Wo = osb("Wo", [128, 8, D], BF16)
                gpb = osb("gpb", [128, D])
                bWo = Buf(); bgp = Buf()
                S.dma("sp", gpb[:], gpost_row[0:1, :].partition_broadcast(128), writes=[bgp])
                with ExitStack() as wso:
                    alloc_staging(wso)
                    for m4 in range(4):
                        st, bst = load_w(w_out_v[:, :, 256 * m4:256 * m4 + 256], 256)
                        S.op("pool", lambda e, st=st, m4=m4: e.tensor_copy(out=Wo[:, :, 256 * m4:256 * m4 + 256], in_=st[:, :, 0:256]),
                             reads=[bst], writes=[bWo])
                    S.barrier()
                ot = [osb("ot%d" % i, [128, D]) for i in range(2)]
                bot = [Buf() for _ in range(2)]
                osm = [osb("osm%d" % i, [128, 8]) for i in range(2)]
                bosm = [Buf() for _ in range(2)]
                ojunk = osb("ojunk", [128, 512], BF16)
                bojunk = Buf()
                def o_tile(tau):
                    k = tau % 2
                    banks = (1 + 2 * k, 2 + 2 * k)
                    if tau == 0:
                        S.dma("sp", xt[0][:], x[0:128, :], writes=[bxt[0]])
                    if tau + 1 < nt_lim:
                        S.dma("sp", xt[1 - k][:], x[(tau + 1) * 128:(tau + 2) * 128, :], writes=[bxt[1 - k]])
                    for hf in range(2):
                        for m in range(8):
                            S.op("pe", lambda e, m=m, hf=hf: e.matmul(PB[banks[hf]][:, :], lhsT=YgT[:, m, tau * 128:(tau + 1) * 128], rhs=Wo[:, m, 512 * hf:512 * hf + 512],
                                                                     start=(m == 0), stop=(m == 7)),
                                 reads=[bYg[m], bWo], writes=[bPB[banks[hf]]])
                    for hf in range(2):
                        S.op("act", lambda e, hf=hf: e.activation(out=ojunk[:], in_=PB[banks[hf]][:, :], func=AF.Square, accum_out=osm[k][:, hf:hf + 1]),
                             reads=[bPB[banks[hf]]], writes=[bojunk, bosm[k]])
                    S.op("dve", lambda e: e.tensor_tensor(out=osm[k][:, 2:3], in0=osm[k][:, 0:1], in1=osm[k][:, 1:2], op=ALU.add), reads=[bosm[k]], writes=[bosm[k]])
                    S.op("dve", lambda e: e.tensor_scalar(out=osm[k][:, 3:4], in0=osm[k][:, 2:3], scalar1=1.0 / D, scalar2=RMS_EPS, op0=ALU.mult, op1=ALU.add),
                         reads=[bosm[k]], writes=[bosm[k]])
                    S.op("pool", lambda e: e.tensor_tensor(out=osm[k][:, 4:5], in0=osm[k][:, 3:4], in1=neghalf[:, 0:1], op=ALU.pow),
                         reads=[bosm[k], bconst], writes=[bosm[k]])
                    for hf in range(2):
                        S.op("dve", lambda e, hf=hf: e.scalar_tensor_tensor(out=ot[k][:, 512 * hf:512 * hf + 512], in0=PB[banks[hf]][:, :], scalar=osm[k][:, 4:5],
                                                                           in1=gpb[:, 512 * hf:512 * hf + 512], op0=ALU.mult, op1=ALU.mult),
                             reads=[bPB[banks[hf]], bosm[k], bgp], writes=[bot[k]])
                    S.op("pool", lambda e: e.tensor_tensor(out=ot[k][:], in0=ot[k][:], in1=xt[k][:], op=ALU.add), reads=[bot[k], bxt[k]], writes=[bot[k]])
                    S.dma("sp", out[tau * 128:(tau + 1) * 128, :], ot[k][:], reads=[bot[k]])
                for tau in range(nt_lim):
                    o_tile(tau)
        S.emit()
        S.close()
    return nc


def make_inputs(x_b, p):
    m = {"x": np.ascontiguousarray(x_b, dtype=np.float32)}
    m["w_in"] = np.ascontiguousarray(p["w_in"][0], dtype=np.float32)
    m["w_out"] = np.ascontiguousarray(p["w_out"][0], dtype=np.float32)
    m["gpre_pc"] = np.ascontiguousarray(p["g_pre"][0].reshape(8, 128).T, dtype=np.float32)
    m["mu_row"] = np.ascontiguousarray(p["tshift_mu"][0].reshape(1, 1664), dtype=np.float32)
    m["w2w0"] = np.ascontiguousarray(np.concatenate([p["w2"][0], p["w0"][0].reshape(1, 512)], axis=0), dtype=np.float32)
    m["a2a0"] = np.ascontiguousarray(np.concatenate([p["a2"][0], p["a0"][0].reshape(1, 512)], axis=0), dtype=np.float32)
    m["prow"] = np.ascontiguousarray(np.stack([p["k_k"][0], p["k_a"][0], p["r_k"][0].reshape(512), p["lnx_w"][0], p["lnx_b"][0]], axis=0),
                                     dtype=np.float32)
    m["gpost_row"] = np.ascontiguousarray(p["g_post"][0].reshape(1, D), dtype=np.float32)
    m.update(host_consts())
    return m


def kernel(x, g_pre, w_in, tshift_mu, w0, w2, a0, a2, k_k, k_a, r_k, lnx_w, lnx_b, w_out, g_post):
    p = dict(g_pre=g_pre, w_in=w_in, tshift_mu=tshift_mu, w0=w0, w2=w2, a0=a0, a2=a2, k_k=k_k, k_a=k_a, r_k=r_k,
             lnx_w=lnx_w, lnx_b=lnx_b, w_out=w_out, g_post=g_post)
    p = {k: np.asarray(v) for k, v in p.items()}
    x = np.asarray(x)
    nc = build()
    in_maps = [make_inputs(x[b], p) for b in range(8)]
    res = run_bass_kernel_spmd(nc, in_maps, core_ids=list(range(8)))
    return np.stack([np.asarray(r["out"], dtype=np.float32) for r in res.results], axis=0)
```

```python
import math
from contextlib import ExitStack

import numpy as np
import ml_dtypes

import concourse.bass as bass
import concourse.mybir as mybir
from concourse.bass_utils import run_bass_kernel_spmd

F32 = mybir.dt.float32
BF16 = mybir.dt.bfloat16
AF = mybir.ActivationFunctionType
ALU = mybir.AluOpType
AX = mybir.AxisListType

T = 4096
D = 1024
NT = T // 128
C0 = math.exp(-0.5)
BIG = 30000.0
RMS_EPS = 1e-6
GN_EPS = 64e-5


class Buf:
    __slots__ = ("name", "last_write", "reads", "excl")

    def __init__(self, name="", excl=False):
        self.name = name
        self.last_write = None
        self.reads = {}
        self.excl = excl


class Sched:
    ENG = ("pe", "act", "dve", "pool", "sp")

    def __init__(self, nc, n_dma_sems=48):
        self.nc = nc
        self.lists = {k: [] for k in self.ENG}
        self.cnt = {k: 0 for k in self.ENG}
        self.known = {k: {} for k in self.ENG}
        self.sems = {}
        self.n_dma = n_dma_sems
        self.dma_tot = [0] * n_dma_sems
        self.dma_next = 0
        self._stack = []
        self.total = 0
        self.log = []
        self.max_ops = None
        self.marks = []

    def mark(self, name):
        self.marks.append((name, self.total))

    def _skip(self):
        self.total += 1
        return self.max_ops is not None and self.total > self.max_ops

    def open(self):
        nc = self.nc
        for k in self.ENG:
            cm = nc.semaphore("s_" + k)
            self.sems[k] = cm.__enter__()
            self._stack.append(cm)
        for i in range(self.n_dma):
            cm = nc.semaphore("s_dma%d" % i)
            self.sems[("dma", i)] = cm.__enter__()
            self._stack.append(cm)

    def close(self):
        for cm in reversed(self._stack):
            cm.__exit__(None, None, None)

    def _deps(self, reads, writes):
        deps = {}

        def add(k, v):
            if deps.get(k, 0) < v:
                deps[k] = v

        for b in reads:
            if b.last_write is not None:
                add(*b.last_write)
            if b.excl:
                for k, v in b.reads.items():
                    add(k, v)
        for b in writes:
            if b.last_write is not None:
                add(*b.last_write)
            for k, v in b.reads.items():
                add(k, v)
        return deps

    def _emit_waits(self, eng, deps):
        kn = self.known[eng]
        for k, v in deps.items():
            if k == eng and eng == "pe":
                continue
            if kn.get(k, 0) >= v:
                continue
            kn[k] = v
            sem = self.sems[k]
            self.log.append((eng, "wait", k, v))
            self.lists[eng].append(lambda e, sem=sem, v=v: e.wait_ge(sem, v))

    def op(self, eng, fn, reads=(), writes=()):
        if self._skip():
            return 0
        deps = self._deps(reads, writes)
        self._emit_waits(eng, deps)
        self.cnt[eng] += 1
        v = self.cnt[eng]
        sem = self.sems[eng]
        self.log.append((eng, "op", self.total, v))
        self.lists[eng].append(lambda e, fn=fn, sem=sem: fn(e).then_inc(sem, 1))
        for b in reads:
            if b.reads.get(eng, 0) < v:
                b.reads[eng] = v
        for b in writes:
            b.last_write = (eng, v)
            b.reads = {}
        return v

    def dma(self, eng, out, in_, reads=(), writes=(), force=False):
        if self._skip() and not force:
            return None
        deps = self._deps(reads, writes)
        i = self.dma_next
        self.dma_next = (self.dma_next + 1) % self.n_dma
        k = ("dma", i)
        if self.dma_tot[i] > 0:
            deps[k] = max(deps.get(k, 0), self.dma_tot[i])
        self._emit_waits(eng, deps)
        self.dma_tot[i] += 16
        v = self.dma_tot[i]
        sem = self.sems[k]
        self.lists[eng].append(
            lambda e, out=out, in_=in_, sem=sem: e.dma_start(out=out, in_=in_).then_inc(sem, 16))
        for b in reads:
            if b.reads.get(k, 0) < v:
                b.reads[k] = v
        for b in writes:
            b.last_write = (k, v)
            b.reads = {}
        return (k, v)

    def _all_deps(self):
        deps = {}
        for i in range(self.n_dma):
            if self.dma_tot[i] > 0:
                deps[("dma", i)] = self.dma_tot[i]
        for k in self.ENG:
            if self.cnt[k] > 0:
                deps[k] = self.cnt[k]
        return deps

    def barrier(self):
        deps = self._all_deps()
        for e in self.ENG:
            self._emit_waits(e, dict(deps))

    def emit(self):
        nc = self.nc
        self._emit_waits("sp", self._all_deps())
        with nc.Block() as block:
            @block.sync
            def _(e):
                for f in self.lists["sp"]:
                    f(e)

            @block.tensor
            def _(e):
                for f in self.lists["pe"]:
                    f(e)

            @block.scalar
            def _(e):
                for f in self.lists["act"]:
                    f(e)

            @block.vector
            def _(e):
                for f in self.lists["dve"]:
                    f(e)

            @block.gpsimd
            def _(e):
                for f in self.lists["pool"]:
                    f(e)


def host_consts():
    c = {}
    c["c_ident"] = np.eye(128, dtype=np.float32)
    tri = np.zeros((3, 128, 128), np.float32)
    for s in range(128):
        for t in range(128):
            if s // 64 == t // 64:
                tri[2, s, t] = -C0
                if s <= t:
                    tri[0, s, t] = -C0
                else:
                    tri[1, s, t] = -C0
    c["c_tri"] = tri
    mu = np.triu(np.ones((64, 64), np.float32), 1)
    mle = np.triu(np.ones((64, 64), np.float32), 0)
    c["c_maskAM"] = np.concatenate([-mu, mle], axis=1)
    c["c_maskX"] = np.ascontiguousarray(-mu.T)
    past = np.zeros((16, 16), np.float32)
    own = np.full((16, 16), -BIG, np.float32)
    for blk in range(16):
        for n in range(16):
            if n >= blk:
                past[blk, n] = -1e30
            if n == blk:
                own[blk, n] = 0.0
    c["c_past"] = past.reshape(1, 256)
    c["c_own"] = own.reshape(1, 256)
    bf = ml_dtypes.bfloat16
    onehot = np.zeros((16, T), np.float32)
    for n in range(16):
        onehot[n, n * 256:(n + 1) * 256] = 1.0
    c["c_onehot"] = onehot.astype(bf)
    tw = (np.arange(T) % 256).astype(np.float32)
    qrows = np.zeros((8, 2, T), np.float32)
    krows = np.zeros((8, 2, T), np.float32)
    for h in range(8):
        slope = 2.0 ** (-(h + 1))
        qrows[h, 0] = -slope * tw
        qrows[h, 1] = 1.0
        krows[h, 0] = 1.0
        krows[h, 1] = slope * tw
    c["c_qrows"] = qrows.astype(bf)
    c["c_krows"] = krows.astype(bf)
    causal = np.zeros((128, 2, 256), np.float32)
    for sc in range(2):
        for p in range(128):
            s = sc * 128 + p
            causal[p, sc, :s] = -BIG
    c["c_causal"] = causal.astype(bf)
    return c


CONST_DT = {"c_ident": F32, "c_tri": F32, "c_maskAM": F32, "c_maskX": F32, "c_past": F32, "c_own": F32,
            "c_onehot": BF16, "c_qrows": BF16, "c_krows": BF16, "c_causal": BF16}


def build(dbg=None):
    dbg = dbg or {}
    do_R = dbg.get("R", (0, 1))
    do_M = dbg.get("M", (0, 1, 2, 3))
    do_O = dbg.get("O", True)
    dump_yg = dbg.get("dump_yg", False)
    nt_lim = dbg.get("nt", NT)

    nc = bass.Bass("TRN2", target_bir_lowering=False)
    dr = {}

    def din(name, shape, dt=F32):
        dr[name] = nc.dram_tensor(name, list(shape), dt, kind="ExternalInput").ap()
        return dr[name]

    x = din("x", [T, D])
    w_in = din("w_in", [D, 4224])
    w_out = din("w_out", [D, D])
    gpre_pc = din("gpre_pc", [128, 8])
    mu_row = din("mu_row", [1, 1664])
    w2w0 = din("w2w0", [65, 512])
    a2a0 = din("a2a0", [65, 512])
    prow = din("prow", [5, 512])
    gpost_row = din("gpost_row", [1, D])
    hc = host_consts()
    for k, v in hc.items():
        din(k, v.shape, CONST_DT[k])
    out = nc.dram_tensor("out", [T, D], F32, kind="ExternalOutput").ap()
    if dump_yg:
        ygdump = nc.dram_tensor("ygdump", [128, 8, T], BF16, kind="ExternalOutput").ap()

    S = Sched(nc)
    S.max_ops = dbg.get("max_ops")
    build.last_sched = S
    w_in_v = w_in.rearrange("(c p) n -> p c n", p=128)
    w_out_v = w_out.rearrange("(c p) n -> p c n", p=128)

    with ExitStack() as es:
        S.open()

        def sb(name, shape, dt=F32, stack=es):
            return stack.enter_context(nc.sbuf_tensor(name, list(shape), dt))

        PB = [es.enter_context(nc.psum_tensor("pb%d" % i, [128, 512], F32)) for i in range(8)]
        bPB = [Buf("pb%d" % i, excl=True) for i in range(8)]

        def pb_bf(i):
            return PB[i][:].bitcast(BF16)

        YgT = sb("YgT", [128, 8, T], BF16)
        bYg = [Buf("YgT%d" % i) for i in range(8)]
        identf = sb("identf", [128, 128])
        identb = sb("identb", [128, 128], BF16)
        neghalf = sb("neghalf", [128, 4])
        gpre = sb("gpre", [128, 8])
        bconst = Buf("const")
        S.dma("sp", identf[:], dr["c_ident"][:], writes=[bconst])
        S.dma("sp", gpre[:], gpre_pc[:], writes=[bconst])
        S.op("dve", lambda e: e.tensor_copy(out=identb[:], in_=identf[:]), reads=[bconst], writes=[bconst])
        S.op("pool", lambda e: e.memset(neghalf[:], -0.5), writes=[bconst])

        xt = [sb("xt%d" % i, [128, D]) for i in range(2)]
        bxt = [Buf("xt%d" % i) for i in range(2)]
        xnb = [sb("xnb%d" % i, [128, D], BF16) for i in range(2)]
        bxnb = [Buf("xnb%d" % i) for i in range(2)]
        junk = sb("junk", [128, D], BF16)
        bjunk = Buf("junk")
        fes = [sb("fes%d" % i, [128, 4]) for i in range(2)]
        bfes = [Buf("fes%d" % i) for i in range(2)]

        fe_nbuf = [2]

        def fe1(tau):
            k = tau % fe_nbuf[0]
            xtk, xnbk, fesk, bxtk, bxnbk, bfesk = xt[k], xnb[k], fes[k], bxt[k], bxnb[k], bfes[k]
            S.dma("sp", xtk[:], x[tau * 128:(tau + 1) * 128, :], writes=[bxtk])
            S.op("act", lambda e: e.activation(out=junk[:], in_=xtk[:], func=AF.Square, accum_out=fesk[:, 0:1]),
                 reads=[bxtk], writes=[bjunk, bfesk])
            S.op("pool", lambda e: e.tensor_scalar(out=fesk[:, 1:2], in0=fesk[:, 0:1], scalar1=1.0 / D, scalar2=RMS_EPS,
                                                   op0=ALU.mult, op1=ALU.add), reads=[bfesk], writes=[bfesk])
            S.op("pool", lambda e: e.tensor_tensor(out=fesk[:, 2:3], in0=fesk[:, 1:2], in1=neghalf[:, 0:1], op=ALU.pow),
                 reads=[bfesk, bconst], writes=[bfesk])

        def fe2(tau, dst_ap, bdst, evac_eng="act"):
            k = tau % fe_nbuf[0]
            xtk, xnbk, fesk, bxtk, bxnbk, bfesk = xt[k], xnb[k], fes[k], bxt[k], bxnb[k], bfes[k]
            S.op("act", lambda e: e.activation(out=xnbk[:], in_=xtk[:], func=AF.Copy, scale=fesk[:, 2:3]),
                 reads=[bxtk, bfesk], writes=[bxnbk])
            psT = pb_bf(0).rearrange("p (c t) -> p c t", c=8)
            for c in range(8):
                S.op("pe", lambda e, c=c: e.transpose(out=psT[:, c, :], in_=xnbk[:, c * 128:(c + 1) * 128], identity=identb[:]),
                     reads=[bxnbk, bconst], writes=[bPB[0]])
            if evac_eng == "act":
                S.op("act", lambda e: e.copy(out=dst_ap, in_=psT), reads=[bPB[0]], writes=[bdst])
            else:
                S.op("dve", lambda e: e.tensor_copy(out=dst_ap, in_=psT), reads=[bPB[0]], writes=[bdst])

        def frontend(tau, dst_ap, bdst, evac_eng="act"):
            fe1(tau)
            fe2(tau, dst_ap, bdst, evac_eng)

        wst = [None, None]
        bwst = [None, None]
        wst_ctr = [0]
        wst_gen = [0]

        def alloc_staging(stack):
            g = wst_gen[0]
            wst_gen[0] += 1
            for i in range(2):
                wst[i] = sb("wst%d_%d" % (g, i), [128, 8, 256], stack=stack)
                bwst[i] = Buf("wst%d" % i)

        def load_w(src_ap, ncols):
            k = wst_ctr[0] % 2
            wst_ctr[0] += 1
            S.dma("sp", wst[k][:, :, 0:ncols], src_ap, writes=[bwst[k]])
            return wst[k], bwst[k]

        def phase_R(half):
            with ExitStack() as rs:
                def rsb(name, shape, dt=F32):
                    return sb("R%d_%s" % (half, name), shape, dt, stack=rs)

                ch0 = 256 * half
                tri = rsb("tri", [128, 3, 128])
                maskAM = rsb("maskAM", [128, 128])
                maskX = rsb("maskX", [128, 64])
                pbc = rsb("pbc", [128, 5, 256])
                w2w0h = rsb("w2w0h", [65, 256])
                a2a0h = rsb("a2a0h", [65, 256])
                brc = Buf("rconst")
                S.dma("sp", tri[:], dr["c_tri"].rearrange("k s t -> s k t"), writes=[brc])
                for j in range(2):
                    S.dma("sp", maskAM[64 * j:64 * j + 64, :], dr["c_maskAM"][:], writes=[brc])
                    S.dma("sp", maskX[64 * j:64 * j + 64, :], dr["c_maskX"][:], writes=[brc])
                for i in range(5):
                    S.dma("sp", pbc[:, i, :], prow[i:i + 1, ch0:ch0 + 256].partition_broadcast(128), writes=[brc])
                S.dma("sp", w2w0h[:], w2w0[:, ch0:ch0 + 256], writes=[brc])
                S.dma("sp", a2a0h[:], a2a0[:, ch0:ch0 + 256], writes=[brc])
                W1 = rsb("W1", [128, 8, 896], BF16)
                W2 = rsb("W2", [128, 8, 896], BF16)
                Wg = rsb("Wg", [128, 8, 256], BF16)
                bW = Buf("RW")
                with ExitStack() as ws:
                    alloc_staging(ws)
                    mub = sb("R%d_mub" % half, [128, 896], stack=ws)
                    omub = sb("R%d_omub" % half, [128, 896], stack=ws)
                    bmu = Buf("mu")
                    srcs = [(0 + ch0, 256), (512 + ch0, 256), (1024 + ch0, 256), (1536, 128)]
                    off = 0
                    for (c0, n) in srcs:
                        S.dma("sp", mub[:, off:off + n], mu_row[0:1, c0:c0 + n].partition_broadcast(128), writes=[bmu])
                        off += n
                    S.op("pool", lambda e: e.tensor_scalar(out=omub[:], in0=mub[:], scalar1=-1.0, scalar2=1.0, op0=ALU.mult, op1=ALU.add),
                         reads=[bmu], writes=[bmu])
                    off = 0
                    for (c0, n) in srcs:
                        st, bst = load_w(w_in_v[:, :, c0:c0 + n], n)
                        S.op("pool", lambda e, st=st, n=n: e.tensor_tensor(out=st[:, :, 0:n], in0=st[:, :, 0:n],
                                                                          in1=gpre[:, :, None].to_broadcast([128, 8, n]), op=ALU.mult),
                             reads=[bst, bconst], writes=[bst])
                        S.op("pool", lambda e, st=st, n=n, off=off: e.tensor_tensor(out=W2[:, :, off:off + n], in0=st[:, :, 0:n],
                                                                                   in1=mub[:, None, off:off + n].to_broadcast([128, 8, n]), op=ALU.mult),
                             reads=[bst, bmu], writes=[bW])
                        S.op("pool", lambda e, st=st, n=n, off=off: e.tensor_tensor(out=W1[:, :, off:off + n], in0=st[:, :, 0:n],
                                                                                   in1=omub[:, None, off:off + n].to_broadcast([128, 8, n]), op=ALU.mult),
                             reads=[bst, bmu], writes=[bW])
                        off += n
                    st, bst = load_w(w_in_v[:, :, 3200 + ch0:3200 + ch0 + 256], 256)
                    S.op("pool", lambda e, st=st: e.tensor_tensor(out=Wg[:], in0=st[:, :, 0:256],
                                                                 in1=gpre[:, :, None].to_broadcast([128, 8, 256]), op=ALU.mult),
                         reads=[bst, bconst], writes=[bW])
                    S.barrier()
                NXT = 4
                xT = [rsb("xT%d" % i, [128, 8, 129], BF16) for i in range(NXT)]
                bxT = [Buf("xT%d" % i) for i in range(NXT)]
                TW = rsb("TW", [65, 128]); AD = rsb("AD", [65, 128])
                E3 = rsb("E3", [128, 256]); sq = rsb("sq", [128, 256]); kkn = rsb("kkn", [128, 256])
                kmod = rsb("kmod", [128, 256]); bvec = rsb("bvec", [128, 256]); rkt = rsb("rkt", [128, 256])
                dbl = {}
                for nm_, shp_ in [("rk32", [128, 512]), ("v32", [128, 256]), ("thz", [128, 512]), ("sgi", [128, 512]), ("d3", [128, 256]),
                                  ("E1", [128, 256]), ("E2", [128, 256]), ("E4", [128, 256]), ("kk", [128, 256]), ("km1", [128, 256]),
                                  ("thg", [128, 256]), ("g32", [128, 256]), ("sm", [128, 16])]:
                    dbl[nm_] = [rsb("%s_%d" % (nm_, i_), shp_) for i_ in range(2)]
                bws = [{n: Buf(n) for n in "rk32 v32 thz sgi d3 E1 E2 E4 kk km1 thg g32 sm".split()} for _ in range(2)]
                bw = {n: Buf(n) for n in "TW AD E3 sq kkn kmod bvec rkt".split()}
                S.op("pool", lambda e: e.memset(TW[64:65, :], 1.0), writes=[bw["TW"]])
                S.op("pool", lambda e: e.memset(AD[64:65, :], 1.0), writes=[bw["AD"]])
                TM = [rsb("TM%d" % i, [128, 6, 256], BF16) for i in range(3)]
                Vb = [rsb("Vb%d" % i, [128, 256], BF16) for i in range(3)]
                EC = [rsb("EC%d" % i, [128, 256]) for i in range(3)]
                bonus = [rsb("bonus%d" % i, [128, 256]) for i in range(3)]
                FM = [rsb("FM%d" % i, [128, 4, 4, 64], BF16) for i in range(3)]
                bTM = [Buf() for _ in range(3)]; bVb = [Buf() for _ in range(3)]; bEC = [Buf() for _ in range(3)]
                bbonus = [Buf() for _ in range(3)]; bFM = [Buf() for _ in range(3)]
                SAM = rsb("SAM", [128, 4, 128], BF16); SKM = rsb("SKM", [128, 4, 128], BF16)
                XY = [rsb("XY%d" % i, [128, 2, 4, 64], BF16) for i in range(2)]
                Wt = rsb("Wt", [128, 4, 128], BF16)
                DG = rsb("DG", [128, 4, 64]); MTs = rsb("MTs", [128, 4, 64]); Gs = rsb("Gs", [128, 4, 64])
                RpT = rsb("RpT", [128, 4, 64], BF16)
                Hf = rsb("Hf", [128, 4, 64]); Hb = rsb("Hb", [128, 4, 64], BF16)
                bSAM = [Buf() for _ in range(2)]; bSKM = [Buf() for _ in range(2)]
                bXY = [[Buf() for _ in range(2)] for _ in range(2)]
                bWt = [Buf() for _ in range(2)]; bDG = [Buf() for _ in range(2)]; bMTs = [Buf() for _ in range(2)]
                bGs = [Buf() for _ in range(2)]; bRpT = [Buf() for _ in range(2)]
                bHf = [Buf() for _ in range(2)]; bHb = [Buf() for _ in range(2)]
                S.op("pool", lambda e: e.memset(Hf[:], 0.0), writes=[bHf[0], bHf[1]])
                S.op("pool", lambda e: e.memset(Hb[:], 0.0), writes=[bHb[0], bHb[1]])
                Yt = [rsb("Yt%d" % i, [128, 256]) for i in range(2)]
                bYt = [Buf() for _ in range(2)]
                t1g = [rsb("t1g%d" % i, [128, 256]) for i in range(3)]
                bt1g = [Buf() for _ in range(3)]
                yc = rsb("yc", [128, 256]); ysq = rsb("ysq", [128, 256])
                ygb = rsb("ygb", [128, 256], BF16); pm = rsb("pm", [128, 16])
                bp = {n: Buf(n) for n in "yc ysq ygb pm".split()}

                def tile_RF(tau):
                    kx = tau % NXT
                    if tau == 0:
                        S.op("act", lambda e: e.memset(xT[kx][:, :, 0:1], 0.0), writes=[bxT[kx]]) if False else \
                            S.op("dve", lambda e: e.memset(xT[kx][:, :, 0:1], 0.0), writes=[bxT[kx]])
                    else:
                        kp = (tau - 1) % NXT
                        S.op("act", lambda e: e.copy(out=xT[kx][:, :, 0:1], in_=xT[kp][:, :, 128:129]),
                             reads=[bxT[kp]], writes=[bxT[kx]])
                    frontend(tau, xT[kx][:, :, 1:129], bxT[kx])
                    yield

                def tile_RA(tau):
                    k = tau % NXT
                    k3 = tau % 3
                    ka = tau % 2
                    rk32, v32, thz, sgi, d3, E1, E2, E4, kk, km1, thg, g32, sm = [dbl[n_][ka] for n_ in
                        "rk32 v32 thz sgi d3 E1 E2 E4 kk km1 thg g32 sm".split()]
                    bwd = bws[ka]
                    cur = lambda c: xT[k][:, c, 1:129]
                    prv = lambda c: xT[k][:, c, 0:128]
                    for i, (wt_, xv) in enumerate([(W1, cur), (W2, prv)]):
                        for c in range(8):
                            S.op("pe", lambda e, c=c, wt_=wt_, xv=xv, i=i: e.matmul(PB[1][:, :], lhsT=xv(c), rhs=wt_[:, c, 0:512],
                                                                                 start=(i == 0 and c == 0), stop=(i == 1 and c == 7)),
                                 reads=[bxT[k], bW], writes=[bPB[1]])
                    for i, (wt_, xv) in enumerate([(W1, cur), (W2, prv)]):
                        for c in range(8):
                            S.op("pe", lambda e, c=c, wt_=wt_, xv=xv, i=i: e.matmul(PB[2][:, 0:256], lhsT=xv(c), rhs=wt_[:, c, 512:768],
                                                                                 start=(i == 0 and c == 0), stop=(i == 1 and c == 7)),
                                 reads=[bxT[k], bW], writes=[bPB[2]])
                    for i, (wt_, xv) in enumerate([(W1, cur), (W2, prv)]):
                        for c in range(8):
                            S.op("pe", lambda e, c=c, wt_=wt_, xv=xv, i=i: e.matmul(PB[2][:, 256:384], lhsT=wt_[:, c, 768:896], rhs=xv(c),
                                                                                 start=(i == 0 and c == 0), stop=(i == 1 and c == 7)),
                                 reads=[bxT[k], bW], writes=[bPB[2]])
                    yield
                    S.op("act", lambda e: e.copy(out=rk32[:], in_=PB[1][:, :]), reads=[bPB[1]], writes=[bwd["rk32"]])
                    S.op("act", lambda e: e.copy(out=v32[:], in_=PB[2][:, 0:256]), reads=[bPB[2]], writes=[bwd["v32"]])
                    S.op("act", lambda e: e.copy(out=Vb[k3][:], in_=PB[2][:, 0:256]), reads=[bPB[2]], writes=[bVb[k3]])
                    S.op("act", lambda e: e.activation(out=TW[0:64, :], in_=PB[2][0:64, 256:384], func=AF.Tanh),
                         reads=[bPB[2]], writes=[bw["TW"]])
                    S.op("act", lambda e: e.copy(out=AD[0:64, :], in_=PB[2][64:128, 256:384]), reads=[bPB[2]], writes=[bw["AD"]])
                    yield
                    yield
                    S.op("pe", lambda e: e.matmul(PB[3][:, 0:256], lhsT=TW[:, :], rhs=w2w0h[:, :], start=True, stop=True),
                         reads=[bw["TW"], brc], writes=[bPB[3]])
                    S.op("pe", lambda e: e.matmul(PB[3][:, 256:512], lhsT=AD[:, :], rhs=a2a0h[:, :], start=True, stop=True),
                         reads=[bw["AD"], brc], writes=[bPB[3]])
                    for c in range(8):
                        S.op("pe", lambda e, c=c: e.matmul(PB[1][:, 256:512], lhsT=cur(c), rhs=Wg[:, c, :], start=(c == 0), stop=(c == 7)),
                             reads=[bxT[k], bW], writes=[bPB[1]])
                    S.op("act", lambda e: e.activation(out=thz[:], in_=PB[3][:, :], func=AF.Tanh, scale=0.5), reads=[bPB[3]], writes=[bwd["thz"]])
                    S.op("act", lambda e: e.activation(out=thg[:], in_=PB[1][:, 256:512], func=AF.Tanh, scale=0.5), reads=[bPB[1]], writes=[bwd["thg"]])
                    S.op("act", lambda e: e.copy(out=g32[:], in_=PB[1][:, 256:512]), reads=[bPB[1]], writes=[bwd["g32"]])
                    S.op("pool", lambda e: e.tensor_scalar(out=sgi[:], in0=thz[:], scalar1=0.5, scalar2=0.5, op0=ALU.mult, op1=ALU.add),
                         reads=[bwd["thz"]], writes=[bwd["sgi"]])
                    sg = sgi[:, 0:256]
                    icl = sgi[:, 256:512]
                    r32 = rk32[:, 0:256]
                    k32 = rk32[:, 256:512]
                    v3 = lambda ap: ap.rearrange("p (h c) -> p h c", h=4)
                    S.op("pool", lambda e: e.tensor_scalar(out=km1[:], in0=thz[:, 256:512], scalar1=0.5, scalar2=-0.5, op0=ALU.mult, op1=ALU.add),
                         reads=[bwd["thz"]], writes=[bwd["km1"]])
                    S.op("pool", lambda e: e.tensor_tensor(out=kk[:], in0=k32, in1=pbc[:, 0, :], op=ALU.mult), reads=[bwd["rk32"], brc], writes=[bwd["kk"]])
                    yield
                    yield
                    S.op("pe", lambda e: e.matmul(PB[3][:, 0:256], lhsT=tri[:, 0, :], rhs=sg, start=True, stop=True),
                         reads=[bwd["sgi"], brc], writes=[bPB[3]])
                    S.op("pe", lambda e: e.matmul(PB[3][:, 256:512], lhsT=tri[:, 1, :], rhs=sg, start=True, stop=True),
                         reads=[bwd["sgi"], brc], writes=[bPB[3]])
                    S.op("pe", lambda e: e.matmul(PB[1][:, 0:256], lhsT=tri[:, 2, :], rhs=sg, start=True, stop=True),
                         reads=[bwd["sgi"], brc], writes=[bPB[1]])
                    S.op("act", lambda e: e.activation(out=E1[:], in_=PB[3][:, 0:256], func=AF.Exp), reads=[bPB[3]], writes=[bwd["E1"]])
                    S.op("act", lambda e: e.activation(out=E2[:], in_=PB[3][:, 0:256], func=AF.Exp, scale=-1.0), reads=[bPB[3]], writes=[bwd["E2"]])
                    S.op("act", lambda e: e.activation(out=E4[:], in_=PB[3][:, 256:512], func=AF.Exp), reads=[bPB[3]], writes=[bwd["E4"]])
                    S.op("act", lambda e: e.activation(out=EC[k3][:], in_=PB[1][:, 0:256], func=AF.Exp), reads=[bPB[1]], writes=[bEC[k3]])
                    S.op("act", lambda e: e.activation(out=d3[:], in_=sg, func=AF.Exp, scale=C0), reads=[bwd["sgi"]], writes=[bwd["d3"]])
                    for hl in range(4):
                        S.op("act", lambda e, hl=hl: e.activation(out=sq[:, hl * 64:(hl + 1) * 64], in_=kk[:, hl * 64:(hl + 1) * 64], func=AF.Square,
                                                                 accum_out=sm[:, hl:hl + 1]), reads=[bwd["kk"]], writes=[bw["sq"], bwd["sm"]])
                    yield
                def tile_RB(tau):
                    k = tau % 2
                    k3 = tau % 3
                    ka = tau % 2
                    rk32, v32, thz, sgi, d3, E1, E2, E4, kk, km1, thg, g32, sm = [dbl[n_][ka] for n_ in
                        "rk32 v32 thz sgi d3 E1 E2 E4 kk km1 thg g32 sm".split()]
                    bwd = bws[ka]
                    sg = sgi[:, 0:256]
                    icl = sgi[:, 256:512]
                    r32 = rk32[:, 0:256]
                    k32 = rk32[:, 256:512]
                    v3 = lambda ap: ap.rearrange("p (h c) -> p h c", h=4)
                    S.op("pool", lambda e: e.tensor_scalar(out=t1g[k3][:], in0=thg[:], scalar1=0.5, scalar2=0.5, op0=ALU.mult, op1=ALU.add),
                         reads=[bwd["thg"]], writes=[bt1g[k3]])
                    S.op("pool", lambda e: e.tensor_tensor(out=t1g[k3][:], in0=t1g[k3][:], in1=g32[:], op=ALU.mult), reads=[bt1g[k3], bwd["g32"]], writes=[bt1g[k3]])
                    S.op("pool", lambda e: e.tensor_tensor(out=E3[:], in0=E1[:], in1=d3[:], op=ALU.mult), reads=[bwd["E1"], bwd["d3"]], writes=[bw["E3"]])
                    S.op("pool", lambda e: e.tensor_scalar(out=sm[:, 4:8], in0=sm[:, 0:4], scalar1=1e-24, scalar2=None, op0=ALU.max),
                         reads=[bwd["sm"]], writes=[bwd["sm"]])
                    S.op("pool", lambda e: e.tensor_tensor(out=sm[:, 8:12], in0=sm[:, 4:8], in1=neghalf[:, 0:4], op=ALU.pow),
                         reads=[bwd["sm"], bconst], writes=[bwd["sm"]])
                    S.op("pool", lambda e: e.tensor_tensor(out=v3(kkn[:]), in0=v3(kk[:]), in1=sm[:, 8:12, None].to_broadcast([128, 4, 64]), op=ALU.mult),
                         reads=[bwd["kk"], bwd["sm"]], writes=[bw["kkn"]])
                    yield
                    S.op("pool", lambda e: e.tensor_tensor(out=km1[:], in0=km1[:], in1=pbc[:, 1, :], op=ALU.mult), reads=[bwd["km1"], brc], writes=[bwd["km1"]])
                    S.op("pool", lambda e: e.tensor_tensor(out=kmod[:], in0=km1[:], in1=k32, op=ALU.mult), reads=[bwd["km1"], bwd["rk32"]], writes=[bw["kmod"]])
                    S.op("pool", lambda e: e.tensor_tensor(out=kmod[:], in0=kmod[:], in1=k32, op=ALU.add), reads=[bw["kmod"], bwd["rk32"]], writes=[bw["kmod"]])
                    S.op("pool", lambda e: e.tensor_tensor(out=bvec[:], in0=kkn[:], in1=icl, op=ALU.mult), reads=[bw["kkn"], bwd["sgi"]], writes=[bw["bvec"]])
                    yield
                    S.op("pool", lambda e: e.tensor_tensor(out=TM[k3][:, 0, :], in0=kkn[:], in1=E3[:], op=ALU.mult), reads=[bw["kkn"], bw["E3"]], writes=[bTM[k3]])
                    S.op("pool", lambda e: e.tensor_tensor(out=TM[k3][:, 1, :], in0=r32, in1=E1[:], op=ALU.mult), reads=[bwd["rk32"], bwd["E1"]], writes=[bTM[k3]])
                    S.op("pool", lambda e: e.tensor_tensor(out=TM[k3][:, 2, :], in0=bvec[:], in1=E2[:], op=ALU.mult), reads=[bw["bvec"], bwd["E2"]], writes=[bTM[k3]])
                    S.op("pool", lambda e: e.tensor_tensor(out=TM[k3][:, 3, :], in0=kmod[:], in1=E2[:], op=ALU.mult), reads=[bw["kmod"], bwd["E2"]], writes=[bTM[k3]])
                    yield
                    S.op("pool", lambda e: e.tensor_tensor(out=TM[k3][:, 4, :], in0=bvec[:], in1=E4[:], op=ALU.mult), reads=[bw["bvec"], bwd["E4"]], writes=[bTM[k3]])
                    S.op("pool", lambda e: e.tensor_tensor(out=TM[k3][:, 5, :], in0=kmod[:], in1=E4[:], op=ALU.mult), reads=[bw["kmod"], bwd["E4"]], writes=[bTM[k3]])
                    S.op("pool", lambda e: e.tensor_tensor(out=rkt[:], in0=r32, in1=kmod[:], op=ALU.mult), reads=[bwd["rk32"], bw["kmod"]], writes=[bw["rkt"]])
                    S.op("pool", lambda e: e.tensor_tensor(out=rkt[:], in0=rkt[:], in1=pbc[:, 2, :], op=ALU.mult), reads=[bw["rkt"], brc], writes=[bw["rkt"]])
                    for hl in range(4):
                        S.op("act", lambda e, hl=hl: e.activation(out=sq[:, hl * 64:(hl + 1) * 64], in_=rkt[:, hl * 64:(hl + 1) * 64], func=AF.Copy,
                                                                 accum_out=sm[:, 12 + hl:13 + hl]), reads=[bw["rkt"]], writes=[bw["sq"], bwd["sm"]])
                    yield
                    yield
                    for hp in range(2):
                        fbank = (0, 3)[hp]
                        pf = pb_bf(fbank).rearrange("p (h q t) -> p h q t", h=2, q=4)
                        for hh in range(2):
                            hl = 2 * hp + hh
                            for q in range(4):
                                S.op("pe", lambda e, hh=hh, hl=hl, q=q, pf=pf: e.transpose(out=pf[0:64, hh, q, :], in_=TM[k3][:, q, hl * 64:(hl + 1) * 64],
                                                                                         identity=identb[:]),
                                     reads=[bTM[k3], bconst], writes=[bPB[fbank]])
                        S.op("act", lambda e, hp=hp, pf=pf: e.copy(out=FM[k3][0:64, 2 * hp:2 * hp + 2, :, :], in_=pf[0:64, :, :, 0:64]),
                             reads=[bPB[fbank]], writes=[bFM[k3]])
                        S.op("act", lambda e, hp=hp, pf=pf: e.copy(out=FM[k3][64:128, 2 * hp:2 * hp + 2, :, :], in_=pf[0:64, :, :, 64:128]),
                             reads=[bPB[fbank]], writes=[bFM[k3]])
                        yield
                    S.op("pool", lambda e: e.tensor_tensor(out=v3(bonus[k3][:]), in0=v3(v32[:]), in1=sm[:, 12:16, None].to_broadcast([128, 4, 64]), op=ALU.mult),
                         reads=[bwd["v32"], bwd["sm"]], writes=[bbonus[k3]])

                def chunk_R(tau, j):
                    k = tau % 2
                    k3 = tau % 3
                    lo = 64 * j
                    L = slice(lo, lo + 64)
                    Ba, Bb = 4 + 2 * j, 5 + 2 * j
                    bBa, bBb = bPB[Ba], bPB[Bb]
                    pa3 = PB[Ba][0:64, :].rearrange("p (h c) -> p h c", h=4)
                    pb3 = PB[Bb][0:64, :].rearrange("p (h c) -> p h c", h=4)
                    pa4 = PB[Ba][0:64, :].rearrange("p (a h c) -> p a h c", a=2, h=4)
                    pb4 = PB[Bb][0:64, :].rearrange("p (a h c) -> p a h c", a=2, h=4)
                    fm = FM[k3]
                    for hl in range(4):
                        S.op("pe", lambda e, hl=hl: e.matmul(pa3[:, hl, :].rearrange("p (a b) -> p a b", a=2), lhsT=fm[L, hl, 2, :], rhs=fm[L, hl, 0:2, :],
                                                             start=True, stop=True), reads=[bFM[k3]], writes=[bBa])
                    for hl in range(4):
                        S.op("pe", lambda e, hl=hl: e.matmul(pb3[:, hl, :].rearrange("p (a b) -> p a b", a=2), lhsT=fm[L, hl, 3, :], rhs=fm[L, hl, 0:2, :],
                                                             start=True, stop=True), reads=[bFM[k3]], writes=[bBb])
                    S.op("dve", lambda e: e.tensor_tensor(out=SAM[L, :, :], in0=pa3, in1=maskAM[L, None, :].to_broadcast([64, 4, 128]), op=ALU.mult),
                         reads=[bBa, brc], writes=[bSAM[j]])
                    S.op("dve", lambda e: e.tensor_tensor(out=SKM[L, :, :], in0=pb3, in1=maskAM[L, None, :].to_broadcast([64, 4, 128]), op=ALU.mult),
                         reads=[bBb, brc], writes=[bSKM[j]])
                    yield
                    for hl in range(4):
                        S.op("pe", lambda e, hl=hl: e.matmul(pa4[:, 0, hl, :], lhsT=fm[L, hl, 0, :], rhs=fm[L, hl, 2, :], start=True, stop=True),
                             reads=[bFM[k3]], writes=[bBa])
                    for hl in range(4):
                        S.op("pe", lambda e, hl=hl: e.matmul(pa4[:, 1, hl, :], lhsT=SKM[L, hl, 0:64], rhs=Vb[k3][L, hl * 64:(hl + 1) * 64], start=True, stop=True),
                             reads=[bSKM[j], bVb[k3]], writes=[bBa])
                    xy0 = XY[0]
                    S.op("dve", lambda e: e.tensor_tensor(out=xy0[L, 0, :, :], in0=pa4[:, 0, :, :], in1=maskX[L, None, :].to_broadcast([64, 4, 64]), op=ALU.mult),
                         reads=[bBa, brc], writes=[bXY[0][j]])
                    S.op("dve", lambda e: e.tensor_copy(out=xy0[L, 1, :, :], in_=SAM[L, :, 0:64]), reads=[bSAM[j]], writes=[bXY[0][j]])
                    S.op("dve", lambda e: e.tensor_copy(out=Wt[L, :, 64:128], in_=pa4[:, 1, :, :]), reads=[bBa], writes=[bWt[j]])
                    S.op("dve", lambda e: e.tensor_scalar(out=Wt[L, :, 0:64], in0=TM[k3][L, 0, :].rearrange("p (h c) -> p h c", h=4), scalar1=-1.0, scalar2=None, op0=ALU.mult),
                         reads=[bTM[k3]], writes=[bWt[j]])
                    yield
                    for kx in range(6):
                        cur_, nxt_ = XY[kx % 2], XY[(kx + 1) % 2]
                        bcur, bnxt = bXY[kx % 2][j], bXY[(kx + 1) % 2][j]
                        if kx < 5:
                            for hl in range(4):
                                S.op("pe", lambda e, hl=hl, cur_=cur_: e.matmul(pb4[:, 0, hl, :], lhsT=cur_[L, 1, hl, :], rhs=cur_[L, 0, hl, :], start=True, stop=True),
                                     reads=[bcur], writes=[bBb])
                                S.op("pe", lambda e, hl=hl, cur_=cur_: e.matmul(pb4[:, 1, hl, :], lhsT=cur_[L, 0, hl, :], rhs=cur_[L, 1, hl, :], start=True, stop=True),
                                     reads=[bcur], writes=[bBb])
                        for hl in range(4):
                            S.op("pe", lambda e, hl=hl, cur_=cur_: e.matmul(pa3[:, hl, :], lhsT=cur_[L, 1, hl, :], rhs=Wt[L, hl, :], start=True, stop=True),
                                 reads=[bcur, bWt[j]], writes=[bBa])
                        if kx < 5:
                            S.op("dve", lambda e, nxt_=nxt_: e.tensor_copy(out=nxt_[L, :, :, :], in_=pb4), reads=[bBb], writes=[bnxt])
                        S.op("dve", lambda e: e.tensor_tensor(out=Wt[L, :, :], in0=pa3, in1=Wt[L, :, :], op=ALU.add), reads=[bBa, bWt[j]], writes=[bWt[j]])
                        yield
                    tm = TM[k3]
                    hc = lambda hl: slice(hl * 64, (hl + 1) * 64)
                    for hl in range(4):
                        S.op("pe", lambda e, hl=hl: e.matmul(pb4[:, 0, hl, :], lhsT=Wt[L, hl, 0:64], rhs=tm[L, 4, hc(hl)], start=True, stop=True),
                             reads=[bWt[j], bTM[k3]], writes=[bBb])
                    for hl in range(4):
                        S.op("pe", lambda e, hl=hl: e.matmul(pb4[:, 1, hl, :], lhsT=tm[L, 4, hc(hl)], rhs=Wt[L, hl, 64:128], start=True, stop=False),
                             reads=[bWt[j], bTM[k3]], writes=[bBb])
                        S.op("pe", lambda e, hl=hl: e.matmul(pb4[:, 1, hl, :], lhsT=tm[L, 5, hc(hl)], rhs=Vb[k3][L, hc(hl)], start=False, stop=True),
                             reads=[bVb[k3], bTM[k3]], writes=[bBb])
                    for hl in range(4):
                        S.op("pe", lambda e, hl=hl: e.matmul(pa4[:, 0, hl, :], lhsT=Wt[L, hl, 0:64], rhs=SAM[L, hl, 64:128], start=True, stop=False),
                             reads=[bWt[j], bSAM[j]], writes=[bBa])
                        S.op("pe", lambda e, hl=hl: e.matmul(pa4[:, 0, hl, :], lhsT=tm[L, 1, hc(hl)], rhs=identb[L, L], start=False, stop=True),
                             reads=[bTM[k3], bconst], writes=[bBa])
                    S.op("dve", lambda e: e.tensor_tensor(out=DG[L, :, :], in0=EC[k3][L, :].rearrange("p (h c) -> p h c", h=4),
                                                          in1=identf[L, None, lo:lo + 64].to_broadcast([64, 4, 64]), op=ALU.mult),
                         reads=[bEC[k3], bconst], writes=[bDG[j]])
                    S.op("dve", lambda e: e.tensor_tensor(out=MTs[L, :, :], in0=pb4[:, 0, :, :], in1=DG[L, :, :], op=ALU.add),
                         reads=[bBb, bDG[j]], writes=[bMTs[j]])
                    S.op("dve", lambda e: e.tensor_copy(out=Gs[L, :, :], in_=pb4[:, 1, :, :]), reads=[bBb], writes=[bGs[j]])
                    S.op("dve", lambda e: e.tensor_copy(out=RpT[L, :, :], in_=pa4[:, 0, :, :]), reads=[bBa], writes=[bRpT[j]])
                    yield
                    for hl in range(4):
                        S.op("pe", lambda e, hl=hl: e.matmul(pa4[:, 1, hl, :], lhsT=SAM[L, hl, 64:128], rhs=Wt[L, hl, 64:128], start=True, stop=False),
                             reads=[bWt[j], bSAM[j]], writes=[bBa])
                        S.op("pe", lambda e, hl=hl: e.matmul(pa4[:, 1, hl, :], lhsT=SKM[L, hl, 64:128], rhs=Vb[k3][L, hc(hl)], start=False, stop=False),
                             reads=[bSKM[j], bVb[k3]], writes=[bBa])
                        S.op("pe", lambda e, hl=hl: e.matmul(pa4[:, 1, hl, :], lhsT=RpT[L, hl, :], rhs=Hb[L, hl, :], start=False, stop=True),
                             reads=[bRpT[j], bHb[j]], writes=[bBa])
                    for hl in range(4):
                        S.op("pe", lambda e, hl=hl: e.matmul(pb4[:, 0, hl, :], lhsT=MTs[L, hl, :], rhs=Hf[L, hl, :], start=True, stop=True),
                             reads=[bMTs[j], bHf[j]], writes=[bBb])
                    S.op("dve", lambda e: e.tensor_copy(out=Yt[k][L, :].rearrange("p (h c) -> p h c", h=4), in_=pa4[:, 1, :, :]), reads=[bBa], writes=[bYt[k]])
                    Lo = slice(64 * (1 - j), 64 * (1 - j) + 64)
                    S.op("dve", lambda e: e.tensor_tensor(out=Hf[Lo, :, :], in0=pb4[:, 0, :, :], in1=Gs[L, :, :], op=ALU.add),
                         reads=[bBb, bGs[j]], writes=[bHf[1 - j]])
                    S.op("dve", lambda e: e.tensor_copy(out=Hb[Lo, :, :], in_=Hf[Lo, :, :]), reads=[bHf[1 - j]], writes=[bHb[1 - j]])
                    yield

                def post_R(tau):
                    k = tau % 2
                    k3 = tau % 3
                    v3 = lambda ap: ap.rearrange("p (h c) -> p h c", h=4)
                    yt = Yt[k]
                    for hl in range(4):
                        S.op("act", lambda e, hl=hl: e.activation(out=ysq[:, hl * 64:(hl + 1) * 64], in_=yt[:, hl * 64:(hl + 1) * 64], func=AF.Copy,
                                                                 accum_out=pm[:, hl:hl + 1]), reads=[bYt[k]], writes=[bp["ysq"], bp["pm"]])
                    S.op("pool", lambda e: e.tensor_scalar(out=pm[:, 4:8], in0=pm[:, 0:4], scalar1=1.0 / 64, scalar2=None, op0=ALU.mult),
                         reads=[bp["pm"]], writes=[bp["pm"]])
                    yield
                    S.op("pool", lambda e: e.tensor_tensor(out=v3(yc[:]), in0=v3(yt[:]), in1=pm[:, 4:8, None].to_broadcast([128, 4, 64]), op=ALU.subtract),
                         reads=[bYt[k], bp["pm"]], writes=[bp["yc"]])
                    yield
                    for hl in range(4):
                        S.op("act", lambda e, hl=hl: e.activation(out=ysq[:, hl * 64:(hl + 1) * 64], in_=yc[:, hl * 64:(hl + 1) * 64], func=AF.Square,
                                                                 accum_out=pm[:, 8 + hl:9 + hl]), reads=[bp["yc"]], writes=[bp["ysq"], bp["pm"]])
                    S.op("pool", lambda e: e.tensor_scalar(out=pm[:, 8:12], in0=pm[:, 8:12], scalar1=1.0 / 64, scalar2=GN_EPS, op0=ALU.mult, op1=ALU.add),
                         reads=[bp["pm"]], writes=[bp["pm"]])
                    yield
                    S.op("pool", lambda e: e.tensor_tensor(out=pm[:, 12:16], in0=pm[:, 8:12], in1=neghalf[:, 0:4], op=ALU.pow),
                         reads=[bp["pm"], bconst], writes=[bp["pm"]])
                    S.op("pool", lambda e: e.tensor_tensor(out=v3(yc[:]), in0=v3(yc[:]), in1=pm[:, 12:16, None].to_broadcast([128, 4, 64]), op=ALU.mult),
                         reads=[bp["yc"], bp["pm"]], writes=[bp["yc"]])
                    S.op("pool", lambda e: e.tensor_tensor(out=yc[:], in0=yc[:], in1=pbc[:, 3, :], op=ALU.mult), reads=[bp["yc"], brc], writes=[bp["yc"]])
                    S.op("pool", lambda e: e.tensor_tensor(out=yc[:], in0=yc[:], in1=pbc[:, 4, :], op=ALU.add), reads=[bp["yc"], brc], writes=[bp["yc"]])
                    S.op("pool", lambda e: e.tensor_tensor(out=yc[:], in0=yc[:], in1=bonus[k3][:], op=ALU.add), reads=[bp["yc"], bbonus[k3]], writes=[bp["yc"]])
                    S.op("pool", lambda e: e.tensor_tensor(out=ygb[:], in0=yc[:], in1=t1g[k3][:], op=ALU.mult), reads=[bp["yc"], bt1g[k3]], writes=[bp["ygb"]])
                    yield
                    yield
                    yield
                    yield
                    pyt = pb_bf(3)[:, 0:256].rearrange("p (a t) -> p a t", a=2)
                    for a_ in range(2):
                        S.op("pe", lambda e, a_=a_: e.transpose(out=pyt[:, a_, :], in_=ygb[:, a_ * 128:(a_ + 1) * 128], identity=identb[:]),
                             reads=[bp["ygb"], bconst], writes=[bPB[3]])
                    S.op("act", lambda e: e.copy(out=YgT[:, 2 * half:2 * half + 2, tau * 128:(tau + 1) * 128], in_=pyt),
                         reads=[bPB[3]], writes=[bYg[2 * half], bYg[2 * half + 1]])
                    yield

                def backend_R(tau):
                    g0 = chunk_R(tau, 0)
                    g1 = chunk_R(tau, 1)
                    for st_ in range(9):
                        next(g0)
                        next(g1)
                        yield
                    next(g0)
                    yield
                    next(g1)
                    yield

                def run_streams(streams):
                    live = list(streams)
                    while live:
                        for g in list(live):
                            try:
                                next(g)
                            except StopIteration:
                                live.remove(g)

                ntr = dbg.get('nt_R', {}).get(half, nt_lim)
                a_gen = None; a_next = 0; a_done = 0
                p_gen = None; p_next = 0; p_done = 0
                b_gen = None; b_tile = 0
                f_gen = None; f_next = 0; f_done = 0
                q_gen = None; q_next = 0; q_done = 0
                while q_done < ntr:
                    if f_gen is None and f_next < ntr and f_next <= a_next + dbg.get('aheadF', 2):
                        f_gen = tile_RF(f_next)
                    if f_gen is not None:
                        try:
                            next(f_gen)
                        except StopIteration:
                            f_gen = None
                            f_next += 1
                            f_done = f_next
                    if a_gen is None and a_next < ntr and f_done > a_next and a_next <= p_done + dbg.get('aheadA', 1) and a_next <= b_tile + 2:
                        a_gen = tile_RA(a_next)
                    if a_gen is not None:
                        try:
                            next(a_gen)
                        except StopIteration:
                            a_gen = None
                            a_next += 1
                            a_done = a_next
                    if p_gen is None and p_next < ntr and a_done > p_next and p_next <= q_next + dbg.get('aheadB', 2):
                        p_gen = tile_RB(p_next)
                    if p_gen is not None:
                        try:
                            next(p_gen)
                        except StopIteration:
                            p_gen = None
                            p_next += 1
                            p_done = p_next
                    if b_gen is None and b_tile < ntr and p_done > b_tile and q_done > b_tile - 2:
                        b_gen = backend_R(b_tile)
                    if b_gen is not None:
                        try:
                            next(b_gen)
                        except StopIteration:
                            b_gen = None
                            b_tile += 1
                    if q_gen is None and q_next < ntr and b_tile > q_next:
                        q_gen = post_R(q_next)
                    if q_gen is not None:
                        try:
                            next(q_gen)
                        except StopIteration:
                            q_gen = None
                            q_next += 1
                            q_done = q_next
                S.barrier()

        for half_ in do_R:
            phase_R(half_)

        def phase_M(pair):
            with ExitStack() as ms:
                def msb(name, shape, dt=F32):
                    return sb("M%d_%s" % (pair, name), shape, dt, stack=ms)

                KA = [msb("KA%d" % i, [82, T], BF16) for i in range(2)]
                QA = [msb("QA%d" % i, [82, T], BF16) for i in range(2)]
                Vaug = msb("Vaug", [128, NT, 2, 65], BF16)
                SG = msb("SG", [128, T], BF16)
                GT = msb("GT", [128, T], BF16)
                Yall = msb("Yall", [128, NT, 128], BF16)
                Wp = msb("Wp", [128, 8, 512], BF16)
                xTb = [msb("xTb%d" % i, [128, 8, 512], BF16) for i in range(2)]
                qT32 = msb("qT32", [128, 512])
                kmBD = msb("kmBD", [128, 32])
                pastb = msb("pastb", [128, 16, 16])
                ownb = msb("ownb", [128, 16, 16])
                causal = msb("causal", [128, 2, 256], BF16)
                gm = msb("gm", [128, 4, 16]); m8 = msb("m8", [128, 4, 8]); lt = msb("lt", [128, 4, 16])
                mbts = [msb("mbt%d" % i, [128, 4, 2, 32], BF16) for i in range(2)]
                bmbts = [Buf() for _ in range(2)]
                PT = [msb("PT%d" % i, [128, 2, 256], BF16) for i in range(3)]
                rec = msb("rec", [128, 4])
                for i_ in (2, 3):
                    xt.append(msb("xt%d" % i_, [128, D])); bxt.append(Buf())
                    xnb.append(msb("xnb%d" % i_, [128, D], BF16)); bxnb.append(Buf())
                    fes.append(msb("fes%d" % i_, [128, 4])); bfes.append(Buf())
                fe_nbuf[0] = 4
                bKA = [Buf() for _ in range(2)]; bQA = [Buf() for _ in range(2)]
                bVaug = Buf(); bSG = Buf(); bYall = Buf(); bWp = Buf()
                bxTb = [Buf() for _ in range(2)]; bq32 = Buf(); bkm = Buf(); bmc = Buf()
                bgm = Buf(); bm8 = Buf(); blt = Buf()
                bPT = [Buf() for _ in range(3)]; brec = Buf()
                S.dma("sp", pastb[:].rearrange("p a b -> p (a b)"), dr["c_past"][0:1, :].partition_broadcast(128), writes=[bmc])
                S.dma("sp", ownb[:].rearrange("p a b -> p (a b)"), dr["c_own"][0:1, :].partition_broadcast(128), writes=[bmc])
                S.dma("sp", causal[:], dr["c_causal"][:], writes=[bmc])
                S.op("pool", lambda e: e.memset(kmBD[:], 0.0), writes=[bkm])
                for i_ in range(2):
                    S.op("pool", lambda e, i_=i_: e.memset(mbts[i_][:], 0.0), writes=[bmbts[i_]])
                S.op("pool", lambda e: e.memset(Vaug[:, :, :, 64:65], 1.0), writes=[bVaug])
                for hq in range(2):
                    h = 2 * pair + hq
                    S.dma("sp", KA[hq][64:80, :], dr["c_onehot"][:], writes=[bKA[hq]])
                    S.dma("sp", KA[hq][80:82, :], dr["c_krows"][h], writes=[bKA[hq]])
                    S.dma("sp", QA[hq][80:82, :], dr["c_qrows"][h], writes=[bQA[hq]])
                cols = [1664 + 128 * pair, 2176 + 128 * pair, 2688 + 128 * pair, 3712 + 128 * pair]
                with ExitStack() as wsm:
                    alloc_staging(wsm)
                    for i, c0 in enumerate(cols):
                        st, bst = load_w(w_in_v[:, :, c0:c0 + 128], 128)
                        S.op("pool", lambda e, st=st, i=i: e.tensor_tensor(out=Wp[:, :, 128 * i:128 * i + 128], in0=st[:, :, 0:128],
                                                                          in1=gpre[:, :, None].to_broadcast([128, 8, 128]), op=ALU.mult),
                             reads=[bst, bconst], writes=[bWp])
                    S.barrier()

                nblk = nt_lim // 4 if nt_lim >= 4 else 1
                def m_front(tb):
                    kb = tb % 2
                    xb = xTb[kb]
                    for jj in range(4):
                        tile_ = 4 * tb + jj
                        if tile_ == 0:
                            fe1(0)
                            if 4 * nblk > 1:
                                fe1(1)
                        if tile_ + 2 < 4 * nblk:
                            fe1(tile_ + 2)
                        fe2(tile_, xb[:, :, 128 * jj:128 * jj + 128], bxTb[kb], evac_eng="dve")

                def m_block(tb):
                    kb = tb % 2
                    xb = xTb[kb]
                    mbt = mbts[tb % 2]
                    bmbt = bmbts[tb % 2]
                    tsl = slice(512 * tb, 512 * tb + 512)
                    for (bank, wi) in ((1, 1), (2, 0), (4, 3)):
                        for c in range(8):
                            S.op("pe", lambda e, c=c, bank=bank, wi=wi: e.matmul(PB[bank][:, :], lhsT=Wp[:, c, 128 * wi:128 * wi + 128], rhs=xb[:, c, :],
                                                                               start=(c == 0), stop=(c == 7)),
                                 reads=[bWp, bxTb[kb]], writes=[bPB[bank]])
                    pv = PB[3][:, :].rearrange("p (a c) -> p a c", a=4)
                    for jj in range(4):
                        for c in range(8):
                            S.op("pe", lambda e, c=c, jj=jj: e.matmul(pv[:, jj, :], lhsT=xb[:, c, 128 * jj:128 * jj + 128], rhs=Wp[:, c, 256:384],
                                                                     start=(c == 0), stop=(c == 7)),
                                 reads=[bWp, bxTb[kb]], writes=[bPB[3]])
                    S.op("act", lambda e: e.copy(out=KA[0][0:64, tsl], in_=PB[1][0:64, :]), reads=[bPB[1]], writes=[bKA[0]])
                    S.op("act", lambda e: e.copy(out=KA[1][0:64, tsl], in_=PB[1][64:128, :]), reads=[bPB[1]], writes=[bKA[1]])
                    for hq in range(2):
                        for bb in range(2):
                            blk = 2 * tb + bb
                            S.op("dve", lambda e, hq=hq, bb=bb, blk=blk: e.tensor_reduce(out=kmBD[64 * hq:64 * hq + 64, 16 * hq + blk:16 * hq + blk + 1],
                                                                                        in_=PB[1][64 * hq:64 * hq + 64, 256 * bb:256 * bb + 256], axis=AX.X, op=ALU.add),
                                 reads=[bPB[1]], writes=[bkm])
                    S.op("act", lambda e: e.mul(out=QA[0][0:64, tsl], in_=PB[2][0:64, :], mul=0.125), reads=[bPB[2]], writes=[bQA[0]])
                    S.op("act", lambda e: e.mul(out=QA[1][0:64, tsl], in_=PB[2][64:128, :], mul=0.125), reads=[bPB[2]], writes=[bQA[1]])
                    S.op("dve", lambda e: e.tensor_copy(out=qT32[:], in_=PB[2][:, :]), reads=[bPB[2]], writes=[bq32])
                    S.op("act", lambda e: e.activation(out=SG[:, tsl], in_=PB[4][:, :], func=AF.Tanh, scale=0.5), reads=[bPB[4]], writes=[bSG])
                    S.op("dve", lambda e: e.tensor_copy(out=GT[:, tsl], in_=PB[4][:, :]), reads=[bPB[4]], writes=[bSG])
                    S.op("act", lambda e: e.copy(out=Vaug[:, 4 * tb:4 * tb + 4, :, 0:64], in_=PB[3][:, :].rearrange("p (a h c) -> p a h c", a=4, h=2)),
                         reads=[bPB[3]], writes=[bVaug])
                    pg = PB[5][:, 0:128].rearrange("p (a c) -> p a c", a=4)
                    for jj in range(4):
                        S.op("pe", lambda e, jj=jj: e.matmul(pg[:, jj, :], lhsT=qT32[:, 128 * jj:128 * jj + 128], rhs=kmBD[:, :], start=True, stop=True),
                             reads=[bq32, bkm], writes=[bPB[5]])
                    for bb in range(2):
                        blk = 2 * tb + bb
                        pg2 = PB[5][:, 64 * bb:64 * bb + 64].rearrange("p (a c) -> p a c", a=4)
                        S.op("dve", lambda e, pg2=pg2, blk=blk: e.tensor_tensor(out=gm[:], in0=pg2, in1=pastb[:, blk:blk + 1, :].to_broadcast([128, 4, 16]), op=ALU.add),
                             reads=[bPB[5], bmc], writes=[bgm])
                        for g in range(4):
                            S.op("dve", lambda e, g=g: e.max(out=m8[:, g, :], in_=gm[:, g, :]), reads=[bgm], writes=[bm8])
                        S.op("dve", lambda e: e.tensor_tensor(out=lt[:], in0=gm[:], in1=m8[:, :, 2:3].to_broadcast([128, 4, 16]), op=ALU.is_lt),
                             reads=[bgm, bm8], writes=[blt])
                        S.op("dve", lambda e, bb=bb, blk=blk: e.scalar_tensor_tensor(out=mbt[:, 2 * bb:2 * bb + 2, :, 0:16].rearrange("p a h c -> p (a h) c"),
                                                                                   in0=lt[:], scalar=-BIG,
                                                                                   in1=ownb[:, blk:blk + 1, :].to_broadcast([128, 4, 16]),
                                                                                   op0=ALU.mult, op1=ALU.max),
                             reads=[blt, bmc], writes=[bmbt])

                def m_block_b(tb):
                    tsl = slice(512 * tb, 512 * tb + 512)
                    mbt = mbts[tb % 2]
                    bmbt = bmbts[tb % 2]
                    pmt = pb_bf(6)[0:64, 0:512].rearrange("p (a t) -> p a t", a=4)
                    for jj in range(4):
                        S.op("pe", lambda e, jj=jj: e.transpose(out=pmt[:, jj, :], in_=mbt[:, jj, :, :].rearrange("p h c -> p (h c)"), identity=identb[:]),
                             reads=[bmbt, bconst], writes=[bPB[6]])
                    for hq in range(2):
                        S.op("act", lambda e, hq=hq: e.copy(out=QA[hq][64:80, tsl], in_=pb_bf(6)[32 * hq:32 * hq + 16, 0:512]),
                             reads=[bPB[6]], writes=[bQA[hq]])
                m_front(0)
                for tb in range(nblk):
                    if tb + 1 < nblk:
                        m_front(tb + 1)
                    m_block(tb)
                    if tb >= 1:
                        m_block_b(tb - 1)
                m_block_b(nblk - 1)
                S.barrier()
                sbanks = [1, 2, 3, 4]
                obanks = [5, 6]
                items = [(hq, i) for hq in range(2) for i in range(nblk * 2)]
                sctr = [0]

                def qk_stage(hq, i):
                    h = 2 * pair + hq
                    slope = 2.0 ** (-(h + 1))
                    res = []
                    for n in range(i + 1):
                        bank = sbanks[sctr[0] % 4]
                        pi = sctr[0] % 3
                        sctr[0] += 1
                        ps = PB[bank][:, :].rearrange("p (a t) -> p a t", a=2)
                        for sc in range(2):
                            s0 = 256 * n + 128 * sc
                            S.op("pe", lambda e, sc=sc, s0=s0, ps=ps, n=n: e.matmul(ps[:, sc, :], lhsT=KA[hq][0:82, s0:s0 + 128], rhs=QA[hq][0:82, 256 * i:256 * i + 256],
                                                                             start=True, stop=(n != i)),
                                 reads=[bKA[hq], bQA[hq]], writes=[bPB[bank]])
                            if n == i:
                                S.op("pe", lambda e, sc=sc, ps=ps: e.matmul(ps[:, sc, :], lhsT=identb[:, :], rhs=causal[:, sc, :], start=False, stop=True),
                                     reads=[bconst, bmc], writes=[bPB[bank]])
                        S.op("act", lambda e, ps=ps, pi=pi, n=n: e.activation(out=PT[pi][:], in_=ps, func=AF.Exp, bias=float(-slope * 256.0 * (i - n)), scale=1.0),
                             reads=[bPB[bank]], writes=[bPT[pi]])
                        res.append((pi, n))
                        yield (pi, n)

                LOOK = dbg.get("look", 2)

                def pv_stage(hq, i, it, pi, n):
                    ob_idx = ((5, 6), (7, 0))[it % 2]
                    ob = PB[ob_idx[0]], PB[ob_idx[1]]
                    for tc in range(2):
                        for sc in range(2):
                            S.op("pe", lambda e, tc=tc, sc=sc: e.matmul(ob[tc][:, 0:65], lhsT=PT[pi][:, sc, 128 * tc:128 * tc + 128],
                                                                       rhs=Vaug[:, 2 * n + sc, hq, :],
                                                                       start=(n == 0 and sc == 0), stop=(n == i and sc == 1)),
                                 reads=[bPT[pi], bVaug], writes=[bPB[ob_idx[tc]]])
                    if n == i:
                        for tc in range(2):
                            rc = rec[:, 2 * (it % 2) + tc:2 * (it % 2) + tc + 1]
                            S.op("dve", lambda e, tc=tc, rc=rc: e.reciprocal(out=rc, in_=ob[tc][:, 64:65]), reads=[bPB[ob_idx[tc]]], writes=[brec])
                            S.op("dve", lambda e, tc=tc, rc=rc: e.tensor_scalar(out=Yall[:, 2 * i + tc, 64 * hq:64 * hq + 64], in0=ob[tc][:, 0:64], scalar1=rc,
                                                                               scalar2=None, op0=ALU.mult),
                                 reads=[bPB[ob_idx[tc]], brec], writes=[bYall])

                pending = []
                for it, (hq, i) in enumerate(items):
                    for (pi, n) in qk_stage(hq, i):
                        pending.append((hq, i, it, pi, n))
                        if len(pending) > LOOK:
                            pv_stage(*pending.pop(0))
                while pending:
                    pv_stage(*pending.pop(0))
                ygm = msb("ygm", [128, 512], BF16)
                t1m = msb("t1m", [128, 512], BF16)
                bygm = Buf(); bt1m = Buf()
                def m_gate(tb):
                    tsl = slice(512 * tb, 512 * tb + 512)
                    pyt = pb_bf(7)[:, 0:512].rearrange("p (a t) -> p a t", a=4)
                    for jj in range(4):
                        S.op("pe", lambda e, jj=jj, tb=tb: e.transpose(out=pyt[:, jj, :], in_=Yall[:, 4 * tb + jj, :], identity=identb[:]),
                             reads=[bYall, bconst], writes=[bPB[7]])
                    S.op("dve", lambda e, tsl=tsl: e.scalar_tensor_tensor(out=t1m[:], in0=SG[:, tsl], scalar=1.0, in1=GT[:, tsl], op0=ALU.add, op1=ALU.mult),
                         reads=[bSG], writes=[bt1m])
                    S.op("dve", lambda e, tsl=tsl, pyt=pyt: e.scalar_tensor_tensor(out=YgT[:, 4 + pair, tsl], in0=t1m[:], scalar=0.5,
                                                                                  in1=pyt.rearrange("p a t -> p (a t)"), op0=ALU.mult, op1=ALU.mult),
                         reads=[bt1m, bPB[7]], writes=[bYg[4 + pair]])
                for tb in range(nblk):
                    m_gate(tb)
                S.barrier()
                for lst_ in (xt, bxt, xnb, bxnb, fes, bfes):
                    del lst_[2:]
                fe_nbuf[0] = 2

        for pair_ in do_M:
            phase_M(pair_)

        if dump_yg:
            for c in dbg.get("dump_chunks", range(8)):
                S.dma("sp", ygdump[:, c, 0:nt_lim * 128], YgT[:, c, 0:nt_lim * 128], reads=[bYg[c]], force=True)
        if do_O:
            with ExitStack() as os_:
                def osb(name, shape, dt=F32):
                    return sb("O_" + name, shape, dt, stack=os_)
                Wo = osb("Wo", [128, 8, D], BF16)
                gpb = osb("gpb", [128, D])
                bWo = Buf(); bgp = Buf()
                S.dma("sp", gpb[:], gpost_row[0:1, :].partition_broadcast(128), writes=[bgp])
                with ExitStack() as wso:
                    alloc_staging(wso)
                    for m4 in range(4):
                        st, bst = load_w(w_out_v[:, :, 256 * m4:256 * m4 + 256], 256)
                        S.op("pool", lambda e, st=st, m4=m4: e.tensor_copy(out=Wo[:, :, 256 * m4:256 * m4 + 256], in_=st[:, :, 0:256]),
                             reads=[bst], writes=[bWo])
                    S.barrier()
                ot = [osb("ot%d" % i, [128, D]) for i in range(2)]
                bot = [Buf() for _ in range(2)]
                osm = [osb("osm%d" % i, [128, 8]) for i in range(2)]
                bosm = [Buf() for _ in range(2)]
                ojunk = osb("ojunk", [128, 512], BF16)
                bojunk = Buf()
                def o_tile(tau):
                    k = tau % 2
                    banks = (1 + 2 * k, 2 + 2 * k)
                    if tau == 0:
                        S.dma("sp", xt[0][:], x[0:128, :], writes=[bxt[0]])
                    if tau + 1 < nt_lim:
                        S.dma("sp", xt[1 - k][:], x[(tau + 1) * 128:(tau + 2) * 128, :], writes=[bxt[1 - k]])
                    for hf in range(2):
                        for m in range(8):
                            S.op("pe", lambda e, m=m, hf=hf: e.matmul(PB[banks[hf]][:, :], lhsT=YgT[:, m, tau * 128:(tau + 1) * 128], rhs=Wo[:, m, 512 * hf:512 * hf + 512],
                                                                     start=(m == 0), stop=(m == 7)),
                                 reads=[bYg[m], bWo], writes=[bPB[banks[hf]]])
                    for hf in range(2):
                        S.op("act", lambda e, hf=hf: e.activation(out=ojunk[:], in_=PB[banks[hf]][:, :], func=AF.Square, accum_out=osm[k][:, hf:hf + 1]),
                             reads=[bPB[banks[hf]]], writes=[bojunk, bosm[k]])
                    S.op("dve", lambda e: e.tensor_tensor(out=osm[k][:, 2:3], in0=osm[k][:, 0:1], in1=osm[k][:, 1:2], op=ALU.add), reads=[bosm[k]], writes=[bosm[k]])
                    S.op("dve", lambda e: e.tensor_scalar(out=osm[k][:, 3:4], in0=osm[k][:, 2:3], scalar1=1.0 / D, scalar2=RMS_EPS, op0=ALU.mult, op1=ALU.add),
                         reads=[bosm[k]], writes=[bosm[k]])
                    S.op("pool", lambda e: e.tensor_tensor(out=osm[k][:, 4:5], in0=osm[k][:, 3:4], in1=neghalf[:, 0:1], op=ALU.pow),
                         reads=[bosm[k], bconst], writes=[bosm[k]])
                    for hf in range(2):
                        S.op("dve", lambda e, hf=hf: e.scalar_tensor_tensor(out=ot[k][:, 512 * hf:512 * hf + 512], in0=PB[banks[hf]][:, :], scalar=osm[k][:, 4:5],
                                                                           in1=gpb[:, 512 * hf:512 * hf + 512], op0=ALU.mult, op1=ALU.mult),
                             reads=[bPB[banks[hf]], bosm[k], bgp], writes=[bot[k]])
                    S.op("pool", lambda e: e.tensor_tensor(out=ot[k][:], in0=ot[k][:], in1=xt[k][:], op=ALU.add), reads=[bot[k], bxt[k]], writes=[bot[k]])
                    S.dma("sp", out[tau * 128:(tau + 1) * 128, :], ot[k][:], reads=[bot[k]])
                for tau in range(nt_lim):
                    o_tile(tau)
        S.emit()
        S.close()
    return nc


def make_inputs(x_b, p):
    m = {"x": np.ascontiguousarray(x_b, dtype=np.float32)}
    m["w_in"] = np.ascontiguousarray(p["w_in"][0], dtype=np.float32)
    m["w_out"] = np.ascontiguousarray(p["w_out"][0], dtype=np.float32)
    m["gpre_pc"] = np.ascontiguousarray(p["g_pre"][0].reshape(8, 128).T, dtype=np.float32)
    m["mu_row"] = np.ascontiguousarray(p["tshift_mu"][0].reshape(1, 1664), dtype=np.float32)
    m["w2w0"] = np.ascontiguousarray(np.concatenate([p["w2"][0], p["w0"][0].reshape(1, 512)], axis=0), dtype=np.float32)
    m["a2a0"] = np.ascontiguousarray(np.concatenate([p["a2"][0], p["a0"][0].reshape(1, 512)], axis=0), dtype=np.float32)
    m["prow"] = np.ascontiguousarray(np.stack([p["k_k"][0], p["k_a"][0], p["r_k"][0].reshape(512), p["lnx_w"][0], p["lnx_b"][0]], axis=0),
                                     dtype=np.float32)
    m["gpost_row"] = np.ascontiguousarray(p["g_post"][0].reshape(1, D), dtype=np.float32)
    m.update(host_consts())
    return m


def kernel(x, g_pre, w_in, tshift_mu, w0, w2, a0, a2, k_k, k_a, r_k, lnx_w, lnx_b, w_out, g_post):
    p = dict(g_pre=g_pre, w_in=w_in, tshift_mu=tshift_mu, w0=w0, w2=w2, a0=a0, a2=a2, k_k=k_k, k_a=k_a, r_k=r_k,
             lnx_w=lnx_w, lnx_b=lnx_b, w_out=w_out, g_post=g_post)
    p = {k: np.asarray(v) for k, v in p.items()}
    x = np.asarray(x)
    nc = build()
    in_maps = [make_inputs(x[b], p) for b in range(8)]
    res = run_bass_kernel_spmd(nc, in_maps, core_ids=list(range(8)))
    return np.stack([np.asarray(r["out"], dtype=np.float32) for r in res.results], axis=0)
```

```python
import math
from contextlib import ExitStack

import numpy as np
import ml_dtypes

import concourse.bass as bass
import concourse.mybir as mybir
from concourse.bass_utils import run_bass_kernel_spmd

F32 = mybir.dt.float32
BF16 = mybir.dt.bfloat16
AF = mybir.ActivationFunctionType
ALU = mybir.AluOpType
AX = mybir.AxisListType

T = 4096
D = 1024
NT = T // 128
C0 = math.exp(-0.5)
BIG = 30000.0
RMS_EPS = 1e-6
GN_EPS = 64e-5


class Buf:
    __slots__ = ("name", "last_write", "reads", "excl")

    def __init__(self, name="", excl=False):
        self.name = name
        self.last_write = None
        self.reads = {}
        self.excl = excl


class Sched:
    ENG = ("pe", "act", "dve", "pool", "sp")

    def __init__(self, nc, n_dma_sems=48):
        self.nc = nc
        self.lists = {k: [] for k in self.ENG}
        self.cnt = {k: 0 for k in self.ENG}
        self.known = {k: {} for k in self.ENG}
        self.sems = {}
        self.n_dma = n_dma_sems
        self.dma_tot = [0] * n_dma_sems
        self.dma_next = 0
        self._stack = []
        self.total = 0
        self.log = []
        self.max_ops = None
        self.marks = []

    def mark(self, name):
        self.marks.append((name, self.total))

    def _skip(self):
        self.total += 1
        return self.max_ops is not None and self.total > self.max_ops

    def open(self):
        nc = self.nc
        for k in self.ENG:
            cm = nc.semaphore("s_" + k)
            self.sems[k] = cm.__enter__()
            self._stack.append(cm)
        for i in range(self.n_dma):
            cm = nc.semaphore("s_dma%d" % i)
            self.sems[("dma", i)] = cm.__enter__()
            self._stack.append(cm)

    def close(self):
        for cm in reversed(self._stack):
            cm.__exit__(None, None, None)

    def _deps(self, reads, writes):
        deps = {}

        def add(k, v):
            if deps.get(k, 0) < v:
                deps[k] = v

        for b in reads:
            if b.last_write is not None:
                add(*b.last_write)
            if b.excl:
                for k, v in b.reads.items():
                    add(k, v)
        for b in writes:
            if b.last_write is not None:
                add(*b.last_write)
            for k, v in b.reads.items():
                add(k, v)
        return deps

    def _emit_waits(self, eng, deps):
        kn = self.known[eng]
        for k, v in deps.items():
            if k == eng and eng == "pe":
                continue
            if kn.get(k, 0) >= v:
                continue
            kn[k] = v
            sem = self.sems[k]
            self.log.append((eng, "wait", k, v))
            self.lists[eng].append(lambda e, sem=sem, v=v: e.wait_ge(sem, v))

    def op(self, eng, fn, reads=(), writes=()):
        if self._skip():
            return 0
        deps = self._deps(reads, writes)
        self._emit_waits(eng, deps)
        self.cnt[eng] += 1
        v = self.cnt[eng]
        sem = self.sems[eng]
        self.log.append((eng, "op", self.total, v))
        self.lists[eng].append(lambda e, fn=fn, sem=sem: fn(e).then_inc(sem, 1))
        for b in reads:
            if b.reads.get(eng, 0) < v:
                b.reads[eng] = v
        for b in writes:
            b.last_write = (eng, v)
            b.reads = {}
        return v

    def dma(self, eng, out, in_, reads=(), writes=(), force=False):
        if self._skip() and not force:
            return None
        deps = self._deps(reads, writes)
        i = self.dma_next
        self.dma_next = (self.dma_next + 1) % self.n_dma
        k = ("dma", i)
        if self.dma_tot[i] > 0:
            deps[k] = max(deps.get(k, 0), self.dma_tot[i])
        self._emit_waits(eng, deps)
        self.dma_tot[i] += 16
        v = self.dma_tot[i]
        sem = self.sems[k]
        self.lists[eng].append(
            lambda e, out=out, in_=in_, sem=sem: e.dma_start(out=out, in_=in_).then_inc(sem, 16))
        for b in reads:
            if b.reads.get(k, 0) < v:
                b.reads[k] = v
        for b in writes:
            b.last_write = (k, v)
            b.reads = {}
        return (k, v)

    def _all_deps(self):
        deps = {}
        for i in range(self.n_dma):
            if self.dma_tot[i] > 0:
                deps[("dma", i)] = self.dma_tot[i]
        for k in self.ENG:
            if self.cnt[k] > 0:
                deps[k] = self.cnt[k]
        return deps

    def barrier(self):
        deps = self._all_deps()
        for e in self.ENG:
            self._emit_waits(e, dict(deps))

    def emit(self):
        nc = self.nc
        self._emit_waits("sp", self._all_deps())
        with nc.Block() as block:
            @block.sync
            def _(e):
                for f in self.lists["sp"]:
                    f(e)

            @block.tensor
            def _(e):
                for f in self.lists["pe"]:
                    f(e)

            @block.scalar
            def _(e):
                for f in self.lists["act"]:
                    f(e)

            @block.vector
            def _(e):
                for f in self.lists["dve"]:
                    f(e)

            @block.gpsimd
            def _(e):
                for f in self.lists["pool"]:
                    f(e)


def host_consts():
    c = {}
    c["c_ident"] = np.eye(128, dtype=np.float32)
    tri = np.zeros((3, 128, 128), np.float32)
    for s in range(128):
        for t in range(128):
            if s // 64 == t // 64:
                tri[2, s, t] = -C0
                if s <= t:
                    tri[0, s, t] = -C0
                else:
                    tri[1, s, t] = -C0
    c["c_tri"] = tri
    mu = np.triu(np.ones((64, 64), np.float32), 1)
    mle = np.triu(np.ones((64, 64), np.float32), 0)
    c["c_maskAM"] = np.concatenate([-mu, mle], axis=1)
    c["c_maskX"] = np.ascontiguousarray(-mu.T)
    past = np.zeros((16, 16), np.float32)
    own = np.full((16, 16), -BIG, np.float32)
    for blk in range(16):
        for n in range(16):
            if n >= blk:
                past[blk, n] = -1e30
            if n == blk:
                own[blk, n] = 0.0
    c["c_past"] = past.reshape(1, 256)
    c["c_own"] = own.reshape(1, 256)
    bf = ml_dtypes.bfloat16
    onehot = np.zeros((16, T), np.float32)
    for n in range(16):
        onehot[n, n * 256:(n + 1) * 256] = 1.0
    c["c_onehot"] = onehot.astype(bf)
    tw = (np.arange(T) % 256).astype(np.float32)
    qrows = np.zeros((8, 2, T), np.float32)
    krows = np.zeros((8, 2, T), np.float32)
    for h in range(8):
        slope = 2.0 ** (-(h + 1))
        qrows[h, 0] = -slope * tw
        qrows[h, 1] = 1.0
        krows[h, 0] = 1.0
        krows[h, 1] = slope * tw
    c["c_qrows"] = qrows.astype(bf)
    c["c_krows"] = krows.astype(bf)
    causal = np.zeros((128, 2, 256), np.float32)
    for sc in range(2):
        for p in range(128):
            s = sc * 128 + p
            causal[p, sc, :s] = -BIG
    c["c_causal"] = causal.astype(bf)
    return c


CONST_DT = {"c_ident": F32, "c_tri": F32, "c_maskAM": F32, "c_maskX": F32, "c_past": F32, "c_own": F32,
            "c_onehot": BF16, "c_qrows": BF16, "c_krows": BF16, "c_causal": BF16}


def build(dbg=None):
    dbg = dbg or {}
    do_R = dbg.get("R", (0, 1))
    do_M = dbg.get("M", (0, 1, 2, 3))
    do_O = dbg.get("O", True)
    dump_yg = dbg.get("dump_yg", False)
    nt_lim = dbg.get("nt", NT)

    nc = bass.Bass("TRN2", target_bir_lowering=False)
    dr = {}

    def din(name, shape, dt=F32):
        dr[name] = nc.dram_tensor(name, list(shape), dt, kind="ExternalInput").ap()
        return dr[name]

    x = din("x", [T, D])
    w_in = din("w_in", [D, 4224])
    w_out = din("w_out", [D, D])
    gpre_pc = din("gpre_pc", [128, 8])
    mu_row = din("mu_row", [1, 1664])
    w2w0 = din("w2w0", [65, 512])
    a2a0 = din("a2a0", [65, 512])
    prow = din("prow", [5, 512])
    gpost_row = din("gpost_row", [1, D])
    hc = host_consts()
    for k, v in hc.items():
        din(k, v.shape, CONST_DT[k])
    out = nc.dram_tensor("out", [T, D], F32, kind="ExternalOutput").ap()
    if dump_yg:
        ygdump = nc.dram_tensor("ygdump", [128, 8, T], BF16, kind="ExternalOutput").ap()

    S = Sched(nc)
    S.max_ops = dbg.get("max_ops")
    build.last_sched = S
    w_in_v = w_in.rearrange("(c p) n -> p c n", p=128)
    w_out_v = w_out.rearrange("(c p) n -> p c n", p=128)

    with ExitStack() as es:
        S.open()

        def sb(name, shape, dt=F32, stack=es):
            return stack.enter_context(nc.sbuf_tensor(name, list(shape), dt))

        PB = [es.enter_context(nc.psum_tensor("pb%d" % i, [128, 512], F32)) for i in range(8)]
        bPB = [Buf("pb%d" % i, excl=True) for i in range(8)]

        def pb_bf(i):
            return PB[i][:].bitcast(BF16)

        YgT = sb("YgT", [128, 8, T], BF16)
        bYg = [Buf("YgT%d" % i) for i in range(8)]
        identf = sb("identf", [128, 128])
        identb = sb("identb", [128, 128], BF16)
        neghalf = sb("neghalf", [128, 4])
        gpre = sb("gpre", [128, 8])
        bconst = Buf("const")
        S.dma("sp", identf[:], dr["c_ident"][:], writes=[bconst])
        S.dma("sp", gpre[:], gpre_pc[:], writes=[bconst])
        S.op("dve", lambda e: e.tensor_copy(out=identb[:], in_=identf[:]), reads=[bconst], writes=[bconst])
        S.op("pool", lambda e: e.memset(neghalf[:], -0.5), writes=[bconst])
        joint = sb("joint", [128, 4])
        bjoint = Buf("joint")

        def dma_group(pairs, group_bufs):
            bufs = []
            for (o_, i_) in pairs:
                b_ = Buf()
                S.dma("sp", o_, i_, writes=[b_])
                bufs.append(b_)
            S.op("pool", lambda e: e.memset(joint[:, 0:1], 0.0), reads=bufs, writes=list(group_bufs) + [bjoint])

        xt = [sb("xt%d" % i, [128, D]) for i in range(2)]
        bxt = [Buf("xt%d" % i) for i in range(2)]
        xnb = [sb("xnb%d" % i, [128, D], BF16) for i in range(2)]
        bxnb = [Buf("xnb%d" % i) for i in range(2)]
        junk = sb("junk", [128, D], BF16)
        bjunk = Buf("junk")
        fes = [sb("fes%d" % i, [128, 4]) for i in range(2)]
        bfes = [Buf("fes%d" % i) for i in range(2)]

        fe_nbuf = [2]

        def fe1(tau):
            k = tau % fe_nbuf[0]
            xtk, xnbk, fesk, bxtk, bxnbk, bfesk = xt[k], xnb[k], fes[k], bxt[k], bxnb[k], bfes[k]
            S.dma("sp", xtk[:], x[tau * 128:(tau + 1) * 128, :], writes=[bxtk])
            S.op("act", lambda e: e.activation(out=junk[:], in_=xtk[:], func=AF.Square, accum_out=fesk[:, 0:1]),
                 reads=[bxtk], writes=[bjunk, bfesk])
            S.op("pool", lambda e: e.tensor_scalar(out=fesk[:, 1:2], in0=fesk[:, 0:1], scalar1=1.0 / D, scalar2=RMS_EPS,
                                                   op0=ALU.mult, op1=ALU.add), reads=[bfesk], writes=[bfesk])
            S.op("pool", lambda e: e.tensor_tensor(out=fesk[:, 2:3], in0=fesk[:, 1:2], in1=neghalf[:, 0:1], op=ALU.pow),
                 reads=[bfesk, bconst], writes=[bfesk])

        def fe2(tau, dst_ap, bdst, evac_eng="act"):
            k = tau % fe_nbuf[0]
            xtk, xnbk, fesk, bxtk, bxnbk, bfesk = xt[k], xnb[k], fes[k], bxt[k], bxnb[k], bfes[k]
            S.op("act", lambda e: e.activation(out=xnbk[:], in_=xtk[:], func=AF.Copy, scale=fesk[:, 2:3]),
                 reads=[bxtk, bfesk], writes=[bxnbk])
            psT = pb_bf(0).rearrange("p (c t) -> p c t", c=8)
            for c in range(8):
                S.op("pe", lambda e, c=c: e.transpose(out=psT[:, c, :], in_=xnbk[:, c * 128:(c + 1) * 128], identity=identb[:]),
                     reads=[bxnbk, bconst], writes=[bPB[0]])
            if evac_eng == "act":
                S.op("act", lambda e: e.copy(out=dst_ap, in_=psT), reads=[bPB[0]], writes=[bdst])
            else:
                S.op("dve", lambda e: e.tensor_copy(out=dst_ap, in_=psT), reads=[bPB[0]], writes=[bdst])

        def frontend(tau, dst_ap, bdst, evac_eng="act"):
            fe1(tau)
            fe2(tau, dst_ap, bdst, evac_eng)

        wst = [None, None]
        bwst = [None, None]
        wst_ctr = [0]
        wst_gen = [0]

        def alloc_staging(stack):
            g = wst_gen[0]
            wst_gen[0] += 1
            for i in range(2):
                wst[i] = sb("wst%d_%d" % (g, i), [128, 8, 256], stack=stack)
                bwst[i] = Buf("wst%d" % i)

        def load_w(src_ap, ncols):
            k = wst_ctr[0] % 2
            wst_ctr[0] += 1
            S.dma("sp", wst[k][:, :, 0:ncols], src_ap, writes=[bwst[k]])
            return wst[k], bwst[k]

        def phase_R(half):
            with ExitStack() as rs:
                def rsb(name, shape, dt=F32):
                    return sb("R%d_%s" % (half, name), shape, dt, stack=rs)

                ch0 = 256 * half
                tri = rsb("tri", [128, 3, 128])
                maskAM = rsb("maskAM", [128, 128])
                maskX = rsb("maskX", [128, 64])
                pbc = rsb("pbc", [128, 5, 256])
                w2w0h = rsb("w2w0h", [65, 256])
                a2a0h = rsb("a2a0h", [65, 256])
                brc = Buf("rconst")
                prs = [(tri[:], dr["c_tri"].rearrange("k s t -> s k t"))]
                for j in range(2):
                    prs.append((maskAM[64 * j:64 * j + 64, :], dr["c_maskAM"][:]))
                    prs.append((maskX[64 * j:64 * j + 64, :], dr["c_maskX"][:]))
                for i in range(5):
                    prs.append((pbc[:, i, :], prow[i:i + 1, ch0:ch0 + 256].partition_broadcast(128)))
                prs.append((w2w0h[:], w2w0[:, ch0:ch0 + 256]))
                prs.append((a2a0h[:], a2a0[:, ch0:ch0 + 256]))
                dma_group(prs, [brc])
                W1 = rsb("W1", [128, 8, 896], BF16)
                W2 = rsb("W2", [128, 8, 896], BF16)
                Wg = rsb("Wg", [128, 8, 256], BF16)
                bW = Buf("RW")
                with ExitStack() as ws:
                    alloc_staging(ws)
                    mub = sb("R%d_mub" % half, [128, 896], stack=ws)
                    omub = sb("R%d_omub" % half, [128, 896], stack=ws)
                    bmu = Buf("mu")
                    srcs = [(0 + ch0, 256), (512 + ch0, 256), (1024 + ch0, 256), (1536, 128)]
                    off = 0
                    prs_ = []
                    for (c0, n) in srcs:
                        prs_.append((mub[:, off:off + n], mu_row[0:1, c0:c0 + n].partition_broadcast(128)))
                        off += n
                    dma_group(prs_, [bmu])
                    S.op("pool", lambda e: e.tensor_scalar(out=omub[:], in0=mub[:], scalar1=-1.0, scalar2=1.0, op0=ALU.mult, op1=ALU.add),
                         reads=[bmu], writes=[bmu])
                    off = 0
                    for (c0, n) in srcs:
                        st, bst = load_w(w_in_v[:, :, c0:c0 + n], n)
                        S.op("pool", lambda e, st=st, n=n: e.tensor_tensor(out=st[:, :, 0:n], in0=st[:, :, 0:n],
                                                                          in1=gpre[:, :, None].to_broadcast([128, 8, n]), op=ALU.mult),
                             reads=[bst, bconst], writes=[bst])
                        S.op("pool", lambda e, st=st, n=n, off=off: e.tensor_tensor(out=W2[:, :, off:off + n], in0=st[:, :, 0:n],
                                                                                   in1=mub[:, None, off:off + n].to_broadcast([128, 8, n]), op=ALU.mult),
                             reads=[bst, bmu], writes=[bW])
                        S.op("pool", lambda e, st=st, n=n, off=off: e.tensor_tensor(out=W1[:, :, off:off + n], in0=st[:, :, 0:n],
                                                                                   in1=omub[:, None, off:off + n].to_broadcast([128, 8, n]), op=ALU.mult),
                             reads=[bst, bmu], writes=[bW])
                        off += n
                    st, bst = load_w(w_in_v[:, :, 3200 + ch0:3200 + ch0 + 256], 256)
                    S.op("pool", lambda e, st=st: e.tensor_tensor(out=Wg[:], in0=st[:, :, 0:256],
                                                                 in1=gpre[:, :, None].to_broadcast([128, 8, 256]), op=ALU.mult),
                         reads=[bst, bconst], writes=[bW])
                    S.barrier()
                NXT = 4
                xT = [rsb("xT%d" % i, [128, 8, 129], BF16) for i in range(NXT)]
                bxT = [Buf("xT%d" % i) for i in range(NXT)]
                TW = rsb("TW", [65, 128]); AD = rsb("AD", [65, 128])
                E3 = rsb("E3", [128, 256]); sq = rsb("sq", [128, 256]); kkn = rsb("kkn", [128, 256])
                kmod = rsb("kmod", [128, 256]); bvec = rsb("bvec", [128, 256]); rkt = rsb("rkt", [128, 256])
                dbl = {}
                for nm_, shp_ in [("rk32", [128, 512]), ("v32", [128, 256]), ("thz", [128, 512]), ("sgi", [128, 512]), ("d3", [128, 256]),
                                  ("E1", [128, 256]), ("E2", [128, 256]), ("E4", [128, 256]), ("kk", [128, 256]), ("km1", [128, 256]),
                                  ("thg", [128, 256]), ("g32", [128, 256]), ("sm", [128, 16])]:
                    dbl[nm_] = [rsb("%s_%d" % (nm_, i_), shp_) for i_ in range(2)]
                bws = [{n: Buf(n) for n in "rk32 v32 thz sgi d3 E1 E2 E4 kk km1 thg g32 sm".split()} for _ in range(2)]
                bw = {n: Buf(n) for n in "TW AD E3 sq kkn kmod bvec rkt".split()}
                S.op("pool", lambda e: e.memset(TW[64:65, :], 1.0), writes=[bw["TW"]])
                S.op("pool", lambda e: e.memset(AD[64:65, :], 1.0), writes=[bw["AD"]])
                TM = [rsb("TM%d" % i, [128, 6, 256], BF16) for i in range(3)]
                Vb = [rsb("Vb%d" % i, [128, 256], BF16) for i in range(3)]
                EC = [rsb("EC%d" % i, [128, 256]) for i in range(3)]
                bonus = [rsb("bonus%d" % i, [128, 256]) for i in range(3)]
                FM = [rsb("FM%d" % i, [128, 4, 4, 64], BF16) for i in range(3)]
                bTM = [Buf() for _ in range(3)]; bVb = [Buf() for _ in range(3)]; bEC = [Buf() for _ in range(3)]
                bbonus = [Buf() for _ in range(3)]; bFM = [Buf() for _ in range(3)]
                SAM = rsb("SAM", [128, 4, 128], BF16); SKM = rsb("SKM", [128, 4, 128], BF16)
                XY = [rsb("XY%d" % i, [128, 2, 4, 64], BF16) for i in range(2)]
                Wt = rsb("Wt", [128, 4, 128], BF16)
                DG = rsb("DG", [128, 4, 64]); MTs = rsb("MTs", [128, 4, 64]); Gs = rsb("Gs", [128, 4, 64])
                RpT = rsb("RpT", [128, 4, 64], BF16)
                Hf = rsb("Hf", [128, 4, 64]); Hb = rsb("Hb", [128, 4, 64], BF16)
                bSAM = [Buf() for _ in range(2)]; bSKM = [Buf() for _ in range(2)]
                bXY = [[Buf() for _ in range(2)] for _ in range(2)]
                bWt = [Buf() for _ in range(2)]; bDG = [Buf() for _ in range(2)]; bMTs = [Buf() for _ in range(2)]
                bGs = [Buf() for _ in range(2)]; bRpT = [Buf() for _ in range(2)]
                bHf = [Buf() for _ in range(2)]; bHb = [Buf() for _ in range(2)]
                S.op("pool", lambda e: e.memset(Hf[:], 0.0), writes=[bHf[0], bHf[1]])
                S.op("pool", lambda e: e.memset(Hb[:], 0.0), writes=[bHb[0], bHb[1]])
                Yt = [rsb("Yt%d" % i, [128, 256]) for i in range(2)]
                bYt = [Buf() for _ in range(2)]
                t1g = [rsb("t1g%d" % i, [128, 256]) for i in range(3)]
                bt1g = [Buf() for _ in range(3)]
                yc = rsb("yc", [128, 256]); ysq = rsb("ysq", [128, 256])
                ygb = rsb("ygb", [128, 256], BF16); pm = rsb("pm", [128, 16])
                bp = {n: Buf(n) for n in "yc ysq ygb pm".split()}

                def tile_RF(tau):
                    kx = tau % NXT
                    if tau == 0:
                        S.op("act", lambda e: e.memset(xT[kx][:, :, 0:1], 0.0), writes=[bxT[kx]]) if False else \
                            S.op("dve", lambda e: e.memset(xT[kx][:, :, 0:1], 0.0), writes=[bxT[kx]])
                    else:
                        kp = (tau - 1) % NXT
                        S.op("act", lambda e: e.copy(out=xT[kx][:, :, 0:1], in_=xT[kp][:, :, 128:129]),
                             reads=[bxT[kp]], writes=[bxT[kx]])
                    frontend(tau, xT[kx][:, :, 1:129], bxT[kx])
                    yield

                def tile_RA(tau):
                    k = tau % NXT
                    k3 = tau % 3
                    ka = tau % 2
                    rk32, v32, thz, sgi, d3, E1, E2, E4, kk, km1, thg, g32, sm = [dbl[n_][ka] for n_ in
                        "rk32 v32 thz sgi d3 E1 E2 E4 kk km1 thg g32 sm".split()]
                    bwd = bws[ka]
                    cur = lambda c: xT[k][:, c, 1:129]
                    prv = lambda c: xT[k][:, c, 0:128]
                    for i, (wt_, xv) in enumerate([(W1, cur), (W2, prv)]):
                        for c in range(8):
                            S.op("pe", lambda e, c=c, wt_=wt_, xv=xv, i=i: e.matmul(PB[1][:, :], lhsT=xv(c), rhs=wt_[:, c, 0:512],
                                                                                 start=(i == 0 and c == 0), stop=(i == 1 and c == 7)),
                                 reads=[bxT[k], bW], writes=[bPB[1]])
                    for i, (wt_, xv) in enumerate([(W1, cur), (W2, prv)]):
                        for c in range(8):
                            S.op("pe", lambda e, c=c, wt_=wt_, xv=xv, i=i: e.matmul(PB[2][:, 0:256], lhsT=xv(c), rhs=wt_[:, c, 512:768],
                                                                                 start=(i == 0 and c == 0), stop=(i == 1 and c == 7)),
                                 reads=[bxT[k], bW], writes=[bPB[2]])
                    for i, (wt_, xv) in enumerate([(W1, cur), (W2, prv)]):
                        for c in range(8):
                            S.op("pe", lambda e, c=c, wt_=wt_, xv=xv, i=i: e.matmul(PB[2][:, 256:384], lhsT=wt_[:, c, 768:896], rhs=xv(c),
                                                                                 start=(i == 0 and c == 0), stop=(i == 1 and c == 7)),
                                 reads=[bxT[k], bW], writes=[bPB[2]])
                    yield
                    S.op("act", lambda e: e.copy(out=rk32[:], in_=PB[1][:, :]), reads=[bPB[1]], writes=[bwd["rk32"]])
                    S.op("act", lambda e: e.copy(out=v32[:], in_=PB[2][:, 0:256]), reads=[bPB[2]], writes=[bwd["v32"]])
                    S.op("act", lambda e: e.copy(out=Vb[k3][:], in_=PB[2][:, 0:256]), reads=[bPB[2]], writes=[bVb[k3]])
                    S.op("act", lambda e: e.activation(out=TW[0:64, :], in_=PB[2][0:64, 256:384], func=AF.Tanh),
                         reads=[bPB[2]], writes=[bw["TW"]])
                    S.op("act", lambda e: e.copy(out=AD[0:64, :], in_=PB[2][64:128, 256:384]), reads=[bPB[2]], writes=[bw["AD"]])
                    yield
                    yield
                    S.op("pe", lambda e: e.matmul(PB[3][:, 0:256], lhsT=TW[:, :], rhs=w2w0h[:, :], start=True, stop=True),
                         reads=[bw["TW"], brc], writes=[bPB[3]])
                    S.op("pe", lambda e: e.matmul(PB[3][:, 256:512], lhsT=AD[:, :], rhs=a2a0h[:, :], start=True, stop=True),
                         reads=[bw["AD"], brc], writes=[bPB[3]])
                    for c in range(8):
                        S.op("pe", lambda e, c=c: e.matmul(PB[1][:, 256:512], lhsT=cur(c), rhs=Wg[:, c, :], start=(c == 0), stop=(c == 7)),
                             reads=[bxT[k], bW], writes=[bPB[1]])
                    S.op("act", lambda e: e.activation(out=thz[:], in_=PB[3][:, :], func=AF.Tanh, scale=0.5), reads=[bPB[3]], writes=[bwd["thz"]])
                    S.op("act", lambda e: e.activation(out=thg[:], in_=PB[1][:, 256:512], func=AF.Tanh, scale=0.5), reads=[bPB[1]], writes=[bwd["thg"]])
                    S.op("act", lambda e: e.copy(out=g32[:], in_=PB[1][:, 256:512]), reads=[bPB[1]], writes=[bwd["g32"]])
                    S.op("pool", lambda e: e.tensor_scalar(out=sgi[:], in0=thz[:], scalar1=0.5, scalar2=0.5, op0=ALU.mult, op1=ALU.add),
                         reads=[bwd["thz"]], writes=[bwd["sgi"]])
                    sg = sgi[:, 0:256]
                    icl = sgi[:, 256:512]
                    r32 = rk32[:, 0:256]
                    k32 = rk32[:, 256:512]
                    v3 = lambda ap: ap.rearrange("p (h c) -> p h c", h=4)
                    S.op("pool", lambda e: e.tensor_scalar(out=km1[:], in0=thz[:, 256:512], scalar1=0.5, scalar2=-0.5, op0=ALU.mult, op1=ALU.add),
                         reads=[bwd["thz"]], writes=[bwd["km1"]])
                    S.op("pool", lambda e: e.tensor_tensor(out=kk[:], in0=k32, in1=pbc[:, 0, :], op=ALU.mult), reads=[bwd["rk32"], brc], writes=[bwd["kk"]])
                    yield
                    yield
                    S.op("pe", lambda e: e.matmul(PB[3][:, 0:256], lhsT=tri[:, 0, :], rhs=sg, start=True, stop=True),
                         reads=[bwd["sgi"], brc], writes=[bPB[3]])
                    S.op("pe", lambda e: e.matmul(PB[3][:, 256:512], lhsT=tri[:, 1, :], rhs=sg, start=True, stop=True),
                         reads=[bwd["sgi"], brc], writes=[bPB[3]])
                    S.op("pe", lambda e: e.matmul(PB[1][:, 0:256], lhsT=tri[:, 2, :], rhs=sg, start=True, stop=True),
                         reads=[bwd["sgi"], brc], writes=[bPB[1]])
                    S.op("act", lambda e: e.activation(out=E1[:], in_=PB[3][:, 0:256], func=AF.Exp), reads=[bPB[3]], writes=[bwd["E1"]])
                    S.op("act", lambda e: e.activation(out=E2[:], in_=PB[3][:, 0:256], func=AF.Exp, scale=-1.0), reads=[bPB[3]], writes=[bwd["E2"]])
                    S.op("act", lambda e: e.activation(out=E4[:], in_=PB[3][:, 256:512], func=AF.Exp), reads=[bPB[3]], writes=[bwd["E4"]])
                    S.op("act", lambda e: e.activation(out=EC[k3][:], in_=PB[1][:, 0:256], func=AF.Exp), reads=[bPB[1]], writes=[bEC[k3]])
                    S.op("act", lambda e: e.activation(out=d3[:], in_=sg, func=AF.Exp, scale=C0), reads=[bwd["sgi"]], writes=[bwd["d3"]])
                    for hl in range(4):
                        S.op("act", lambda e, hl=hl: e.activation(out=sq[:, hl * 64:(hl + 1) * 64], in_=kk[:, hl * 64:(hl + 1) * 64], func=AF.Square,
                                                                 accum_out=sm[:, hl:hl + 1]), reads=[bwd["kk"]], writes=[bw["sq"], bwd["sm"]])
                    yield
                def tile_RB(tau):
                    k = tau % 2
                    k3 = tau % 3
                    ka = tau % 2
                    rk32, v32, thz, sgi, d3, E1, E2, E4, kk, km1, thg, g32, sm = [dbl[n_][ka] for n_ in
                        "rk32 v32 thz sgi d3 E1 E2 E4 kk km1 thg g32 sm".split()]
                    bwd = bws[ka]
                    sg = sgi[:, 0:256]
                    icl = sgi[:, 256:512]
                    r32 = rk32[:, 0:256]
                    k32 = rk32[:, 256:512]
                    v3 = lambda ap: ap.rearrange("p (h c) -> p h c", h=4)
                    S.op("pool", lambda e: e.tensor_scalar(out=t1g[k3][:], in0=thg[:], scalar1=0.5, scalar2=0.5, op0=ALU.mult, op1=ALU.add),
                         reads=[bwd["thg"]], writes=[bt1g[k3]])
                    S.op("pool", lambda e: e.tensor_tensor(out=t1g[k3][:], in0=t1g[k3][:], in1=g32[:], op=ALU.mult), reads=[bt1g[k3], bwd["g32"]], writes=[bt1g[k3]])
                    S.op("pool", lambda e: e.tensor_tensor(out=E3[:], in0=E1[:], in1=d3[:], op=ALU.mult), reads=[bwd["E1"], bwd["d3"]], writes=[bw["E3"]])
                    S.op("pool", lambda e: e.tensor_scalar(out=sm[:, 4:8], in0=sm[:, 0:4], scalar1=1e-24, scalar2=None, op0=ALU.max),
                         reads=[bwd["sm"]], writes=[bwd["sm"]])
                    S.op("pool", lambda e: e.tensor_tensor(out=sm[:, 8:12], in0=sm[:, 4:8], in1=neghalf[:, 0:4], op=ALU.pow),
                         reads=[bwd["sm"], bconst], writes=[bwd["sm"]])
                    S.op("pool", lambda e: e.tensor_tensor(out=v3(kkn[:]), in0=v3(kk[:]), in1=sm[:, 8:12, None].to_broadcast([128, 4, 64]), op=ALU.mult),
                         reads=[bwd["kk"], bwd["sm"]], writes=[bw["kkn"]])
                    yield
                    S.op("pool", lambda e: e.tensor_tensor(out=km1[:], in0=km1[:], in1=pbc[:, 1, :], op=ALU.mult), reads=[bwd["km1"], brc], writes=[bwd["km1"]])
                    S.op("pool", lambda e: e.tensor_tensor(out=kmod[:], in0=km1[:], in1=k32, op=ALU.mult), reads=[bwd["km1"], bwd["rk32"]], writes=[bw["kmod"]])
                    S.op("pool", lambda e: e.tensor_tensor(out=kmod[:], in0=kmod[:], in1=k32, op=ALU.add), reads=[bw["kmod"], bwd["rk32"]], writes=[bw["kmod"]])
                    S.op("pool", lambda e: e.tensor_tensor(out=bvec[:], in0=kkn[:], in1=icl, op=ALU.mult), reads=[bw["kkn"], bwd["sgi"]], writes=[bw["bvec"]])
                    yield
                    S.op("pool", lambda e: e.tensor_tensor(out=TM[k3][:, 0, :], in0=kkn[:], in1=E3[:], op=ALU.mult), reads=[bw["kkn"], bw["E3"]], writes=[bTM[k3]])
                    S.op("pool", lambda e: e.tensor_tensor(out=TM[k3][:, 1, :], in0=r32, in1=E1[:], op=ALU.mult), reads=[bwd["rk32"], bwd["E1"]], writes=[bTM[k3]])
                    S.op("pool", lambda e: e.tensor_tensor(out=TM[k3][:, 2, :], in0=bvec[:], in1=E2[:], op=ALU.mult), reads=[bw["bvec"], bwd["E2"]], writes=[bTM[k3]])
                    S.op("pool", lambda e: e.tensor_tensor(out=TM[k3][:, 3, :], in0=kmod[:], in1=E2[:], op=ALU.mult), reads=[bw["kmod"], bwd["E2"]], writes=[bTM[k3]])
                    yield
                    S.op("pool", lambda e: e.tensor_tensor(out=TM[k3][:, 4, :], in0=bvec[:], in1=E4[:], op=ALU.mult), reads=[bw["bvec"], bwd["E4"]], writes=[bTM[k3]])
                    S.op("pool", lambda e: e.tensor_tensor(out=TM[k3][:, 5, :], in0=kmod[:], in1=E4[:], op=ALU.mult), reads=[bw["kmod"], bwd["E4"]], writes=[bTM[k3]])
                    S.op("pool", lambda e: e.tensor_tensor(out=rkt[:], in0=r32, in1=kmod[:], op=ALU.mult), reads=[bwd["rk32"], bw["kmod"]], writes=[bw["rkt"]])
                    S.op("pool", lambda e: e.tensor_tensor(out=rkt[:], in0=rkt[:], in1=pbc[:, 2, :], op=ALU.mult), reads=[bw["rkt"], brc], writes=[bw["rkt"]])
                    for hl in range(4):
                        S.op("act", lambda e, hl=hl: e.activation(out=sq[:, hl * 64:(hl + 1) * 64], in_=rkt[:, hl * 64:(hl + 1) * 64], func=AF.Copy,
                                                                 accum_out=sm[:, 12 + hl:13 + hl]), reads=[bw["rkt"]], writes=[bw["sq"], bwd["sm"]])
                    yield
                    yield
                    for hp in range(2):
                        fbank = (0, 3)[hp]
                        pf = pb_bf(fbank).rearrange("p (h q t) -> p h q t", h=2, q=4)
                        for hh in range(2):
                            hl = 2 * hp + hh
                            for q in range(4):
                                S.op("pe", lambda e, hh=hh, hl=hl, q=q, pf=pf: e.transpose(out=pf[0:64, hh, q, :], in_=TM[k3][:, q, hl * 64:(hl + 1) * 64],
                                                                                         identity=identb[:]),
                                     reads=[bTM[k3], bconst], writes=[bPB[fbank]])
                        S.op("act", lambda e, hp=hp, pf=pf: e.copy(out=FM[k3][0:64, 2 * hp:2 * hp + 2, :, :], in_=pf[0:64, :, :, 0:64]),
                             reads=[bPB[fbank]], writes=[bFM[k3]])
                        S.op("act", lambda e, hp=hp, pf=pf: e.copy(out=FM[k3][64:128, 2 * hp:2 * hp + 2, :, :], in_=pf[0:64, :, :, 64:128]),
                             reads=[bPB[fbank]], writes=[bFM[k3]])
                        yield
                    S.op("pool", lambda e: e.tensor_tensor(out=v3(bonus[k3][:]), in0=v3(v32[:]), in1=sm[:, 12:16, None].to_broadcast([128, 4, 64]), op=ALU.mult),
                         reads=[bwd["v32"], bwd["sm"]], writes=[bbonus[k3]])

                def chunk_R(tau, j):
                    k = tau % 2
                    k3 = tau % 3
                    lo = 64 * j
                    L = slice(lo, lo + 64)
                    Ba, Bb = 4 + 2 * j, 5 + 2 * j
                    bBa, bBb = bPB[Ba], bPB[Bb]
                    pa3 = PB[Ba][0:64, :].rearrange("p (h c) -> p h c", h=4)
                    pb3 = PB[Bb][0:64, :].rearrange("p (h c) -> p h c", h=4)
                    pa4 = PB[Ba][0:64, :].rearrange("p (a h c) -> p a h c", a=2, h=4)
                    pb4 = PB[Bb][0:64, :].rearrange("p (a h c) -> p a h c", a=2, h=4)
                    fm = FM[k3]
                    for hl in range(4):
                        S.op("pe", lambda e, hl=hl: e.matmul(pa3[:, hl, :].rearrange("p (a b) -> p a b", a=2), lhsT=fm[L, hl, 2, :], rhs=fm[L, hl, 0:2, :],
                                                             start=True, stop=True), reads=[bFM[k3]], writes=[bBa])
                    for hl in range(4):
                        S.op("pe", lambda e, hl=hl: e.matmul(pb3[:, hl, :].rearrange("p (a b) -> p a b", a=2), lhsT=fm[L, hl, 3, :], rhs=fm[L, hl, 0:2, :],
                                                             start=True, stop=True), reads=[bFM[k3]], writes=[bBb])
                    S.op("dve", lambda e: e.tensor_tensor(out=SAM[L, :, :], in0=pa3, in1=maskAM[L, None, :].to_broadcast([64, 4, 128]), op=ALU.mult),
                         reads=[bBa, brc], writes=[bSAM[j]])
                    S.op("dve", lambda e: e.tensor_tensor(out=SKM[L, :, :], in0=pb3, in1=maskAM[L, None, :].to_broadcast([64, 4, 128]), op=ALU.mult),
                         reads=[bBb, brc], writes=[bSKM[j]])
                    yield
                    for hl in range(4):
                        S.op("pe", lambda e, hl=hl: e.matmul(pa4[:, 0, hl, :], lhsT=fm[L, hl, 0, :], rhs=fm[L, hl, 2, :], start=True, stop=True),
                             reads=[bFM[k3]], writes=[bBa])
                    for hl in range(4):
                        S.op("pe", lambda e, hl=hl: e.matmul(pa4[:, 1, hl, :], lhsT=SKM[L, hl, 0:64], rhs=Vb[k3][L, hl * 64:(hl + 1) * 64], start=True, stop=True),
                             reads=[bSKM[j], bVb[k3]], writes=[bBa])
                    xy0 = XY[0]
                    S.op("dve", lambda e: e.tensor_tensor(out=xy0[L, 0, :, :], in0=pa4[:, 0, :, :], in1=maskX[L, None, :].to_broadcast([64, 4, 64]), op=ALU.mult),
                         reads=[bBa, brc], writes=[bXY[0][j]])
                    S.op("dve", lambda e: e.tensor_copy(out=xy0[L, 1, :, :], in_=SAM[L, :, 0:64]), reads=[bSAM[j]], writes=[bXY[0][j]])
                    S.op("dve", lambda e: e.tensor_copy(out=Wt[L, :, 64:128], in_=pa4[:, 1, :, :]), reads=[bBa], writes=[bWt[j]])
                    S.op("dve", lambda e: e.tensor_scalar(out=Wt[L, :, 0:64], in0=TM[k3][L, 0, :].rearrange("p (h c) -> p h c", h=4), scalar1=-1.0, scalar2=None, op0=ALU.mult),
                         reads=[bTM[k3]], writes=[bWt[j]])
                    yield
                    for kx in range(6):
                        cur_, nxt_ = XY[kx % 2], XY[(kx + 1) % 2]
                        bcur, bnxt = bXY[kx % 2][j], bXY[(kx + 1) % 2][j]
                        if kx < 5:
                            for hl in range(4):
                                S.op("pe", lambda e, hl=hl, cur_=cur_: e.matmul(pb4[:, 0, hl, :], lhsT=cur_[L, 1, hl, :], rhs=cur_[L, 0, hl, :], start=True, stop=True),
                                     reads=[bcur], writes=[bBb])
                                S.op("pe", lambda e, hl=hl, cur_=cur_: e.matmul(pb4[:, 1, hl, :], lhsT=cur_[L, 0, hl, :], rhs=cur_[L, 1, hl, :], start=True, stop=True),
                                     reads=[bcur], writes=[bBb])
                        for hl in range(4):
                            S.op("pe", lambda e, hl=hl, cur_=cur_: e.matmul(pa3[:, hl, :], lhsT=cur_[L, 1, hl, :], rhs=Wt[L, hl, :], start=True, stop=True),
                                 reads=[bcur, bWt[j]], writes=[bBa])
                        if kx < 5:
                            S.op("dve", lambda e, nxt_=nxt_: e.tensor_copy(out=nxt_[L, :, :, :], in_=pb4), reads=[bBb], writes=[bnxt])
                        S.op("dve", lambda e: e.tensor_tensor(out=Wt[L, :, :], in0=pa3, in1=Wt[L, :, :], op=ALU.add), reads=[bBa, bWt[j]], writes=[bWt[j]])
                        yield
                    tm = TM[k3]
                    hc = lambda hl: slice(hl * 64, (hl + 1) * 64)
                    for hl in range(4):
                        S.op("pe", lambda e, hl=hl: e.matmul(pb4[:, 0, hl, :], lhsT=Wt[L, hl, 0:64], rhs=tm[L, 4, hc(hl)], start=True, stop=True),
                             reads=[bWt[j], bTM[k3]], writes=[bBb])
                    for hl in range(4):
                        S.op("pe", lambda e, hl=hl: e.matmul(pb4[:, 1, hl, :], lhsT=tm[L, 4, hc(hl)], rhs=Wt[L, hl, 64:128], start=True, stop=False),
                             reads=[bWt[j], bTM[k3]], writes=[bBb])
                        S.op("pe", lambda e, hl=hl: e.matmul(pb4[:, 1, hl, :], lhsT=tm[L, 5, hc(hl)], rhs=Vb[k3][L, hc(hl)], start=False, stop=True),
                             reads=[bVb[k3], bTM[k3]], writes=[bBb])
                    for hl in range(4):
                        S.op("pe", lambda e, hl=hl: e.matmul(pa4[:, 0, hl, :], lhsT=Wt[L, hl, 0:64], rhs=SAM[L, hl, 64:128], start=True, stop=False),
                             reads=[bWt[j], bSAM[j]], writes=[bBa])
                        S.op("pe", lambda e, hl=hl: e.matmul(pa4[:, 0, hl, :], lhsT=tm[L, 1, hc(hl)], rhs=identb[L, L], start=False, stop=True),
                             reads=[bTM[k3], bconst], writes=[bBa])
                    S.op("dve", lambda e: e.tensor_tensor(out=DG[L, :, :], in0=EC[k3][L, :].rearrange("p (h c) -> p h c", h=4),
                                                          in1=identf[L, None, lo:lo + 64].to_broadcast([64, 4, 64]), op=ALU.mult),
                         reads=[bEC[k3], bconst], writes=[bDG[j]])
                    S.op("dve", lambda e: e.tensor_tensor(out=MTs[L, :, :], in0=pb4[:, 0, :, :], in1=DG[L, :, :], op=ALU.add),
                         reads=[bBb, bDG[j]], writes=[bMTs[j]])
                    S.op("dve", lambda e: e.tensor_copy(out=Gs[L, :, :], in_=pb4[:, 1, :, :]), reads=[bBb], writes=[bGs[j]])
                    S.op("dve", lambda e: e.tensor_copy(out=RpT[L, :, :], in_=pa4[:, 0, :, :]), reads=[bBa], writes=[bRpT[j]])
                    yield
                    for hl in range(4):
                        S.op("pe", lambda e, hl=hl: e.matmul(pa4[:, 1, hl, :], lhsT=SAM[L, hl, 64:128], rhs=Wt[L, hl, 64:128], start=True, stop=False),
                             reads=[bWt[j], bSAM[j]], writes=[bBa])
                        S.op("pe", lambda e, hl=hl: e.matmul(pa4[:, 1, hl, :], lhsT=SKM[L, hl, 64:128], rhs=Vb[k3][L, hc(hl)], start=False, stop=False),
                             reads=[bSKM[j], bVb[k3]], writes=[bBa])
                        S.op("pe", lambda e, hl=hl: e.matmul(pa4[:, 1, hl, :], lhsT=RpT[L, hl, :], rhs=Hb[L, hl, :], start=False, stop=True),
                             reads=[bRpT[j], bHb[j]], writes=[bBa])
                    for hl in range(4):
                        S.op("pe", lambda e, hl=hl: e.matmul(pb4[:, 0, hl, :], lhsT=MTs[L, hl, :], rhs=Hf[L, hl, :], start=True, stop=True),
                             reads=[bMTs[j], bHf[j]], writes=[bBb])
                    S.op("dve", lambda e: e.tensor_copy(out=Yt[k][L, :].rearrange("p (h c) -> p h c", h=4), in_=pa4[:, 1, :, :]), reads=[bBa], writes=[bYt[k]])
                    Lo = slice(64 * (1 - j), 64 * (1 - j) + 64)
                    S.op("dve", lambda e: e.tensor_tensor(out=Hf[Lo, :, :], in0=pb4[:, 0, :, :], in1=Gs[L, :, :], op=ALU.add),
                         reads=[bBb, bGs[j]], writes=[bHf[1 - j]])
                    S.op("dve", lambda e: e.tensor_copy(out=Hb[Lo, :, :], in_=Hf[Lo, :, :]), reads=[bHf[1 - j]], writes=[bHb[1 - j]])
                    yield

                def post_R(tau):
                    k = tau % 2
                    k3 = tau % 3
                    v3 = lambda ap: ap.rearrange("p (h c) -> p h c", h=4)
                    yt = Yt[k]
                    for hl in range(4):
                        S.op("act", lambda e, hl=hl: e.activation(out=ysq[:, hl * 64:(hl + 1) * 64], in_=yt[:, hl * 64:(hl + 1) * 64], func=AF.Copy,
                                                                 accum_out=pm[:, hl:hl + 1]), reads=[bYt[k]], writes=[bp["ysq"], bp["pm"]])
                    S.op("pool", lambda e: e.tensor_scalar(out=pm[:, 4:8], in0=pm[:, 0:4], scalar1=1.0 / 64, scalar2=None, op0=ALU.mult),
                         reads=[bp["pm"]], writes=[bp["pm"]])
                    yield
                    S.op("pool", lambda e: e.tensor_tensor(out=v3(yc[:]), in0=v3(yt[:]), in1=pm[:, 4:8, None].to_broadcast([128, 4, 64]), op=ALU.subtract),
                         reads=[bYt[k], bp["pm"]], writes=[bp["yc"]])
                    yield
                    for hl in range(4):
                        S.op("act", lambda e, hl=hl: e.activation(out=ysq[:, hl * 64:(hl + 1) * 64], in_=yc[:, hl * 64:(hl + 1) * 64], func=AF.Square,
                                                                 accum_out=pm[:, 8 + hl:9 + hl]), reads=[bp["yc"]], writes=[bp["ysq"], bp["pm"]])
                    S.op("pool", lambda e: e.tensor_scalar(out=pm[:, 8:12], in0=pm[:, 8:12], scalar1=1.0 / 64, scalar2=GN_EPS, op0=ALU.mult, op1=ALU.add),
                         reads=[bp["pm"]], writes=[bp["pm"]])
                    yield
                    S.op("pool", lambda e: e.tensor_tensor(out=pm[:, 12:16], in0=pm[:, 8:12], in1=neghalf[:, 0:4], op=ALU.pow),
                         reads=[bp["pm"], bconst], writes=[bp["pm"]])
                    S.op("pool", lambda e: e.tensor_tensor(out=v3(yc[:]), in0=v3(yc[:]), in1=pm[:, 12:16, None].to_broadcast([128, 4, 64]), op=ALU.mult),
                         reads=[bp["yc"], bp["pm"]], writes=[bp["yc"]])
                    S.op("pool", lambda e: e.tensor_tensor(out=yc[:], in0=yc[:], in1=pbc[:, 3, :], op=ALU.mult), reads=[bp["yc"], brc], writes=[bp["yc"]])
                    S.op("pool", lambda e: e.tensor_tensor(out=yc[:], in0=yc[:], in1=pbc[:, 4, :], op=ALU.add), reads=[bp["yc"], brc], writes=[bp["yc"]])
                    S.op("pool", lambda e: e.tensor_tensor(out=yc[:], in0=yc[:], in1=bonus[k3][:], op=ALU.add), reads=[bp["yc"], bbonus[k3]], writes=[bp["yc"]])
                    S.op("pool", lambda e: e.tensor_tensor(out=ygb[:], in0=yc[:], in1=t1g[k3][:], op=ALU.mult), reads=[bp["yc"], bt1g[k3]], writes=[bp["ygb"]])
                    yield
                    yield
                    yield
                    yield
                    pyt = pb_bf(3)[:, 0:256].rearrange("p (a t) -> p a t", a=2)
                    for a_ in range(2):
                        S.op("pe", lambda e, a_=a_: e.transpose(out=pyt[:, a_, :], in_=ygb[:, a_ * 128:(a_ + 1) * 128], identity=identb[:]),
                             reads=[bp["ygb"], bconst], writes=[bPB[3]])
                    S.op("act", lambda e: e.copy(out=YgT[:, 2 * half:2 * half + 2, tau * 128:(tau + 1) * 128], in_=pyt),
                         reads=[bPB[3]], writes=[bYg[2 * half], bYg[2 * half + 1]])
                    yield

                def backend_R(tau):
                    g0 = chunk_R(tau, 0)
                    g1 = chunk_R(tau, 1)
                    for st_ in range(9):
                        next(g0)
                        next(g1)
                        yield
                    next(g0)
                    yield
                    next(g1)
                    yield

                def run_streams(streams):
                    live = list(streams)
                    while live:
                        for g in list(live):
                            try:
                                next(g)
                            except StopIteration:
                                live.remove(g)

                ntr = dbg.get('nt_R', {}).get(half, nt_lim)
                a_gen = None; a_next = 0; a_done = 0
                p_gen = None; p_next = 0; p_done = 0
                b_gen = None; b_tile = 0
                f_gen = None; f_next = 0; f_done = 0
                q_gen = None; q_next = 0; q_done = 0
                while q_done < ntr:
                    if f_gen is None and f_next < ntr and f_next <= a_next + dbg.get('aheadF', 2):
                        f_gen = tile_RF(f_next)
                    if f_gen is not None:
                        try:
                            next(f_gen)
                        except StopIteration:
                            f_gen = None
                            f_next += 1
                            f_done = f_next
                    if a_gen is None and a_next < ntr and f_done > a_next and a_next <= p_done + dbg.get('aheadA', 1) and a_next <= b_tile + 2:
                        a_gen = tile_RA(a_next)
                    if a_gen is not None:
                        try:
                            next(a_gen)
                        except StopIteration:
                            a_gen = None
                            a_next += 1
                            a_done = a_next
                    if p_gen is None and p_next < ntr and a_done > p_next and p_next <= q_next + dbg.get('aheadB', 2):
                        p_gen = tile_RB(p_next)
                    if p_gen is not None:
                        try:
                            next(p_gen)
                        except StopIteration:
                            p_gen = None
                            p_next += 1
                            p_done = p_next
                    if b_gen is None and b_tile < ntr and p_done > b_tile and q_done > b_tile - 2:
                        b_gen = backend_R(b_tile)
                    if b_gen is not None:
                        try:
                            next(b_gen)
                        except StopIteration:
                            b_gen = None
                            b_tile += 1
                    if q_gen is None and q_next < ntr and b_tile > q_next:
                        q_gen = post_R(q_next)
                    if q_gen is not None:
                        try:
                            next(q_gen)
                        except StopIteration:
                            q_gen = None
                            q_next += 1
                            q_done = q_next
                S.barrier()

        for half_ in do_R:
            phase_R(half_)

        def phase_M(pair):
            with ExitStack() as ms:
                def msb(name, shape, dt=F32):
                    return sb("M%d_%s" % (pair, name), shape, dt, stack=ms)

                KA = [msb("KA%d" % i, [82, T], BF16) for i in range(2)]
                QA = [msb("QA%d" % i, [82, T], BF16) for i in range(2)]
                Vaug = msb("Vaug", [128, NT, 2, 65], BF16)
                SG = msb("SG", [128, T], BF16)
                GT = msb("GT", [128, T], BF16)
                Yall = msb("Yall", [128, NT, 128], BF16)
                Wp = msb("Wp", [128, 8, 512], BF16)
                xTb = [msb("xTb%d" % i, [128, 8, 512], BF16) for i in range(2)]
                qT32 = msb("qT32", [128, 512])
                kmBD = msb("kmBD", [128, 32])
                pastb = msb("pastb", [128, 16, 16])
                ownb = msb("ownb", [128, 16, 16])
                causal = msb("causal", [128, 2, 256], BF16)
                gm = msb("gm", [128, 4, 16]); m8 = msb("m8", [128, 4, 8]); lt = msb("lt", [128, 4, 16])
                mbts = [msb("mbt%d" % i, [128, 4, 2, 32], BF16) for i in range(2)]
                bmbts = [Buf() for _ in range(2)]
                PT = [msb("PT%d" % i, [128, 2, 256], BF16) for i in range(3)]
                rec = msb("rec", [128, 4])
                for i_ in (2, 3):
                    xt.append(msb("xt%d" % i_, [128, D])); bxt.append(Buf())
                    xnb.append(msb("xnb%d" % i_, [128, D], BF16)); bxnb.append(Buf())
                    fes.append(msb("fes%d" % i_, [128, 4])); bfes.append(Buf())
                fe_nbuf[0] = 4
                bKA = [Buf() for _ in range(2)]; bQA = [Buf() for _ in range(2)]
                bVaug = Buf(); bSG = Buf(); bYall = Buf(); bWp = Buf()
                bxTb = [Buf() for _ in range(2)]; bq32 = Buf(); bkm = Buf(); bmc = Buf()
                bgm = Buf(); bm8 = Buf(); blt = Buf()
                bPT = [Buf() for _ in range(3)]; brec = Buf()
                dma_group([(pastb[:].rearrange("p a b -> p (a b)"), dr["c_past"][0:1, :].partition_broadcast(128)),
                           (ownb[:].rearrange("p a b -> p (a b)"), dr["c_own"][0:1, :].partition_broadcast(128)),
                           (causal[:], dr["c_causal"][:])], [bmc])
                S.op("pool", lambda e: e.memset(kmBD[:], 0.0), writes=[bkm])
                for i_ in range(2):
                    S.op("pool", lambda e, i_=i_: e.memset(mbts[i_][:], 0.0), writes=[bmbts[i_]])
                S.op("pool", lambda e: e.memset(Vaug[:, :, :, 64:65], 1.0), writes=[bVaug])
                for hq in range(2):
                    h = 2 * pair + hq
                    dma_group([(KA[hq][64:80, :], dr["c_onehot"][:]), (KA[hq][80:82, :], dr["c_krows"][h]),
                               (QA[hq][80:82, :], dr["c_qrows"][h])], [bKA[hq], bQA[hq]])
                cols = [1664 + 128 * pair, 2176 + 128 * pair, 2688 + 128 * pair, 3712 + 128 * pair]
                with ExitStack() as wsm:
                    alloc_staging(wsm)
                    for i, c0 in enumerate(cols):
                        st, bst = load_w(w_in_v[:, :, c0:c0 + 128], 128)
                        S.op("pool", lambda e, st=st, i=i: e.tensor_tensor(out=Wp[:, :, 128 * i:128 * i + 128], in0=st[:, :, 0:128],
                                                                          in1=gpre[:, :, None].to_broadcast([128, 8, 128]), op=ALU.mult),
                             reads=[bst, bconst], writes=[bWp])
                    S.barrier()

                nblk = nt_lim // 4 if nt_lim >= 4 else 1
                def m_front(tb):
                    kb = tb % 2
                    xb = xTb[kb]
                    for jj in range(4):
                        tile_ = 4 * tb + jj
                        if tile_ == 0:
                            fe1(0)
                            if 4 * nblk > 1:
                                fe1(1)
                        if tile_ + 2 < 4 * nblk:
                            fe1(tile_ + 2)
                        fe2(tile_, xb[:, :, 128 * jj:128 * jj + 128], bxTb[kb], evac_eng="dve")

                def m_block(tb):
                    kb = tb % 2
                    xb = xTb[kb]
                    mbt = mbts[tb % 2]
                    bmbt = bmbts[tb % 2]
                    tsl = slice(512 * tb, 512 * tb + 512)
                    for (bank, wi) in ((1, 1), (2, 0), (4, 3)):
                        for c in range(8):
                            S.op("pe", lambda e, c=c, bank=bank, wi=wi: e.matmul(PB[bank][:, :], lhsT=Wp[:, c, 128 * wi:128 * wi + 128], rhs=xb[:, c, :],
                                                                               start=(c == 0), stop=(c == 7)),
                                 reads=[bWp, bxTb[kb]], writes=[bPB[bank]])
                    pv = PB[3][:, :].rearrange("p (a c) -> p a c", a=4)
                    for jj in range(4):
                        for c in range(8):
                            S.op("pe", lambda e, c=c, jj=jj: e.matmul(pv[:, jj, :], lhsT=xb[:, c, 128 * jj:128 * jj + 128], rhs=Wp[:, c, 256:384],
                                                                     start=(c == 0), stop=(c == 7)),
                                 reads=[bWp, bxTb[kb]], writes=[bPB[3]])
                    S.op("act", lambda e: e.copy(out=KA[0][0:64, tsl], in_=PB[1][0:64, :]), reads=[bPB[1]], writes=[bKA[0]])
                    S.op("act", lambda e: e.copy(out=KA[1][0:64, tsl], in_=PB[1][64:128, :]), reads=[bPB[1]], writes=[bKA[1]])
                    for hq in range(2):
                        for bb in range(2):
                            blk = 2 * tb + bb
                            S.op("dve", lambda e, hq=hq, bb=bb, blk=blk: e.tensor_reduce(out=kmBD[64 * hq:64 * hq + 64, 16 * hq + blk:16 * hq + blk + 1],
                                                                                        in_=PB[1][64 * hq:64 * hq + 64, 256 * bb:256 * bb + 256], axis=AX.X, op=ALU.add),
                                 reads=[bPB[1]], writes=[bkm])
                    S.op("act", lambda e: e.mul(out=QA[0][0:64, tsl], in_=PB[2][0:64, :], mul=0.125), reads=[bPB[2]], writes=[bQA[0]])
                    S.op("act", lambda e: e.mul(out=QA[1][0:64, tsl], in_=PB[2][64:128, :], mul=0.125), reads=[bPB[2]], writes=[bQA[1]])
                    S.op("dve", lambda e: e.tensor_copy(out=qT32[:], in_=PB[2][:, :]), reads=[bPB[2]], writes=[bq32])
                    S.op("act", lambda e: e.activation(out=SG[:, tsl], in_=PB[4][:, :], func=AF.Tanh, scale=0.5), reads=[bPB[4]], writes=[bSG])
                    S.op("dve", lambda e: e.tensor_copy(out=GT[:, tsl], in_=PB[4][:, :]), reads=[bPB[4]], writes=[bSG])
                    S.op("act", lambda e: e.copy(out=Vaug[:, 4 * tb:4 * tb + 4, :, 0:64], in_=PB[3][:, :].rearrange("p (a h c) -> p a h c", a=4, h=2)),
                         reads=[bPB[3]], writes=[bVaug])
                    pg = PB[5][:, 0:128].rearrange("p (a c) -> p a c", a=4)
                    for jj in range(4):
                        S.op("pe", lambda e, jj=jj: e.matmul(pg[:, jj, :], lhsT=qT32[:, 128 * jj:128 * jj + 128], rhs=kmBD[:, :], start=True, stop=True),
                             reads=[bq32, bkm], writes=[bPB[5]])
                    for bb in range(2):
                        blk = 2 * tb + bb
                        pg2 = PB[5][:, 64 * bb:64 * bb + 64].rearrange("p (a c) -> p a c", a=4)
                        S.op("dve", lambda e, pg2=pg2, blk=blk: e.tensor_tensor(out=gm[:], in0=pg2, in1=pastb[:, blk:blk + 1, :].to_broadcast([128, 4, 16]), op=ALU.add),
                             reads=[bPB[5], bmc], writes=[bgm])
                        for g in range(4):
                            S.op("dve", lambda e, g=g: e.max(out=m8[:, g, :], in_=gm[:, g, :]), reads=[bgm], writes=[bm8])
                        S.op("dve", lambda e: e.tensor_tensor(out=lt[:], in0=gm[:], in1=m8[:, :, 2:3].to_broadcast([128, 4, 16]), op=ALU.is_lt),
                             reads=[bgm, bm8], writes=[blt])
                        S.op("dve", lambda e, bb=bb, blk=blk: e.scalar_tensor_tensor(out=mbt[:, 2 * bb:2 * bb + 2, :, 0:16].rearrange("p a h c -> p (a h) c"),
                                                                                   in0=lt[:], scalar=-BIG,
                                                                                   in1=ownb[:, blk:blk + 1, :].to_broadcast([128, 4, 16]),
                                                                                   op0=ALU.mult, op1=ALU.max),
                             reads=[blt, bmc], writes=[bmbt])

                def m_block_b(tb):
                    tsl = slice(512 * tb, 512 * tb + 512)
                    mbt = mbts[tb % 2]
                    bmbt = bmbts[tb % 2]
                    pmt = pb_bf(6)[0:64, 0:512].rearrange("p (a t) -> p a t", a=4)
                    for jj in range(4):
                        S.op("pe", lambda e, jj=jj: e.transpose(out=pmt[:, jj, :], in_=mbt[:, jj, :, :].rearrange("p h c -> p (h c)"), identity=identb[:]),
                             reads=[bmbt, bconst], writes=[bPB[6]])
                    for hq in range(2):
                        S.op("act", lambda e, hq=hq: e.copy(out=QA[hq][64:80, tsl], in_=pb_bf(6)[32 * hq:32 * hq + 16, 0:512]),
                             reads=[bPB[6]], writes=[bQA[hq]])
                m_front(0)
                for tb in range(nblk):
                    if tb + 1 < nblk:
                        m_front(tb + 1)
                    m_block(tb)
                    if tb >= 1:
                        m_block_b(tb - 1)
                m_block_b(nblk - 1)
                S.barrier()
                sbanks = [1, 2, 3, 4]
                obanks = [5, 6]
                items = [(hq, i) for hq in range(2) for i in range(nblk * 2)]
                sctr = [0]

                def qk_stage(hq, i):
                    h = 2 * pair + hq
                    slope = 2.0 ** (-(h + 1))
                    res = []
                    for n in range(i + 1):
                        bank = sbanks[sctr[0] % 4]
                        pi = sctr[0] % 3
                        sctr[0] += 1
                        ps = PB[bank][:, :].rearrange("p (a t) -> p a t", a=2)
                        for sc in range(2):
                            s0 = 256 * n + 128 * sc
                            S.op("pe", lambda e, sc=sc, s0=s0, ps=ps, n=n: e.matmul(ps[:, sc, :], lhsT=KA[hq][0:82, s0:s0 + 128], rhs=QA[hq][0:82, 256 * i:256 * i + 256],
                                                                             start=True, stop=(n != i)),
                                 reads=[bKA[hq], bQA[hq]], writes=[bPB[bank]])
                            if n == i:
                                S.op("pe", lambda e, sc=sc, ps=ps: e.matmul(ps[:, sc, :], lhsT=identb[:, :], rhs=causal[:, sc, :], start=False, stop=True),
                                     reads=[bconst, bmc], writes=[bPB[bank]])
                        S.op("act", lambda e, ps=ps, pi=pi, n=n: e.activation(out=PT[pi][:], in_=ps, func=AF.Exp, bias=float(-slope * 256.0 * (i - n)), scale=1.0),
                             reads=[bPB[bank]], writes=[bPT[pi]])
                        res.append((pi, n))
                        yield (pi, n)

                LOOK = dbg.get("look", 2)

                def pv_stage(hq, i, it, pi, n):
                    ob_idx = ((5, 6), (7, 0))[it % 2]
                    ob = PB[ob_idx[0]], PB[ob_idx[1]]
                    for tc in range(2):
                        for sc in range(2):
                            S.op("pe", lambda e, tc=tc, sc=sc: e.matmul(ob[tc][:, 0:65], lhsT=PT[pi][:, sc, 128 * tc:128 * tc + 128],
                                                                       rhs=Vaug[:, 2 * n + sc, hq, :],
                                                                       start=(n == 0 and sc == 0), stop=(n == i and sc == 1)),
                                 reads=[bPT[pi], bVaug], writes=[bPB[ob_idx[tc]]])
                    if n == i:
                        for tc in range(2):
                            rc = rec[:, 2 * (it % 2) + tc:2 * (it % 2) + tc + 1]
                            S.op("dve", lambda e, tc=tc, rc=rc: e.reciprocal(out=rc, in_=ob[tc][:, 64:65]), reads=[bPB[ob_idx[tc]]], writes=[brec])
                            S.op("dve", lambda e, tc=tc, rc=rc: e.tensor_scalar(out=Yall[:, 2 * i + tc, 64 * hq:64 * hq + 64], in0=ob[tc][:, 0:64], scalar1=rc,
                                                                               scalar2=None, op0=ALU.mult),
                                 reads=[bPB[ob_idx[tc]], brec], writes=[bYall])

                pending = []
                for it, (hq, i) in enumerate(items):
                    for (pi, n) in qk_stage(hq, i):
                        pending.append((hq, i, it, pi, n))
                        if len(pending) > LOOK:
                            pv_stage(*pending.pop(0))
                while pending:
                    pv_stage(*pending.pop(0))
                ygm = msb("ygm", [128, 512], BF16)
                t1m = msb("t1m", [128, 512], BF16)
                bygm = Buf(); bt1m = Buf()
                def m_gate(tb):
                    tsl = slice(512 * tb, 512 * tb + 512)
                    pyt = pb_bf(7)[:, 0:512].rearrange("p (a t) -> p a t", a=4)
                    for jj in range(4):
                        S.op("pe", lambda e, jj=jj, tb=tb: e.transpose(out=pyt[:, jj, :], in_=Yall[:, 4 * tb + jj, :], identity=identb[:]),
                             reads=[bYall, bconst], writes=[bPB[7]])
                    S.op("dve", lambda e, tsl=tsl: e.scalar_tensor_tensor(out=t1m[:], in0=SG[:, tsl], scalar=1.0, in1=GT[:, tsl], op0=ALU.add, op1=ALU.mult),
                         reads=[bSG], writes=[bt1m])
                    S.op("dve", lambda e, tsl=tsl, pyt=pyt: e.scalar_tensor_tensor(out=YgT[:, 4 + pair, tsl], in0=t1m[:], scalar=0.5,
                                                                                  in1=pyt.rearrange("p a t -> p (a t)"), op0=ALU.mult, op1=ALU.mult),
                         reads=[bt1m, bPB[7]], writes=[bYg[4 + pair]])
                for tb in range(nblk):
                    m_gate(tb)
                S.barrier()
                for lst_ in (xt, bxt, xnb, bxnb, fes, bfes):
                    del lst_[2:]
                fe_nbuf[0] = 2

        for pair_ in do_M:
            phase_M(pair_)

        if dump_yg:
            for c in dbg.get("dump_chunks", range(8)):
                S.dma("sp", ygdump[:, c, 0:nt_lim * 128], YgT[:, c, 0:nt_lim * 128], reads=[bYg[c]], force=True)
        if do_O:
            with ExitStack() as os_:
                def osb(name, shape, dt=F32):
                    return sb("O_" + name, shape, dt, stack=os_)
                Wo = osb("Wo", [128, 8, D], BF16)
                gpb = osb("gpb", [128, D])
                bWo = Buf(); bgp = Buf()
                S.dma("sp", gpb[:], gpost_row[0:1, :].partition_broadcast(128), writes=[bgp])
                with ExitStack() as wso:
                    alloc_staging(wso)
                    for m4 in range(4):
                        st, bst = load_w(w_out_v[:, :, 256 * m4:256 * m4 + 256], 256)
                        S.op("pool", lambda e, st=st, m4=m4: e.tensor_copy(out=Wo[:, :, 256 * m4:256 * m4 + 256], in_=st[:, :, 0:256]),
                             reads=[bst], writes=[bWo])
                    S.barrier()
                ot = [osb("ot%d" % i, [128, D]) for i in range(2)]
                bot = [Buf() for _ in range(2)]
                osm = [osb("osm%d" % i, [128, 8]) for i in range(2)]
                bosm = [Buf() for _ in range(2)]
                ojunk = osb("ojunk", [128, 512], BF16)
                bojunk = Buf()
                def o_tile(tau):
                    k = tau % 2
                    banks = (1 + 2 * k, 2 + 2 * k)
                    if tau == 0:
                        S.dma("sp", xt[0][:], x[0:128, :], writes=[bxt[0]])
                    if tau + 1 < nt_lim:
                        S.dma("sp", xt[1 - k][:], x[(tau + 1) * 128:(tau + 2) * 128, :], writes=[bxt[1 - k]])
                    for hf in range(2):
                        for m in range(8):
                            S.op("pe", lambda e, m=m, hf=hf: e.matmul(PB[banks[hf]][:, :], lhsT=YgT[:, m, tau * 128:(tau + 1) * 128], rhs=Wo[:, m, 512 * hf:512 * hf + 512],
                                                                     start=(m == 0), stop=(m == 7)),
                                 reads=[bYg[m], bWo], writes=[bPB[banks[hf]]])
                    for hf in range(2):
                        S.op("act", lambda e, hf=hf: e.activation(out=ojunk[:], in_=PB[banks[hf]][:, :], func=AF.Square, accum_out=osm[k][:, hf:hf + 1]),
                             reads=[bPB[banks[hf]]], writes=[bojunk, bosm[k]])
                    S.op("dve", lambda e: e.tensor_tensor(out=osm[k][:, 2:3], in0=osm[k][:, 0:1], in1=osm[k][:, 1:2], op=ALU.add), reads=[bosm[k]], writes=[bosm[k]])
                    S.op("dve", lambda e: e.tensor_scalar(out=osm[k][:, 3:4], in0=osm[k][:, 2:3], scalar1=1.0 / D, scalar2=RMS_EPS, op0=ALU.mult, op1=ALU.add),
                         reads=[bosm[k]], writes=[bosm[k]])
                    S.op("pool", lambda e: e.tensor_tensor(out=osm[k][:, 4:5], in0=osm[k][:, 3:4], in1=neghalf[:, 0:1], op=ALU.pow),
                         reads=[bosm[k], bconst], writes=[bosm[k]])
                    for hf in range(2):
                        S.op("dve", lambda e, hf=hf: e.scalar_tensor_tensor(out=ot[k][:, 512 * hf:512 * hf + 512], in0=PB[banks[hf]][:, :], scalar=osm[k][:, 4:5],
                                                                           in1=gpb[:, 512 * hf:512 * hf + 512], op0=ALU.mult, op1=ALU.mult),
                             reads=[bPB[banks[hf]], bosm[k], bgp], writes=[bot[k]])
                    S.op("pool", lambda e: e.tensor_tensor(out=ot[k][:], in0=ot[k][:], in1=xt[k][:], op=ALU.add), reads=[bot[k], bxt[k]], writes=[bot[k]])
                    S.dma("sp", out[tau * 128:(tau + 1) * 128, :], ot[k][:], reads=[bot[k]])
                for tau in range(nt_lim):
                    o_tile(tau)
        S.emit()
        S.close()
    return nc


def make_inputs(x_b, p):
    m = {"x": np.ascontiguousarray(x_b, dtype=np.float32)}
    m["w_in"] = np.ascontiguousarray(p["w_in"][0], dtype=np.float32)
    m["w_out"] = np.ascontiguousarray(p["w_out"][0], dtype=np.float32)
    m["gpre_pc"] = np.ascontiguousarray(p["g_pre"][0].reshape(8, 128).T, dtype=np.float32)
    m["mu_row"] = np.ascontiguousarray(p["tshift_mu"][0].reshape(1, 1664), dtype=np.float32)
    m["w2w0"] = np.ascontiguousarray(np.concatenate([p["w2"][0], p["w0"][0].reshape(1, 512)], axis=0), dtype=np.float32)
    m["a2a0"] = np.ascontiguousarray(np.concatenate([p["a2"][0], p["a0"][0].reshape(1, 512)], axis=0), dtype=np.float32)
    m["prow"] = np.ascontiguousarray(np.stack([p["k_k"][0], p["k_a"][0], p["r_k"][0].reshape(512), p["lnx_w"][0], p["lnx_b"][0]], axis=0),
                                     dtype=np.float32)
    m["gpost_row"] = np.ascontiguousarray(p["g_post"][0].reshape(1, D), dtype=np.float32)
    m.update(host_consts())
    return m


def kernel(x, g_pre, w_in, tshift_mu, w0, w2, a0, a2, k_k, k_a, r_k, lnx_w, lnx_b, w_out, g_post):
    p = dict(g_pre=g_pre, w_in=w_in, tshift_mu=tshift_mu, w0=w0, w2=w2, a0=a0, a2=a2, k_k=k_k, k_a=k_a, r_k=r_k,
             lnx_w=lnx_w, lnx_b=lnx_b, w_out=w_out, g_post=g_post)
    p = {k: np.asarray(v) for k, v in p.items()}
    x = np.asarray(x)
    nc = build()
    in_maps = [make_inputs(x[b], p) for b in range(8)]
    res = run_bass_kernel_spmd(nc, in_maps, core_ids=list(range(8)))
    return np.stack([np.asarray(r["out"], dtype=np.float32) for r in res.results], axis=0)
```
